# Optimizing a Trainium2 kernel written in Bass

```python
import math
import jax, jax.numpy as jnp
from jax import lax
import numpy as np

D_MODEL = 1024
BATCH = 8
SEQ = 2048
DEPTH = 1
DEC_BATCH = 128
DEC_SEQ = 8
PAST_LEN = 16384
PAGE_SIZE = 128

D_MIX = D_MODEL
D_A = D_MIX // 2
A_HEADS = 4
A_HEAD_DIM = D_A // A_HEADS
CHUNK = 128
D_B = D_MIX - D_A
B_HEADS = 8
B_HEAD_DIM = D_B // B_HEADS
CONV_W = 4
LRU_C = 8.0
D_IN = 2 * D_A + 2 * D_B
N_GROUPS = 4
EXPERTS_PER_GROUP = 8
N_EXPERTS = N_GROUPS * EXPERTS_PER_GROUP
TOP_K = 2
D_EXPERT = D_MODEL // 2
LN_EPS = 1e-5
ALPHA = (2.0 * DEPTH) ** 0.25
BETA = (8.0 * DEPTH) ** -0.25

kernel_name = "hymba_gmlp_rglru_hmoe_step"


def layer_norm(x, g, b):
    xf = x.astype(jnp.float32)
    mu = jnp.mean(xf, axis=-1, keepdims=True)
    var = jnp.mean(jnp.square(xf - mu), axis=-1, keepdims=True)
    y = (xf - mu) * lax.rsqrt(var + LN_EPS)
    return (y * g.astype(jnp.float32) + b.astype(jnp.float32)).astype(x.dtype)


def gmlp_spatial(u, v, w_s, b_s):
    bsz, s, _ = v.shape
    pad = (-s) % CHUNK
    vp = jnp.pad(v, ((0, 0), (0, pad), (0, 0)))
    n_chunks = (s + pad) // CHUNK
    v5 = vp.reshape(bsz, n_chunks, CHUNK, A_HEADS, A_HEAD_DIM)
    mask = jnp.tril(jnp.ones((CHUNK, CHUNK), dtype=bool))
    ws = jnp.where(mask[None], w_s, jnp.zeros_like(w_s))
    mixed = jnp.einsum('hij,bcjhd->bcihd', ws, v5)
    mixed = mixed + jnp.transpose(b_s)[None, None, :, :, None]
    mixed = mixed.reshape(bsz, n_chunks * CHUNK, D_A)[:, :s]
    return u * mixed


def causal_conv(x, buf, w, b):
    s = x.shape[1]
    xp = jnp.concatenate([buf.astype(x.dtype), x], axis=1)
    out = b
    for k in range(CONV_W):
        out = out + xp[:, k:k + s] * w[k]
    return out, xp[:, -(CONV_W - 1):]


def rglru(x, h0, w_a, b_a, w_x, b_x, lam):
    bsz, s, _ = x.shape
    xh = x.reshape(bsz, s, B_HEADS, B_HEAD_DIM)
    gate_a = jax.nn.sigmoid(jnp.einsum('bshi,hij->bshj', xh, w_a) + b_a).reshape(bsz, s, D_B)
    gate_x = jax.nn.sigmoid(jnp.einsum('bshi,hij->bshj', xh, w_x) + b_x).reshape(bsz, s, D_B)
    log_a = -LRU_C * gate_a.astype(jnp.float32) * jax.nn.softplus(-lam.astype(jnp.float32))
    a = jnp.exp(log_a)
    mult = jnp.sqrt(-jnp.expm1(2.0 * log_a))
    bx = mult * (gate_x * x).astype(jnp.float32)

    def step(h, ab):
        a_t, b_t = ab
        h = a_t * h + b_t
        return h, h

    h_last, hs = lax.scan(step, h0.astype(jnp.float32),
                          (jnp.transpose(a, (1, 0, 2)), jnp.transpose(bx, (1, 0, 2))))
    return jnp.transpose(hs, (1, 0, 2)).astype(x.dtype), h_last.astype(x.dtype)


def hier_moe(x, w_rg, b_rg, w_re, b_re, w1, w3, w2):
    shp = x.shape
    x2 = x.reshape(-1, D_MODEL)
    t = x2.shape[0]
    lg = (x2 @ w_rg + b_rg).astype(jnp.float32)
    pg = jax.nn.softmax(lg, axis=-1)
    gsel = jnp.argmax(lg, axis=-1)
    pgsel = jnp.take_along_axis(pg, gsel[:, None], axis=-1)
    le = (x2 @ w_re + b_re).astype(jnp.float32).reshape(t, N_GROUPS, EXPERTS_PER_GROUP)
    le_sel = jnp.take_along_axis(le, gsel[:, None, None], axis=1)[:, 0]
    topv, topi = lax.top_k(le_sel, TOP_K)
    wk = jax.nn.softmax(topv, axis=-1) * pgsel
    within = jnp.sum(jax.nn.one_hot(topi, EXPERTS_PER_GROUP, dtype=jnp.float32) * wk[..., None], axis=1)
    gates = (jax.nn.one_hot(gsel, N_GROUPS, dtype=jnp.float32)[:, :, None]
             * within[:, None, :]).reshape(t, N_EXPERTS).astype(x.dtype)

    def expert_step(acc, p):
        w1e, w3e, w2e, ge = p
        hdn = jax.nn.silu(x2 @ w1e) * (x2 @ w3e)
        return acc + ge[:, None] * (hdn @ w2e), None

    acc, _ = lax.scan(expert_step, jnp.zeros_like(x2), (w1, w3, w2, jnp.transpose(gates)))
    return acc.reshape(shp)


def hybrid_layer(x, c, h0, conv0, w_ada, b_ada, w_in, w_s, b_s, lnv_g, lnv_b,
                 conv_w, conv_b, lru_wa, lru_ba, lru_wx, lru_bx, lru_lam, w_out,
                 ln1_g, ln1_b, w_rg, b_rg, w_re, b_re, w1, w3, w2, ln2_g, ln2_b):
    mod = jax.nn.silu(c) @ w_ada + b_ada
    sh1, sc1, g1, sh2, sc2, g2 = jnp.split(mod[:, None, :], 6, axis=-1)
    h = x * (1.0 + sc1) + sh1
    proj = h @ w_in
    u, v, xr, yr = jnp.split(proj, [D_A, 2 * D_A, 2 * D_A + D_B], axis=-1)
    v = layer_norm(v, lnv_g, lnv_b)
    out_a = gmlp_spatial(u, v, w_s, b_s)
    xc, conv_new = causal_conv(xr, conv0, conv_w, conv_b)
    hs, h_last = rglru(xc, h0, lru_wa, lru_ba, lru_wx, lru_bx, lru_lam)
    out_b = hs * jax.nn.gelu(yr)
    mix = jnp.concatenate([out_a, out_b], axis=-1) @ w_out
    x = layer_norm(ALPHA * x + (1.0 + g1) * mix, ln1_g, ln1_b)
    h = x * (1.0 + sc2) + sh2
    x = layer_norm(ALPHA * x + (1.0 + g2) * hier_moe(h, w_rg, b_rg, w_re, b_re, w1, w3, w2), ln2_g, ln2_b)
    return x, h_last, conv_new, v


def setup_inputs(seed: int = 0) -> dict:
    key = jax.random.key(seed)
    ks = jax.random.split(key, 40)
    f = jnp.float32
    nrm = lambda k, shp, s: jax.random.normal(k, shp, f) * s
    a0 = jax.random.uniform(ks[20], (DEPTH, D_B), f, 0.9, 0.999)
    sig = a0 ** (1.0 / LRU_C)
    lru_lam = jnp.log(sig) - jnp.log1p(-sig)
    return {
        "x_prompt": nrm(ks[0], (BATCH, SEQ, D_MODEL), 1.0),
        "x_sample": nrm(ks[1], (DEC_BATCH, DEC_SEQ, D_MODEL), 1.0),
        "state_rglru_h": nrm(ks[2], (DEPTH, DEC_BATCH, D_B), 1.0),
        "state_conv": nrm(ks[3], (DEPTH, DEC_BATCH, CONV_W - 1, D_B), 1.0),
        "c_prompt": nrm(ks[4], (BATCH, D_MODEL), 1.0),
        "c_sample": nrm(ks[5], (DEC_BATCH, D_MODEL), 1.0),
        "w_ada": nrm(ks[6], (DEPTH, D_MODEL, 6 * D_MODEL), 0.1 * D_MODEL ** -0.5),
        "b_ada": nrm(ks[7], (DEPTH, 6 * D_MODEL), 0.01),
        "w_in": nrm(ks[8], (DEPTH, D_MODEL, D_IN), D_MODEL ** -0.5),
        "w_s": nrm(ks[9], (DEPTH, A_HEADS, CHUNK, CHUNK), CHUNK ** -0.5),
        "b_s": 1.0 + nrm(ks[10], (DEPTH, A_HEADS, CHUNK), 0.02),
        "lnv_g": 1.0 + nrm(ks[11], (DEPTH, D_A), 0.02),
        "lnv_b": nrm(ks[12], (DEPTH, D_A), 0.02),
        "conv_w": nrm(ks[13], (DEPTH, CONV_W, D_B), CONV_W ** -0.5),
        "conv_b": nrm(ks[14], (DEPTH, D_B), 0.02),
        "lru_wa": nrm(ks[15], (DEPTH, B_HEADS, B_HEAD_DIM, B_HEAD_DIM), B_HEAD_DIM ** -0.5),
        "lru_ba": nrm(ks[16], (DEPTH, B_HEADS, B_HEAD_DIM), 0.02),
        "lru_wx": nrm(ks[17], (DEPTH, B_HEADS, B_HEAD_DIM, B_HEAD_DIM), B_HEAD_DIM ** -0.5),
        "lru_bx": nrm(ks[18], (DEPTH, B_HEADS, B_HEAD_DIM), 0.02),
        "lru_lam": lru_lam,
        "w_out": nrm(ks[21], (DEPTH, D_MIX, D_MODEL), BETA * D_MIX ** -0.5),
        "ln1_g": 1.0 + nrm(ks[22], (DEPTH, D_MODEL), 0.02),
        "ln1_b": nrm(ks[23], (DEPTH, D_MODEL), 0.02),
        "w_rg": nrm(ks[24], (DEPTH, D_MODEL, N_GROUPS), D_MODEL ** -0.5),
        "b_rg": nrm(ks[25], (DEPTH, N_GROUPS), 0.01),
        "w_re": nrm(ks[26], (DEPTH, D_MODEL, N_EXPERTS), D_MODEL ** -0.5),
        "b_re": nrm(ks[27], (DEPTH, N_EXPERTS), 0.01),
        "w1": nrm(ks[28], (DEPTH, N_EXPERTS, D_MODEL, D_EXPERT), D_MODEL ** -0.5),
        "w3": nrm(ks[29], (DEPTH, N_EXPERTS, D_MODEL, D_EXPERT), D_MODEL ** -0.5),
        "w2": nrm(ks[30], (DEPTH, N_EXPERTS, D_EXPERT, D_MODEL), BETA * D_EXPERT ** -0.5),
        "ln2_g": 1.0 + nrm(ks[31], (DEPTH, D_MODEL), 0.02),
        "ln2_b": nrm(ks[32], (DEPTH, D_MODEL), 0.02),
    }


def reference(x_prompt, x_sample, state_rglru_h, state_conv, c_prompt, c_sample,
              w_ada, b_ada, w_in, w_s, b_s, lnv_g, lnv_b, conv_w, conv_b,
              lru_wa, lru_ba, lru_wx, lru_bx, lru_lam, w_out, ln1_g, ln1_b,
              w_rg, b_rg, w_re, b_re, w1, w3, w2, ln2_g, ln2_b):
    xp, xs = x_prompt, x_sample
    n_prompt = x_prompt.shape[0]
    hp_list, cp_list, hs_list, cs_list, vs_list = [], [], [], [], []
    for l in range(DEPTH):
        lw = (w_ada[l], b_ada[l], w_in[l], w_s[l], b_s[l], lnv_g[l], lnv_b[l],
              conv_w[l], conv_b[l], lru_wa[l], lru_ba[l], lru_wx[l], lru_bx[l],
              lru_lam[l], w_out[l], ln1_g[l], ln1_b[l], w_rg[l], b_rg[l],
              w_re[l], b_re[l], w1[l], w3[l], w2[l], ln2_g[l], ln2_b[l])
        h0p = jnp.zeros((n_prompt, D_B), x_prompt.dtype)
        conv0p = jnp.zeros((n_prompt, CONV_W - 1, D_B), x_prompt.dtype)
        xp, hp, cp, _ = hybrid_layer(xp, c_prompt, h0p, conv0p, *lw)
        xs, hsn, csn, vsn = hybrid_layer(xs, c_sample, state_rglru_h[l], state_conv[l], *lw)
        hp_list.append(hp)
        cp_list.append(cp)
        hs_list.append(hsn)
        cs_list.append(csn)
        vs_list.append(vsn)
    new_h_prompt = jnp.stack(hp_list)
    new_conv_prompt = jnp.stack(cp_list)
    new_h_sample = jnp.stack(hs_list)
    new_conv_sample = jnp.stack(cs_list)
    new_chunk_v_sample = jnp.stack(vs_list)
    return (xp, xs, new_h_prompt, new_conv_prompt, new_h_sample, new_conv_sample, new_chunk_v_sample)
```

```python
from contextlib import ExitStack
import numpy as np
import concourse.bass as bass
import concourse.mybir as mybir
from concourse.bass_utils import run_bass_kernel_spmd

F32 = mybir.dt.float32
BF16 = mybir.dt.bfloat16
I32 = mybir.dt.int32
AF = mybir.ActivationFunctionType
ALU = mybir.AluOpType
AX = mybir.AxisListType

D = 1024
NCORE = 8
SEQ = 2048
NPT = SEQ // 128
NTILE = NPT + 1
NSS = 16
LS = 8
NE = 32
ALPHA = 2.0 ** 0.25
EPS = 1e-5
KRING = 8
NSLOT = 48
NROWS = 2 * NTILE * 128 + 256


class Buf:
    def __init__(self, name):
        self.name = name
        self.w = None
        self.r = {}


class Q:
    def __init__(self, name, h, stream, sems, is_dma, is_pe=False):
        self.name, self.h, self.stream, self.sems = name, h, stream, sems
        self.is_dma, self.is_pe = is_dma, is_pe
        self.n = 0


class Trk:
    def __init__(self):
        self.seen = {}

    def _wait(self, q, tok):
        sem, val, src = tok
        key = (q.stream, sem.num)
        if self.seen.get(key, 0) >= val:
            return
        q.h.wait_ge(sem, val)
        self.seen[key] = val

    def op(self, q, reads, writes, emit, fin=True):
        toks = []
        for b in reads:
            if b.w is not None:
                toks.append(b.w)
        for b in writes:
            if b.w is not None:
                toks.append(b.w)
            toks.extend(b.r.values())
        for tok in toks:
            if tok[2] is q and q.is_pe:
                continue
            self._wait(q, tok)
        if q.is_dma:
            KR = len(q.sems)
            i = q.n
            sem = q.sems[i % KR]
            val = 16 * (i // KR + 1)
            if i >= KR:
                self._wait(q, (sem, val - 16, q))
            ins = emit()
            ins.then_inc(sem, 16)
            q.n += 1
            tok = (sem, val, q)
        else:
            ins = emit()
            if fin:
                q.n += 1
                ins.then_inc(q.sems[0], 1)
                tok = (q.sems[0], q.n, q)
            else:
                tok = (q.sems[0], q.n + 1, q)
        for b in reads:
            k = (tok[0].num)
            if b.r.get(k, (None, 0, None))[1] < tok[1]:
                b.r[k] = tok
        for b in writes:
            b.w = tok
            b.r = {}
        return ins


def dram_bc(ap, off, nrep, n):
    return bass.AP(ap.tensor, ap.offset + off, [[0, nrep], [1, n]])


def with_last(ap, step, n):
    return bass.AP(ap.tensor, ap.offset, [list(x) for x in ap.ap[:-1]] + [[step, n]])


def build_nc(debug=False):
    nc = bass.Bass("TRN2", target_bir_lowering=False)

    def din(n, s, dt=F32):
        return nc.dram_tensor(n, s, dt, kind="ExternalInput").ap()

    def dout(n, s, dt=F32):
        return nc.dram_tensor(n, s, dt, kind="ExternalOutput").ap()

    def dscr(n, s, dt=F32):
        return nc.dram_tensor(n, s, dt, kind="Internal").ap()

    xp = din("xp", [SEQ, D]); xs = din("xs", [128, D])
    sth = din("sth", [NSS, 512]); stc = din("stc", [NSS * 3, 512])
    cpd = din("cp", [1, D]); csd = din("cs", [NSS, D])
    w_ada = din("w_ada", [D, 6 * D]); b_ada = din("b_ada", [1, 6 * D])
    w_in = din("w_in", [D, 2048]); w_s = din("w_s", [4, 128, 128]); b_s = din("b_s", [4, 128])
    lnv_g = din("lnv_g", [1, 512]); lnv_b = din("lnv_b", [1, 512])
    conv_w = din("conv_w", [16, 128]); conv_b = din("conv_b", [4, 128])
    lru_wa = din("lru_wa", [8, 64, 64]); lru_ba = din("lru_ba", [4, 128])
    lru_wx = din("lru_wx", [8, 64, 64]); lru_bx = din("lru_bx", [4, 128])
    lru_lam = din("lru_lam", [4, 128])
    w_out = din("w_out", [D, D]); ln1_g = din("ln1_g", [1, D]); ln1_b = din("ln1_b", [1, D])
    w_rg = din("w_rg", [D, 4]); b_rg = din("b_rg", [1, 4]); w_re = din("w_re", [D, 32]); b_re = din("b_re", [1, 32])
    w1 = din("w1", [NE * 128, 4096]); w3 = din("w3", [NE * 128, 4096]); w2 = din("w2", [NE * 128, 4096])
    cst = din("cst", [128, 160]); cmat = din("cmat", [128, 256])
    ln2_g = din("ln2_g", [1, D]); ln2_b = din("ln2_b", [1, D])

    yp = dout("yp", [SEQ, D]); ys = dout("ys", [128, D])
    hp_o = dout("hp", [1, 512]); cp_o = dout("cpo", [3, 512])
    hs_o = dout("hs", [NSS, 512]); cs_o = dout("cso", [NSS * 3, 512]); vs_o = dout("vs", [128, 512])

    mods_d = dscr("mods_d", [128, 5 * D])
    x1_d = dscr("x1_d", [NTILE, 128, D])
    h2tok_d = dscr("h2tok_d", [NTILE, 128, D], BF16)
    g2_d = dscr("g2_d", [128, 2, D])
    xs_d = (dout if debug else dscr)("xs_d", [NROWS, D], BF16)
    y_d = (dout if debug else dscr)("y_d", [NROWS, D])
    if debug:
        dbg_pos = dout("dbg_pos", [128, 2 * NTILE], I32); dbg_iw = dout("dbg_iw", [128, NSLOT], I32)
        dbg_ix = dout("dbg_ix", [128, NSLOT], I32); dbg_gw = dout("dbg_gw", [128, 2 * NTILE])

    T = Trk()
    es_all = ExitStack()
    with es_all:
        nsem = [0]

        def S():
            nsem[0] += 1
            return es_all.enter_context(nc.semaphore(f"s{nsem[0]}"))

        pe = Q("pe", nc.tensor, "pe", [S()], False, True)
        act = Q("act", nc.scalar, "act", [S()], False)
        dve = Q("dve", nc.vector, "dve", [S()], False)
        pool = Q("pool", nc.gpsimd, "pool", [S()], False)
        spq = Q("spq", nc.sync, "sp", [S() for _ in range(KRING)], True)
        plq = Q("plq", nc.gpsimd, "pool", [S() for _ in range(32)], True)

        def barrier():
            qs = [pe, act, dve, pool, spq, plq]
            streams = {"pe": pe, "act": act, "dve": dve, "pool": pool, "sp": spq}
            for sname, qq in streams.items():
                for o in qs:
                    if o.is_dma:
                        KR = len(o.sems)
                        for j in range(min(o.n, KR)):
                            last = o.n - 1 - j
                            T._wait(qq, (o.sems[last % KR], 16 * (last // KR + 1), o))
                    elif o.n > 0 and o.stream != sname:
                        T._wait(qq, (o.sems[0], o.n, o))

        def emit_skewed(bodies, nset, serial_tail=0, n_umax=None):
            all_units = []
            for body in bodies:
                cur = []

                def D_(q, reads, writes, emit, fin=True, cur=cur):
                    cur.append((q, reads, writes, emit, fin))

                body(D_)
                units, g = [], []
                for opx in cur:
                    g.append(opx)
                    if opx[4]:
                        units.append(g)
                        g = []
                assert not g
                all_units.append(units)
            nb_ = len(bodies) - serial_tail
            umax = max(len(u) for u in all_units[:(n_umax or nb_)])
            stag = umax / nset
            sched = []
            for t, units in enumerate(all_units):
                base = t * stag if t < nb_ else (nb_ - 1) * stag + umax + (t - nb_) * umax
                for u, unit in enumerate(units):
                    sched.append((base + u, t, u, unit))
            sched.sort(key=lambda z: (z[0], z[1], z[2]))
            for _, _, _, unit in sched:
                for (q, r_, w_, e_, f_) in unit:
                    T.op(q, r_, w_, e_, fin=f_)

        pstk = ExitStack()
        es_all.enter_context(pstk)

        def sbt(stk, name, shape, dt=F32):
            t = stk.enter_context(nc.sbuf_tensor(name, shape, dt))
            return t, Buf(name)

        banks = []
        for i in range(8):
            t = pstk.enter_context(nc.psum_tensor(f"bank{i}", [128, 512], F32))
            banks.append((t, Buf(f"bank{i}")))
        bank_i = [0]

        def nbank():
            b = banks[bank_i[0] % 8]
            bank_i[0] += 1
            return b

        ident, identB = sbt(pstk, "ident", [128, 128])
        dmy, dmyB = sbt(pstk, "dmy", [128, 4])
        identb, identbB = sbt(pstk, "identb", [128, 128], BF16)
        gw, gwB = sbt(pstk, "gw", [128, 2, NTILE])
        pos_i, posB = sbt(pstk, "pos_i", [128, 2, NTILE], I32)
        idx_w, idxB = sbt(pstk, "idx_w", [128, NSLOT], I32)
        idx_x, _ = sbt(pstk, "idx_x", [128, NSLOT], I32)
        idx_y, _ = sbt(pstk, "idx_y", [128, 2, NSLOT], I32)

        T.op(pool, [], [identB], lambda: nc.gpsimd.memset(ident[:], 0.0))
        T.op(pool, [], [identB], lambda: nc.gpsimd.affine_select(
            out=ident[:], in_=ident[:], compare_op=ALU.not_equal, fill=1.0, base=0,
            pattern=[[-1, 128]], channel_multiplier=1))
        T.op(dve, [identB], [identbB], lambda: nc.vector.tensor_copy(identb[:], ident[:]))

        s1 = ExitStack()
        with s1:
            mod, modB = sbt(s1, "mod", [128, 5 * D])
            modBs = [Buf(f"mod{i}") for i in range(5)]
            winb, winB = sbt(s1, "winb", [128, 8, 2048], BF16)
            woutb, woutB = sbt(s1, "woutb", [128, 8, D], BF16)
            wrb, wrB = sbt(s1, "wrb", [128, 8, 36], BF16)
            rbias, rbiasB = sbt(s1, "rbias", [128, 36])
            wsT, wsTB = sbt(s1, "wsT", [128, 4, 128], BF16)
            wsTs, wsTsB = sbt(s1, "wsTs", [128, 4, 128], BF16)
            bsb, bsbB = sbt(s1, "bsb", [128, 4, 128])
            bsbs, bsbsB = sbt(s1, "bsbs", [128, 4, 128])
            wab, waB = sbt(s1, "wab", [128, 4, 128], BF16)
            wxb, wxB = sbt(s1, "wxb", [128, 4, 128], BF16)
            lnvg, lnvgB = sbt(s1, "lnvg", [128, 512]); lnvb, lnvbB = sbt(s1, "lnvb", [128, 512])
            l1g, l1gB = sbt(s1, "l1g", [128, D]); l1b, l1bB = sbt(s1, "l1b", [128, D])
            vt, vtB = sbt(s1, "vt", [128, 36])
            ca, caB = sbt(s1, "ca", [128, 8])
            h0T, h0TB = sbt(s1, "h0T", [128, 4, NSS])
            hcT, hcTB = sbt(s1, "hcT", [128, 4, NSS * 3])

            T.op(plq, [], [winB], lambda: nc.gpsimd.dma_start(
                out=winb[:], in_=w_in.rearrange("(k p) n -> p k n", p=128)))
            def sp_load(dst_ap, dstB, src_ap):
                T.op(spq, [], [dstB], lambda: nc.sync.dma_start(out=dst_ap, in_=src_ap))

            sp_load(lnvg[:], lnvgB, dram_bc(lnv_g, 0, 128, 512))
            sp_load(lnvb[:], lnvbB, dram_bc(lnv_b, 0, 128, 512))
            sp_load(l1g[:], l1gB, dram_bc(ln1_g, 0, 128, D))
            sp_load(l1b[:], l1bB, dram_bc(ln1_b, 0, 128, D))
            sp_load(bsb[:].rearrange("p a b -> p (a b)"), bsbB, dram_bc(b_s, 0, 128, 512))
            sp_load(rbias[:, 0:4], rbiasB, dram_bc(b_rg, 0, 128, 4))
            sp_load(rbias[:, 4:36], rbiasB, dram_bc(b_re, 0, 128, 32))

            s1a = ExitStack()
            s1a.__enter__()
            tmpA, tmpAB = sbt(s1a, "tmpA", [128, 4, 128])

            T.op(spq, [], [tmpAB], lambda: nc.sync.dma_start(
                out=tmpA[:, :, 0:8], in_=bass.AP(b_s.tensor, b_s.offset, [[0, 128], [128, 4], [1, 8]])))
            src = tmpA[:, :, 0:8]
            src4 = bass.AP(src.tensor, src.offset, [list(src.ap[0]), list(src.ap[1]), [0, NSS], [1, 8]])
            T.op(dve, [tmpAB], [bsbsB], lambda: nc.vector.tensor_copy(
                bsbs[:].rearrange("p a (s j) -> p a s j", j=8), src4))

            wsl, wslB = sbt(s1a, "wsl", [128, 4, 128])
            wss, wssB = sbt(s1a, "wss", [128, 4, 128])
            T.op(spq, [], [wslB], lambda: nc.sync.dma_start(out=wsl[:], in_=w_s.rearrange("h i j -> i h j")))
            T.op(pool, [], [wssB], lambda: nc.gpsimd.memset(wss[:], 0.0))
            for hd in range(4):
                for s in range(NSS):
                    T.op(spq, [wssB], [], lambda hd=hd, s=s: nc.sync.dma_start(
                        out=wss[8 * s:8 * s + 8, hd, 8 * s:8 * s + 8], in_=w_s[hd, 0:8, 0:8]))
            for (srcT, srcB, dstT, dstB) in ((wsl, wslB, wsT, wsTB), (wss, wssB, wsTs, wsTsB)):
                for hd in range(4):
                    T.op(pool, [srcB], [srcB], lambda srcT=srcT, hd=hd: nc.gpsimd.affine_select(
                        out=srcT[:, hd, :], in_=srcT[:, hd, :], compare_op=ALU.is_ge, fill=0.0, base=0,
                        pattern=[[-1, 128]], channel_multiplier=1))
                bk, bkB = nbank()
                for hd in range(4):
                    T.op(pe, [srcB, identB], [bkB], lambda srcT=srcT, hd=hd, bk=bk: nc.tensor.transpose(
                        bk[:, hd * 128:(hd + 1) * 128], srcT[:, hd, :], ident[:]), fin=(hd == 3))
                T.op(act, [bkB], [dstB], lambda dstT=dstT, bk=bk: nc.scalar.copy(
                    dstT[:].rearrange("p a b -> p (a b)"), bk[:]))

            vl, vlB = sbt(s1a, "vl", [36, 128])
            sp_load(vl[0:16, :], vlB, conv_w)
            sp_load(vl[16:20, :], vlB, conv_b)
            sp_load(vl[20:24, :], vlB, lru_ba)
            sp_load(vl[24:28, :], vlB, lru_bx)
            sp_load(vl[28:32, :], vlB, lru_lam)
            T.op(pool, [], [vlB], lambda: nc.gpsimd.memset(vl[32:36, :], 0.0))
            bk, bkB = nbank()
            T.op(pe, [vlB, identB], [bkB], lambda: nc.tensor.transpose(bk[:, 0:36], vl[:, :], ident[0:36, 0:36]))
            T.op(act, [bkB], [vtB], lambda: nc.scalar.copy(vt[:], bk[:, 0:36]))
            T.op(act, [vtB], [caB], lambda: nc.scalar.activation(out=ca[:, 0:4], in_=vt[:, 28:32], func=AF.Exp, scale=-1.0))
            T.op(act, [caB], [caB], lambda: nc.scalar.activation(out=ca[:, 0:4], in_=ca[:, 0:4], func=AF.Ln, bias=1.0, scale=1.0))
            T.op(dve, [caB], [caB], lambda: nc.vector.tensor_scalar(ca[:, 4:8], ca[:, 0:4], -16.0, None, op0=ALU.mult))
            T.op(dve, [caB], [caB], lambda: nc.vector.tensor_scalar(ca[:, 0:4], ca[:, 0:4], -8.0, None, op0=ALU.mult))

            stl, stlB = sbt(s1a, "stl", [64, 512])
            sp_load(stl[0:16, :], stlB, sth)
            sp_load(stl[16:64, :], stlB, stc)
            bk, bkB = nbank()
            for c in range(4):
                T.op(pe, [stlB, identB], [bkB], lambda c=c, bk=bk: nc.tensor.transpose(
                    bk[:, c * 64:(c + 1) * 64], stl[:, c * 128:(c + 1) * 128], ident[0:64, 0:64]), fin=(c == 3))
            bkv = bk[:, 0:256].rearrange("p (c r) -> p c r", r=64)
            T.op(act, [bkB], [h0TB], lambda: nc.scalar.copy(h0T[:], bkv[:, :, 0:16]))
            T.op(act, [bkB], [hcTB], lambda: nc.scalar.copy(hcT[:], bkv[:, :, 16:64]))

            for (dstT, dstB, srcw) in ((wab, waB, lru_wa), (wxb, wxB, lru_wx)):
                T.op(pool, [], [dstB], lambda dstT=dstT: nc.gpsimd.memset(dstT[:], 0.0))
                for c in range(4):
                    for hh in range(2):
                        T.op(plq, [dstB], [], lambda dstT=dstT, srcw=srcw, c=c, hh=hh: nc.gpsimd.dma_start(
                            out=dstT[64 * hh:64 * hh + 64, c, 64 * hh:64 * hh + 64], in_=srcw[2 * c + hh]))
                T.op(pool, [], [dstB, dmyB], lambda: nc.gpsimd.memset(dmy[:, 0:1], 0.0))
            with nc.allow_non_contiguous_dma(reason="tiny router weight load"):
                T.op(plq, [], [wrB], lambda: nc.gpsimd.dma_start(
                    out=wrb[:, :, 0:4], in_=w_rg.rearrange("(k p) n -> p k n", p=128)))
                T.op(plq, [], [wrB], lambda: nc.gpsimd.dma_start(
                    out=wrb[:, :, 4:36], in_=w_re.rearrange("(k p) n -> p k n", p=128)))

            ctl, ctlB = sbt(s1a, "ctl", [128, 2, D])
            sp_load(ctl[:, 0, :], ctlB, dram_bc(cpd, 0, 128, D))
            for s in range(NSS):
                T.op(spq, [ctlB], [], lambda s=s: nc.sync.dma_start(out=ctl[8 * s:8 * s + 8, 1, :], in_=dram_bc(csd, s * D, 8, D)))
            T.op(act, [identB], [ctlB, dmyB], lambda: nc.scalar.copy(dmy[:, 1:2], ident[:, 0:1]))
            ctb, ctbB = sbt(s1a, "ctb", [128, 2, D], BF16)
            T.op(act, [ctlB], [ctbB], lambda: nc.scalar.activation(out=ctb[:], in_=ctl[:], func=AF.Silu))
            cT, cTB = sbt(s1a, "cT", [128, 2, 8, 128], BF16)
            for g in range(2):
                bk, bkB = nbank()
                bkb = bk[:].bitcast(BF16)
                for k in range(8):
                    T.op(pe, [ctbB, identbB], [bkB], lambda g=g, k=k, bkb=bkb: nc.tensor.transpose(
                        bkb[:, k * 128:(k + 1) * 128], ctb[:, g, k * 128:(k + 1) * 128], identb[:]), fin=(k == 7))
                T.op(act, [bkB], [cTB], lambda g=g, bkb=bkb: nc.scalar.copy(
                    cT[:, g, :, :].rearrange("p k t -> p (k t)"), bkb[:, 0:1024]))

            g2t, g2B = sbt(s1a, "g2t", [128, 2, D])
            g2dB = Buf("g2_d")
            wad = [sbt(s1a, f"wad{i}", [128, 8, 512], BF16) for i in range(3)]
            bab = [sbt(s1a, f"bab{i}", [128, 512]) for i in range(2)]
            mst = [sbt(s1a, f"mst{i}", [128, 512]) for i in range(2)]
            modsB = Buf("mods_d")
            for n in range(12):
                wt, wB = wad[n % 3]
                T.op(plq, [], [wB], lambda wt=wt, n=n: nc.gpsimd.dma_start(
                    out=wt[:], in_=w_ada[:, n * 512:(n + 1) * 512].rearrange("(k p) n -> p k n", p=128)))
                bt, bB = bab[n % 2]
                sp_load(bt[:], bB, dram_bc(b_ada, n * 512, 128, 512))
                plus1 = 1.0 if (n // 2) in (1, 2, 4, 5) else 0.0
                for g in range(2):
                    bk, bkB = nbank()
                    for k in range(8):
                        T.op(pe, [cTB, wB], [bkB], lambda g=g, k=k, bk=bk, wt=wt: nc.tensor.matmul(
                            bk[:], cT[:, g, k, :], wt[:, k, :], start=(k == 0), stop=(k == 7)), fin=(k == 7))
                    if n >= 10:
                        T.op(dve, [bkB, bB], [g2B], lambda bk=bk, bt=bt, n=n, g=g, plus1=plus1: nc.vector.scalar_tensor_tensor(
                            out=g2t[:, g, (n - 10) * 512:(n - 9) * 512], in0=bk[:], scalar=plus1, in1=bt[:], op0=ALU.add, op1=ALU.add))
                    elif g == 0:
                        T.op(dve, [bkB, bB], [modBs[n // 2]], lambda bk=bk, bt=bt, n=n, plus1=plus1: nc.vector.scalar_tensor_tensor(
                            out=mod[:, n * 512:(n + 1) * 512], in0=bk[:], scalar=plus1, in1=bt[:], op0=ALU.add, op1=ALU.add))
                    else:
                        mt, mB = mst[n % 2]
                        T.op(dve, [bkB, bB], [mB], lambda bk=bk, bt=bt, mt=mt, plus1=plus1: nc.vector.scalar_tensor_tensor(
                            out=mt[:], in0=bk[:], scalar=plus1, in1=bt[:], op0=ALU.add, op1=ALU.add))
                        T.op(spq, [mB], [modsB], lambda mt=mt, n=n: nc.sync.dma_start(
                            out=mods_d[:, n * 512:(n + 1) * 512], in_=mt[:]))
            T.op(plq, [], [woutB], lambda: nc.gpsimd.dma_start(
                out=woutb[:], in_=w_out.rearrange("(k p) n -> p k n", p=128)))
            T.op(spq, [g2B], [g2dB], lambda: nc.sync.dma_start(out=g2_d, in_=g2t[:]))

            barrier()
            s1a.__exit__(None, None, None)
            NSET = 3
            RING = 4

            def mkset(i):
                d = {}
                for (nm, shp, dt) in (("xt", [128, D], F32), ("hb", [128, D], BF16), ("hT", [128, 8, 128], BF16),
                                      ("uT", [128, 4, 128], F32), ("gyT", [128, 4, 128], F32), ("xc", [128, 4, 128], F32),
                                      ("xcb", [128, 4, 128], BF16), ("At", [128, 4, 128], F32), ("Gx", [128, 4, 128], F32),
                                      ("Mt", [128, 4, 128], F32), ("mixT", [128, 8, 128], BF16),
                                      ("vnb", [128, 512], BF16), ("st6", [128, 2, 6], F32), ("mv", [128, 4], F32),
                                      ("zt", [128, D], F32),
                                      ):
                    d[nm] = sbt(s1b, f"{nm}_{i}", shp, dt)
                return d

            rla, rlaB = sbt(s1, "rla", [128, NTILE, 36])
            s1b = ExitStack()
            s1b.__enter__()
            sets = [mkset(i) for i in range(NSET)]
            XR = [sbt(s1b, f"xr{i}", [128, 4, NSS * (3 + LS)]) for i in range(RING)]
            HS = [sbt(s1b, f"hs{i}", [128, 4, 128]) for i in range(RING)]
            xrtok, xrtokB = sbt(s1b, "xrtok", [128, 512])
            hl, hlB = sbt(s1b, "hl", [128, 4, NSS])
            hrow, hrowB = xrtok, xrtokB
            tC, tCB = sbt(s1b, "tC", [128, 4, NSS])
            x1dBs = [Buf(f"x1_d{i}") for i in range(NTILE)]; h2tokBs = [Buf(f"h2tok_d{i}") for i in range(NTILE)]
            outB = Buf("outs")

            mod_reads = [0] * 5

            def tile_body(it, ti, D_):
                samp = (ti == NPT)
                sx = it % NSET
                S_ = sets[sx]
                xtile, xB = S_["xt"]; hb, hbB = S_["hb"]; hT, hTB = S_["hT"]; uT, uTB = S_["uT"]; gyT, gyTB = S_["gyT"]
                xc, xcB = S_["xc"]; xcb, xcbB = S_["xcb"]; At, AB = S_["At"]; Gx, GxB = S_["Gx"]; Mt, MB = S_["Mt"]
                mixT, mixTB = S_["mixT"]; vnb, vnbB = S_["vnb"]; st6, st6B = S_["st6"]; mv, mvB = S_["mv"]
                zt, ztB = S_["zt"]; h2b, h2bB = hb, hbB; h2T, h2TB = hT, hTB
                vn = Mt[:].rearrange("p c t -> p (c t)"); vnB = MB
                x1t, x1B = zt, ztB
                bi = [0]

                def nb():
                    nbk = (3, 3, 2)[sx]
                    b = banks[(0, 3, 6)[sx] + bi[0] % nbk]
                    bi[0] += 1
                    return b

                def layer_norm(src_ap, srcB, Dn, gT, gB, bT, bB, dst_ap, dstB):
                    nch = Dn // 512
                    src_full = src_ap[:, 0:Dn]
                    for i in range(nch):
                        D_(dve, [srcB], [st6B], lambda i=i: nc.vector.bn_stats(st6[:, i, :], src_ap[:, i * 512:(i + 1) * 512]))
                    D_(dve, [st6B], [mvB], lambda: nc.vector.bn_aggr(mv[:, 0:2], st6[:, 0:nch, :].rearrange("p a b -> p (a b)")))
                    D_(act, [mvB], [mvB], lambda: nc.scalar.activation(out=mv[:, 3:4], in_=mv[:, 1:2], func=AF.Sqrt, bias=EPS, scale=1.0))
                    D_(dve, [mvB], [mvB], lambda: nc.vector.reciprocal(mv[:, 2:3], mv[:, 3:4]))
                    D_(dve, [srcB, mvB], [dstB], lambda: nc.vector.tensor_scalar(
                        dst_ap, src_full, mv[:, 0:1], mv[:, 2:3], op0=ALU.subtract, op1=ALU.mult))
                    if Dn == D:
                        D_(pool, [dstB, gB], [dstB], lambda: nc.gpsimd.tensor_tensor(dst_ap, dst_ap, gT[:, 0:Dn], op=ALU.mult))
                        D_(pool, [dstB, bB], [dstB], lambda: nc.gpsimd.tensor_tensor(dst_ap, dst_ap, bT[:, 0:Dn], op=ALU.add))
                    else:
                        D_(dve, [dstB, gB], [dstB], lambda: nc.vector.tensor_tensor(dst_ap, dst_ap, gT[:, 0:Dn], op=ALU.mult))
                        D_(dve, [dstB, bB], [dstB], lambda: nc.vector.tensor_tensor(dst_ap, dst_ap, bT[:, 0:Dn], op=ALU.add))

                def cnt_read(i, ins):
                    if not samp:
                        mod_reads[i] += 1
                    return ins

                def mod_chunk(i):
                    if samp:
                        def reload():
                            assert mod_reads[i] == NPT * (2 if i == 2 else 1), (i, mod_reads[i])
                            return nc.sync.dma_start(out=mod[:, i * D:(i + 1) * D], in_=mods_d[:, i * D:(i + 1) * D])
                        D_(spq, [modsB], [modBs[i]], reload)
                    return modBs[i]

                nseq, L = (NSS, LS) if samp else (1, 128)
                W = 3 + L
                xsrc = xs if samp else xp[ti * 128:(ti + 1) * 128, :]
                D_(spq, [], [xB], lambda: nc.sync.dma_start(out=xtile[:], in_=xsrc))
                m1B = mod_chunk(1)
                m0B = mod_chunk(0)
                D_(dve, [xB, m1B], [ztB], lambda: cnt_read(1, nc.vector.tensor_tensor(zt[:], xtile[:], mod[:, D:2 * D], op=ALU.mult)))
                D_(pool, [ztB, m0B], [hbB], lambda: cnt_read(0, nc.gpsimd.tensor_tensor(hb[:], zt[:], mod[:, 0:D], op=ALU.add)))
                bk, bkB = nb()
                bkb = bk[:].bitcast(BF16)
                for k in range(8):
                    D_(pe, [hbB, identbB], [bkB], lambda k=k, bkb=bkb: nc.tensor.transpose(
                        bkb[:, k * 128:(k + 1) * 128], hb[:, k * 128:(k + 1) * 128], identb[:]), fin=(k == 7))
                D_(act, [bkB], [hTB], lambda bkb=bkb: nc.scalar.copy(hT[:].rearrange("p k t -> p (k t)"), bkb[:, 0:1024]))

                xr, xrB = XR[it % RING]
                xrp, xrpB = XR[(it - 1) % RING]
                xr4 = xr[:, :, 0:nseq * W].rearrange("p c (s w) -> p c s w", w=W)
                pbanks = []
                for gi, col0 in enumerate((0, 1024, 1536)):
                    bk, bkB = nb()
                    for c in range(4):
                        for k in range(8):
                            D_(pe, [winB, hTB], [bkB], lambda c=c, k=k, bk=bk, col0=col0: nc.tensor.matmul(
                                bk[:, c * 128:(c + 1) * 128], winb[:, k, col0 + c * 128:col0 + (c + 1) * 128], hT[:, k, :],
                                start=(k == 0), stop=(k == 7)), fin=(c == 3 and k == 7))
                    if gi == 0:
                        D_(act, [bkB], [uTB], lambda b=bk: nc.scalar.copy(uT[:].rearrange("p c t -> p (c t)"), b[:]))
                    elif gi == 1:
                        D_(act, [bkB], [xrB], lambda b=bk: nc.scalar.copy(
                            xr4[:, :, :, 3:W], b[:].rearrange("p (c s l) -> p c s l", c=4, s=nseq)))
                    else:
                        D_(act, [bkB], [gyTB], lambda b=bk: nc.scalar.activation(
                            out=gyT[:].rearrange("p c t -> p (c t)"), in_=b[:], func=AF.Gelu_apprx_tanh))
                if samp:
                    D_(dve, [hcTB], [xrB], lambda: nc.vector.tensor_copy(
                        xr4[:, :, :, 0:3], hcT[:].rearrange("p c (s r) -> p c s r", r=3)))
                elif ti == 0:
                    D_(dve, [], [xrB], lambda: nc.vector.memset(xr[:, :, 0:3], 0.0))
                else:
                    D_(dve, [xrpB], [xrB], lambda: nc.vector.tensor_copy(xr[:, :, 0:3], xrp[:, :, 128:131]))

                bkv_, bkvB = nb()
                for k in range(8):
                    D_(pe, [winB, hTB], [bkvB], lambda k=k, bkv_=bkv_: nc.tensor.matmul(
                        bkv_[:], hT[:, k, :], winb[:, k, 512:1024], start=(k == 0), stop=(k == 7)), fin=(k == 7))
                layer_norm(bkv_, bkvB, 512, lnvg, lnvgB, lnvb, lnvbB, vn, vnB)
                D_(act, [vnB], [vnbB], lambda: nc.scalar.copy(vnb[:], vn))
                if samp:
                    D_(spq, [vnB], [Buf("o")], lambda: nc.sync.dma_start(out=vs_o, in_=vn))
                if samp or ti == NPT - 1:
                    bkx, bkxB = nb()
                    for k in range(8):
                        D_(pe, [winB, hTB], [bkxB], lambda k=k, bkx=bkx: nc.tensor.matmul(
                            bkx[:], hT[:, k, :], winb[:, k, 1024:1536], start=(k == 0), stop=(k == 7)), fin=(k == 7))
                    D_(act, [bkxB], [xrtokB], lambda bkx=bkx: nc.scalar.copy(xrtok[:], bkx[:]))
                    if samp:
                        for s in range(NSS):
                            D_(spq, [xrtokB], [Buf("o")], lambda s=s: nc.sync.dma_start(
                                out=cs_o[3 * s:3 * s + 3, :], in_=xrtok[8 * s + 5:8 * s + 8, :]))
                    else:
                        D_(spq, [xrtokB], [Buf("o")], lambda: nc.sync.dma_start(out=cp_o, in_=xrtok[125:128, :]))

                wsX, wsXB = (wsTs, wsTsB) if samp else (wsT, wsTB)
                bsX, bsXB = (bsbs, bsbsB) if samp else (bsb, bsbB)
                bkg, bkgB = nb()
                for hd in range(4):
                    D_(pe, [vnbB, wsXB], [bkgB], lambda hd=hd, bkg=bkg, wsX=wsX: nc.tensor.matmul(
                        bkg[:, hd * 128:(hd + 1) * 128], vnb[:, hd * 128:(hd + 1) * 128], wsX[:, hd, :],
                        start=True, stop=True), fin=(hd == 3))
                D_(dve, [bkgB, bsXB], [GxB], lambda bkg=bkg, bsX=bsX: nc.vector.tensor_tensor(
                    Gx[:].rearrange("p a b -> p (a b)"), bkg[:], bsX[:].rearrange("p a b -> p (a b)"), op=ALU.add))
                D_(dve, [GxB, uTB], [mixTB], lambda: nc.vector.tensor_tensor(
                    mixT[:, 0:4, :], Gx[:], uT[:], op=ALU.mult))

                xc4 = xc[:].rearrange("p c (s l) -> p c s l", l=L)
                for c in range(4):
                    D_(dve, [xrB, vtB], [xcB], lambda c=c: nc.vector.tensor_scalar(
                        xc4[:, c, :, :], xr4[:, c, :, 0:L], vt[:, c:c + 1], vt[:, 16 + c:17 + c], op0=ALU.mult, op1=ALU.add))
                    for k in range(1, 4):
                        D_(dve, [xrB, vtB, xcB], [xcB], lambda c=c, k=k: nc.vector.scalar_tensor_tensor(
                            out=xc4[:, c, :, :], in0=xr4[:, c, :, k:k + L], scalar=vt[:, 4 * k + c:4 * k + c + 1],
                            in1=xc4[:, c, :, :], op0=ALU.mult, op1=ALU.add))
                D_(act, [xcB], [xcbB], lambda: nc.scalar.copy(xcb[:], xc[:]))
                bka, bkaB = nb()
                bkx2, bkx2B = nb()
                for c in range(4):
                    D_(pe, [xcbB, waB], [bkaB], lambda c=c, bka=bka: nc.tensor.matmul(
                        bka[:, c * 128:(c + 1) * 128], wab[:, c, :], xcb[:, c, :], start=True, stop=True), fin=(c == 3))
                for c in range(4):
                    D_(pe, [xcbB, wxB], [bkx2B], lambda c=c, bkx2=bkx2: nc.tensor.matmul(
                        bkx2[:, c * 128:(c + 1) * 128], wxb[:, c, :], xcb[:, c, :], start=True, stop=True), fin=(c == 3))
                for c in range(4):
                    D_(act, [bkaB, vtB], [AB], lambda c=c, bka=bka: nc.scalar.activation(
                        out=At[:, c, :], in_=bka[:, c * 128:(c + 1) * 128], func=AF.Sigmoid, bias=vt[:, 20 + c:21 + c], scale=1.0))
                    D_(act, [bkx2B, vtB], [GxB], lambda c=c, bkx2=bkx2: nc.scalar.activation(
                        out=Gx[:, c, :], in_=bkx2[:, c * 128:(c + 1) * 128], func=AF.Sigmoid, bias=vt[:, 24 + c:25 + c], scale=1.0))
                for c in range(4):
                    D_(act, [AB, caB], [MB], lambda c=c: nc.scalar.activation(
                        out=Mt[:, c, :], in_=At[:, c, :], func=AF.Exp, scale=ca[:, 4 + c:5 + c]))
                    D_(act, [AB, caB], [AB], lambda c=c: nc.scalar.activation(
                        out=At[:, c, :], in_=At[:, c, :], func=AF.Exp, scale=ca[:, c:c + 1]))
                D_(act, [MB], [MB], lambda: nc.scalar.activation(out=Mt[:], in_=Mt[:], func=AF.Sqrt, bias=1.0, scale=-1.0))
                D_(dve, [MB, GxB], [MB], lambda: nc.vector.tensor_tensor(Mt[:], Mt[:], Gx[:], op=ALU.mult))
                D_(dve, [MB, xcB], [MB], lambda: nc.vector.tensor_tensor(Mt[:], Mt[:], xc[:], op=ALU.mult))
                hs_, hsB = HS[it % RING]
                hsp, hspB = HS[(it - 1) % RING]
                if samp:
                    A4 = At[:].rearrange("p c (s l) -> p c s l", l=L)
                    M4 = Mt[:].rearrange("p c (s l) -> p c s l", l=L)
                    h04 = h0T[:].rearrange("p c (s o) -> p c s o", o=1)
                    t4 = tC[:].rearrange("p c (s o) -> p c s o", o=1)
                    D_(dve, [AB, h0TB], [tCB], lambda: nc.vector.tensor_tensor(t4, A4[:, :, :, 0:1], h04, op=ALU.mult))
                    D_(dve, [tCB, MB], [MB], lambda: nc.vector.tensor_tensor(M4[:, :, :, 0:1], M4[:, :, :, 0:1], t4, op=ALU.add))
                    D_(dve, [AB], [AB], lambda: nc.vector.memset(A4[:, :, :, 0:1], 0.0))
                for c in range(4):
                    init = 0.0 if (samp or ti == 0) else hsp[:, c, 127:128]
                    rds = [AB, MB] + ([] if (samp or ti == 0) else [hspB])
                    D_(dve, rds, [hsB], lambda c=c, init=init, hs_=hs_: nc.vector.tensor_tensor_scan(
                        out=hs_[:, c, :], data0=At[:, c, :], data1=Mt[:, c, :], initial=init, op0=ALU.mult, op1=ALU.add))
                D_(dve, [hsB, gyTB], [mixTB], lambda hs_=hs_: nc.vector.tensor_tensor(mixT[:, 4:8, :], hs_[:], gyT[:], op=ALU.mult))
                if samp or ti == NPT - 1:
                    ncol = NSS if samp else 1
                    if samp:
                        hs4 = hs_[:].rearrange("p c (s l) -> p c s l", l=L)
                        D_(dve, [hsB], [hlB], lambda hs4=hs4: nc.vector.tensor_copy(
                            hl[:].rearrange("p c (s o) -> p c s o", o=1), hs4[:, :, :, L - 1:L]))
                    else:
                        D_(dve, [hsB], [hlB], lambda hs_=hs_: nc.vector.tensor_copy(hl[:, :, 0:1], hs_[:, :, 127:128]))
                    bkh, bkhB = nb()
                    for c in range(4):
                        D_(pe, [hlB, identB], [bkhB], lambda c=c, bkh=bkh, ncol=ncol: nc.tensor.transpose(
                            bkh[0:ncol, c * 128:(c + 1) * 128], hl[:, c, 0:ncol], ident[:]), fin=(c == 3))
                    D_(act, [bkhB], [hrowB], lambda bkh=bkh, ncol=ncol: nc.scalar.copy(hrow[0:ncol, :], bkh[0:ncol, :]))
                    D_(spq, [hrowB], [Buf("o")], lambda ncol=ncol, samp=samp: nc.sync.dma_start(
                        out=(hs_o if samp else hp_o), in_=hrow[0:ncol, :]))

                m2B = mod_chunk(2)
                for cb in range(2):
                    bk, bkB = nb()
                    for k in range(8):
                        D_(pe, [mixTB, woutB], [bkB], lambda k=k, bk=bk, cb=cb: nc.tensor.matmul(
                            bk[:], mixT[:, k, :], woutb[:, k, cb * 512:(cb + 1) * 512], start=(k == 0), stop=(k == 7)), fin=(k == 7))
                    D_(dve, [bkB, m2B], [ztB], lambda bk=bk, cb=cb: cnt_read(2, nc.vector.tensor_tensor(
                        zt[:, cb * 512:(cb + 1) * 512], bk[:], mod[:, 2 * D + cb * 512:2 * D + (cb + 1) * 512], op=ALU.mult)))
                D_(dve, [xB, ztB], [ztB], lambda: nc.vector.scalar_tensor_tensor(
                    out=zt[:], in0=xtile[:], scalar=ALPHA, in1=zt[:], op0=ALU.mult, op1=ALU.add))
                layer_norm(zt, ztB, D, l1g, l1gB, l1b, l1bB, x1t[:], x1B)
                D_(spq, [x1B], [x1dBs[ti]], lambda: nc.sync.dma_start(out=x1_d[ti], in_=x1t[:]))
                m4B = mod_chunk(4)
                m3B = mod_chunk(3)
                D_(pool, [x1B, m4B], [xB], lambda: cnt_read(4, nc.gpsimd.tensor_tensor(xtile[:], x1t[:], mod[:, 4 * D:5 * D], op=ALU.mult)))
                D_(pool, [xB, m3B], [h2bB], lambda: cnt_read(3, nc.gpsimd.tensor_tensor(h2b[:], xtile[:], mod[:, 3 * D:4 * D], op=ALU.add)))
                D_(spq, [h2bB], [h2tokBs[ti]], lambda: nc.sync.dma_start(out=h2tok_d[ti], in_=h2b[:]))
                bk, bkB = nb()
                bkb = bk[:].bitcast(BF16)
                for k in range(8):
                    D_(pe, [h2bB, identbB], [bkB], lambda k=k, bkb=bkb: nc.tensor.transpose(
                        bkb[:, k * 128:(k + 1) * 128], h2b[:, k * 128:(k + 1) * 128], identb[:]), fin=(k == 7))
                D_(act, [bkB], [h2TB], lambda bkb=bkb: nc.scalar.copy(h2T[:].rearrange("p k t -> p (k t)"), bkb[:, 0:1024]))
                bkr, bkrB = nb()
                for k in range(8):
                    D_(pe, [h2TB, wrB], [bkrB], lambda k=k, bkr=bkr: nc.tensor.matmul(
                        bkr[:, 0:36], h2T[:, k, :], wrb[:, k, :], start=(k == 0), stop=(k == 7)), fin=(k == 7))
                D_(dve, [bkrB, rbiasB], [rlaB], lambda bkr=bkr: nc.vector.tensor_tensor(rla[:, ti, :], bkr[:, 0:36], rbias[:], op=ALU.add))

            order = list(range(NPT)) + [NPT]
            emit_skewed([lambda D_, it=it, ti=ti: tile_body(it, ti, D_) for it, ti in enumerate(order)], NSET, n_umax=NPT - 1)

            barrier()
            s1b.__exit__(None, None, None)
            V = nc.vector
            NT = NTILE
            gmax, rtB = sbt(s1, "gmax", [128, NT])
            ohg, _ = sbt(s1, "ohg", [128, NT, 4]); ex, _ = sbt(s1, "ex", [128, NT, 4])
            sumex, _ = sbt(s1, "sumex", [128, NT]); pg, _ = sbt(s1, "pg", [128, NT])
            lsel, _ = sbt(s1, "lsel", [128, NT, 8]); ltmp, _ = sbt(s1, "ltmp", [128, NT, 8])
            v1, _ = sbt(s1, "v1", [128, NT]); v2, _ = sbt(s1, "v2", [128, NT])
            oh1, _ = sbt(s1, "oh1", [128, NT, 8]); oh2, _ = sbt(s1, "oh2", [128, NT, 8]); msk, _ = sbt(s1, "msk", [128, NT, 8])
            wA, _ = sbt(s1, "wA", [128, NT]); wB_, _ = sbt(s1, "wB", [128, NT]); within, _ = sbt(s1, "within", [128, NT, 8])

            def bcl(ap2, n):
                a = [list(x) for x in ap2.ap]
                if len(a) == 3:
                    assert a[2][1] == 1
                    a = a[:2]
                assert len(a) == 2
                return bass.AP(ap2.tensor, ap2.offset, a + [[0, n]])

            def R(emit, q=dve, extra_r=(), extra_w=()):
                T.op(q, [rtB, rlaB] + list(extra_r), [rtB] + list(extra_w), emit)

            lg = rla[:, :, 0:4]
            R(lambda: V.tensor_reduce(gmax[:], lg, axis=AX.X, op=ALU.max))
            R(lambda: V.tensor_tensor(ohg[:], lg, bcl(gmax[:], 4), op=ALU.is_equal))
            R(lambda: V.tensor_tensor(ex[:], lg, bcl(gmax[:], 4), op=ALU.subtract))
            R(lambda: nc.scalar.activation(out=ex[:], in_=ex[:], func=AF.Exp), q=act)
            R(lambda: V.tensor_reduce(sumex[:], ex[:], axis=AX.X, op=ALU.add))
            R(lambda: V.reciprocal(pg[:], sumex[:]))
            for g in range(4):
                srcl = rla[:, :, 4 + 8 * g:12 + 8 * g]
                selb = bcl(ohg[:, :, g], 8)
                if g == 0:
                    R(lambda srcl=srcl, selb=selb: V.tensor_tensor(lsel[:], srcl, selb, op=ALU.mult))
                else:
                    R(lambda srcl=srcl, selb=selb: V.tensor_tensor(ltmp[:], srcl, selb, op=ALU.mult))
                    R(lambda: V.tensor_tensor(lsel[:], lsel[:], ltmp[:], op=ALU.add))
            R(lambda: V.tensor_reduce(v1[:], lsel[:], axis=AX.X, op=ALU.max))
            R(lambda: V.tensor_tensor(oh1[:], lsel[:], bcl(v1[:], 8), op=ALU.is_equal))
            R(lambda: V.scalar_tensor_tensor(out=msk[:], in0=oh1[:], scalar=-1e30, in1=lsel[:], op0=ALU.mult, op1=ALU.add))
            R(lambda: V.tensor_reduce(v2[:], msk[:], axis=AX.X, op=ALU.max))
            R(lambda: V.tensor_tensor(oh2[:], msk[:], bcl(v2[:], 8), op=ALU.is_equal))
            R(lambda: V.tensor_tensor(wA[:], v1[:], v2[:], op=ALU.subtract))
            R(lambda: nc.scalar.activation(out=wA[:], in_=wA[:], func=AF.Sigmoid), q=act)
            R(lambda: V.tensor_scalar(wB_[:], wA[:], -1.0, 1.0, op0=ALU.mult, op1=ALU.add))
            R(lambda: V.tensor_tensor(wA[:], wA[:], pg[:], op=ALU.mult))
            R(lambda: V.tensor_tensor(wB_[:], wB_[:], pg[:], op=ALU.mult))
            R(lambda: V.tensor_copy(gw[:, 0, :], wA[:]), extra_w=[gwB])
            R(lambda: V.tensor_copy(gw[:, 1, :], wB_[:]), extra_w=[gwB])

            def bcm(ap2, m):
                a = [list(x) for x in ap2.ap]
                assert len(a) == 2
                return bass.AP(ap2.tensor, ap2.offset, [a[0], [0, m], a[1]])

            M1, _ = sbt(s1, "M1", [128, NT, 32]); M2, _ = sbt(s1, "M2", [128, NT, 32])
            Mb, MbB = sbt(s1, "Mb", [128, NT, 32], BF16)
            cst_sb, cstB = sbt(s1, "cst_sb", [128, 160]); cm_f, cmfB = sbt(s1, "cm_f", [128, 256])
            cm_b, cmbB = sbt(s1, "cm_b", [128, 256], BF16)
            rank_sb, _ = sbt(s1, "rank_sb", [128, NT, 32]); tot_sb, _ = sbt(s1, "tot_sb", [128, 32])
            incl, _ = sbt(s1, "incl", [128, 32]); off, _ = sbt(s1, "off", [128, 32])
            posf, _ = sbt(s1, "posf", [128, 2, NT])
            cmpA, _ = sbt(s1, "cmpA", [128, 32, 16]); cmpB, _ = sbt(s1, "cmpB", [128, 16, 32])
            rko, _ = sbt(s1, "rko", [128, 32]); rka, _ = sbt(s1, "rka", [128, 16])
            big, _ = sbt(s1, "big", [128, NSLOT, 32]); big2, _ = sbt(s1, "big2", [128, NSLOT, 16])
            psf, _ = sbt(s1, "psf", [128, NSLOT]); psf2, _ = sbt(s1, "psf2", [128, NSLOT]); esf, _ = sbt(s1, "esf", [128, NSLOT])
            T.op(spq, [], [cstB], lambda: nc.sync.dma_start(out=cst_sb[:], in_=cst))
            T.op(spq, [], [cmfB], lambda: nc.sync.dma_start(out=cm_f[:], in_=cmat))
            T.op(dve, [cmfB], [cmbB], lambda: nc.vector.tensor_copy(cm_b[:], cm_f[:]))
            pidx = cst_sb[:, 0:1]; p2idx = cst_sb[:, 1:2]; a_j = cst_sb[:, 2:18]; eidx = cst_sb[:, 18:50]
            sidx = cst_sb[:, 50:50 + NSLOT]; jm1 = cst_sb[:, 98:114]; ones32 = cst_sb[:, 114:146]
            for g in range(4):
                R(lambda g=g: V.tensor_tensor(M1[:, :, 8 * g:8 * g + 8], oh1[:], bcl(ohg[:, :, g], 8), op=ALU.mult))
                R(lambda g=g: V.tensor_tensor(M2[:, :, 8 * g:8 * g + 8], oh2[:], bcl(ohg[:, :, g], 8), op=ALU.mult))
            R(lambda: V.tensor_tensor(Mb[:], M1[:], M2[:], op=ALU.add), extra_w=[MbB])
            rkA, rkAB = nbank(); rkB, rkBB = nbank()
            for ti in range(NT):
                bk, bkB_ = (rkA, rkAB) if ti < 16 else (rkB, rkBB)
                c0 = (ti % 16) * 32
                T.op(pe, [MbB, cmbB], [bkB_], lambda bk=bk, c0=c0, ti=ti: nc.tensor.matmul(
                    bk[:, c0:c0 + 32], cm_b[:, 0:128], Mb[:, ti, :], start=True, stop=(ti == 0)), fin=(ti == 0))
                for tj in range(ti):
                    T.op(pe, [MbB, cmbB], [bkB_], lambda bk=bk, c0=c0, tj=tj, ti=ti: nc.tensor.matmul(
                        bk[:, c0:c0 + 32], cm_b[:, 128:256], Mb[:, tj, :], start=False, stop=(tj == ti - 1)), fin=(tj == ti - 1))
            for tj in range(NT):
                T.op(pe, [MbB, cmbB], [rkBB], lambda tj=tj: nc.tensor.matmul(
                    rkB[:, 32:64], cm_b[:, 128:256], Mb[:, tj, :], start=(tj == 0), stop=(tj == NT - 1)), fin=(tj == NT - 1))
            R(lambda: nc.scalar.copy(rank_sb[:, 0:16, :].rearrange("p t e -> p (t e)"), rkA[:, 0:512]), q=act, extra_r=[rkAB])
            R(lambda: nc.scalar.copy(rank_sb[:, 16, :], rkB[:, 0:32]), q=act, extra_r=[rkBB])
            R(lambda: nc.scalar.copy(tot_sb[:], rkB[:, 32:64]), q=act, extra_r=[rkBB])
            R(lambda: V.tensor_tensor_scan(out=incl[:], data0=ones32, data1=tot_sb[:], initial=0.0, op0=ALU.mult, op1=ALU.add),
              extra_r=[cstB])
            R(lambda: V.tensor_tensor(off[:], incl[:], tot_sb[:], op=ALU.subtract))
            R(lambda: V.tensor_tensor(rank_sb[:], rank_sb[:], bcm(off[:], NT), op=ALU.add))
            R(lambda: V.tensor_tensor(M1[:], M1[:], rank_sb[:], op=ALU.mult))
            R(lambda: V.tensor_reduce(posf[:, 0, :], M1[:], axis=AX.X, op=ALU.add))
            R(lambda: V.tensor_tensor(M2[:], M2[:], rank_sb[:], op=ALU.mult))
            R(lambda: V.tensor_reduce(posf[:, 1, :], M2[:], axis=AX.X, op=ALU.add))
            R(lambda: V.tensor_copy(pos_i[:], posf[:]), extra_w=[posB])
            R(lambda: V.tensor_tensor(cmpA[:], bcl(off[:], 16), bcm(a_j, 32), op=ALU.is_ge))
            R(lambda: V.tensor_reduce(rko[:], cmpA[:], axis=AX.X, op=ALU.add))
            R(lambda: V.tensor_tensor(rko[:], rko[:], eidx, op=ALU.add))
            R(lambda: V.tensor_tensor(cmpB[:], bcl(a_j, 32), bcm(off[:], 16), op=ALU.is_gt))
            R(lambda: V.tensor_reduce(rka[:], cmpB[:], axis=AX.X, op=ALU.add))
            R(lambda: V.tensor_tensor(rka[:], rka[:], jm1, op=ALU.add))
            R(lambda: V.tensor_tensor(big[:], bcm(rko[:], NSLOT), bcl(sidx, 32), op=ALU.is_equal))
            R(lambda: V.tensor_tensor(big[:], big[:], bcm(off[:], NSLOT), op=ALU.mult))
            R(lambda: V.tensor_reduce(psf[:], big[:], axis=AX.X, op=ALU.add))
            R(lambda: V.tensor_tensor(big2[:], bcm(rka[:], NSLOT), bcl(sidx, 16), op=ALU.is_equal))
            R(lambda: V.tensor_tensor(big2[:], big2[:], bcm(a_j, NSLOT), op=ALU.mult))
            R(lambda: V.tensor_reduce(psf2[:], big2[:], axis=AX.X, op=ALU.add))
            R(lambda: V.tensor_tensor(psf[:], psf[:], psf2[:], op=ALU.add))
            R(lambda: V.tensor_tensor(big[:], bcm(rko[:], NSLOT), bcl(sidx, 32), op=ALU.is_le))
            R(lambda: V.tensor_reduce(esf[:], big[:], axis=AX.X, op=ALU.add))
            R(lambda: V.tensor_scalar(esf[:], esf[:], 128.0, -128.0, op0=ALU.mult, op1=ALU.add))
            R(lambda: V.tensor_scalar(esf[:], esf[:], pidx, None, op0=ALU.add))
            R(lambda: V.tensor_copy(idx_w[:], esf[:]), extra_w=[idxB])
            pend, _ = sbt(s1, "pend", [128, NSLOT]); rowf, _ = sbt(s1, "rowf", [128, NSLOT]); inval, _ = sbt(s1, "inval", [128, NSLOT])
            R(lambda: V.tensor_copy(pend[:, 0:NSLOT - 1], psf[:, 1:NSLOT]))
            R(lambda: V.memset(pend[:, NSLOT - 1:NSLOT], float(2 * NTILE * 128)))
            R(lambda: V.tensor_scalar(psf[:], psf[:], p2idx, None, op0=ALU.add))
            R(lambda: V.tensor_copy(idx_x[:], psf[:]), extra_w=[idxB])
            for sub in range(2):
                R(lambda sub=sub: V.tensor_scalar(rowf[:], psf[:], float(sub), None, op0=ALU.add))
                R(lambda: V.tensor_tensor(inval[:], rowf[:], pend[:], op=ALU.is_ge))
                R(lambda: V.scalar_tensor_tensor(out=rowf[:], in0=inval[:], scalar=1048576.0, in1=rowf[:], op0=ALU.mult, op1=ALU.add))
                R(lambda sub=sub: V.tensor_copy(idx_y[:, sub, :], rowf[:]), extra_w=[idxB])

            barrier()
        s2 = ExitStack()
        with s2:
            G = nc.gpsimd
            bc_x = G.to_reg(NROWS - 1)
            bc_w = G.to_reg(NE * 128 - 1)
            bc_x2 = G.to_reg(NROWS - 2)
            IOA = bass.IndirectOffsetOnAxis
            s2a = ExitStack()
            s2a.__enter__()
            h2all, h2allB = sbt(s2a, "h2all", [128, NTILE, D], BF16)
            T.op(spq, h2tokBs, [h2allB], lambda: nc.sync.dma_start(out=h2all[:], in_=h2tok_d.rearrange("t p n -> p t n")))
            zpad, zpadB = sbt(s2a, "zpad", [128, 2, D])
            T.op(pool, [], [zpadB], lambda: nc.gpsimd.memset(zpad[:], 0.0))
            NPAIR = 2 * NTILE * 128
            T.op(plq, [zpadB], [Buf("xs_pad")], lambda: nc.gpsimd.dma_start(
                out=xs_d[NPAIR:NROWS, :].rearrange("(p a) n -> p a n", a=2), in_=zpad[:]))
            T.op(spq, [zpadB], [Buf("y_pad")], lambda: nc.sync.dma_start(
                out=y_d[NPAIR:NROWS, :].rearrange("(p a) n -> p a n", a=2), in_=zpad[:]))
            scB = []
            for ti in range(NTILE):
                for k in range(2):
                    b_ = Buf(f"xs_{ti}_{k}")
                    scB.append(b_)
                    T.op(plq, [h2allB, posB], [b_], lambda ti=ti, k=k: G.indirect_dma_start(
                        out=xs_d[:, :], out_offset=IOA(ap=pos_i[:, k, ti:ti + 1], axis=0),
                        in_=h2all[:, ti, :], in_offset=None, bounds_check=bc_x, oob_is_err=False))
            barrier()
            s2a.__exit__(None, None, None)

            s2b = ExitStack()
            s2b.__enter__()
            NW = 3
            WB = [[sbt(s2b, f"w{n}b{i}", [128, 4096], BF16) for n in (1, 3, 2)] for i in range(NW)]
            XRb = [sbt(s2b, f"xrb{i}", [128, 2, D], BF16) for i in range(NW)]
            XRb2 = [Buf(f"xrb{i}_1") for i in range(NW)]
            for (xr0_, xr0B_), xr0B2_ in zip(XRb, XRb2):
                T.op(pool, [], [xr0B_, xr0B2_], lambda xr0_=xr0_: nc.gpsimd.memset(xr0_[:], 0.0))
            XT = [sbt(s2b, f"xT{i}", [128, 8, 256], BF16) for i in range(2)]
            hdn = [sbt(s2b, f"hdn{i}", [128, 4, 256], BF16) for i in range(2)]
            sil = [sbt(s2b, f"sil{i}", [128, 256]) for i in range(2)]
            NY = 4
            ysb = [sbt(s2b, f"ysb{i}", [128, 2, D]) for i in range(NY)]
            ydB = Buf("y_d")
            cT = [0]; cH = [0]; cO = [0]

            def bankT():
                b = banks[cT[0] % 2]; cT[0] += 1; return b

            def bankH():
                b = banks[2 + cH[0] % 4]; cH[0] += 1; return b

            def bankO():
                b = banks[6 + cO[0] % 2]; cO[0] += 1; return b

            def loads(s):
                xr_, xrB_ = XRb[s % NW]
                for sub in range(2):
                    T.op(plq, [idxB], [xrB_ if sub == 0 else XRb2[s % NW]], lambda sub=sub: G.indirect_dma_start(
                        out=xr_[:, sub, :], out_offset=None, in_=xs_d[:, :],
                        in_offset=IOA(ap=idx_y[:, sub, s:s + 1], axis=0), bounds_check=bc_x, oob_is_err=False))
                for (wt_, wB_), src in zip(WB[s % NW], (w1, w3, w2)):
                    T.op(plq, [idxB], [wB_], lambda wt_=wt_, src=src: G.indirect_dma_start(
                        out=wt_[:, :], out_offset=None, in_=src[:, :],
                        in_offset=IOA(ap=idx_w[:, s:s + 1], axis=0), bounds_check=bc_w, oob_is_err=False))

            def prep(s):
                xr_, xrB_ = XRb[s % NW]
                xT_, xTB_ = XT[s % 2]
                for sub in range(2):
                    bk, bkB = bankT()
                    bkb = bk[:].bitcast(BF16)
                    for k in range(8):
                        T.op(pe, [xrB_ if sub == 0 else XRb2[s % NW], identbB], [bkB], lambda k=k, bkb=bkb, sub=sub: nc.tensor.transpose(
                            bkb[:, k * 128:(k + 1) * 128], xr_[:, sub, bass.ds(k, 128, step=8)], identb[:]), fin=(k == 7))
                    dstv = xT_[:, :, sub * 128:(sub + 1) * 128]
                    srcv = bkb[:, 0:1024].rearrange("p (k t) -> p k t", k=8)
                    if sub == 0:
                        T.op(act, [bkB], [xTB_], lambda dstv=dstv, srcv=srcv: nc.scalar.copy(dstv, srcv))
                    else:
                        T.op(dve, [bkB], [xTB_], lambda dstv=dstv, srcv=srcv: nc.vector.tensor_copy(dstv, srcv))

            def hstage(s):
                (w1t, w1B), (w3t, w3B), (w2t, w2B) = WB[s % NW]
                xT_, xTB_ = XT[s % 2]
                hd_, hdB = hdn[s % 2]
                for j in range(4):
                    b1, b1B = bankH()
                    b3, b3B = bankH()
                    for k in range(8):
                        T.op(pe, [w1B, xTB_], [b1B], lambda k=k, j=j, b1=b1: nc.tensor.matmul(
                            b1[:, 0:256], w1t[:, bass.ds(k * 512 + j, 128, step=4)], xT_[:, k, :], start=(k == 0), stop=(k == 7)), fin=(k == 7))
                    for k in range(8):
                        T.op(pe, [w3B, xTB_], [b3B], lambda k=k, j=j, b3=b3: nc.tensor.matmul(
                            b3[:, 0:256], w3t[:, bass.ds(k * 512 + j, 128, step=4)], xT_[:, k, :], start=(k == 0), stop=(k == 7)), fin=(k == 7))
                    st_, sB = sil[j % 2]
                    T.op(act, [b1B], [sB], lambda b1=b1, st_=st_: nc.scalar.activation(out=st_[:], in_=b1[:, 0:256], func=AF.Silu))
                    T.op(dve, [sB, b3B], [hdB], lambda b3=b3, st_=st_, j=j: nc.vector.tensor_tensor(
                        hd_[:, j, :], st_[:], b3[:, 0:256], op=ALU.mult))

            def ystage(s):
                (w1t, w1B), (w3t, w3B), (w2t, w2B) = WB[s % NW]
                hd_, hdB = hdn[s % 2]
                y_, yB_ = ysb[s % NY]
                for sub in range(2):
                    for cb in range(2):
                        bo, boB = bankO()
                        for j in range(4):
                            T.op(pe, [hdB, w2B], [boB], lambda j=j, bo=bo, sub=sub, cb=cb: nc.tensor.matmul(
                                bo[:], hd_[:, j, sub * 128:(sub + 1) * 128], w2t[:, j * 1024 + cb * 512:j * 1024 + (cb + 1) * 512],
                                start=(j == 0), stop=(j == 3)), fin=(j == 3))
                        dst = y_[:, sub, cb * 512:(cb + 1) * 512]
                        if cb == 0:
                            T.op(act, [boB], [yB_], lambda bo=bo, dst=dst: nc.scalar.copy(dst, bo[:]))
                        else:
                            T.op(dve, [boB], [yB_], lambda bo=bo, dst=dst: nc.vector.tensor_copy(dst, bo[:]))

            def scatter(s):
                y_, yB_ = ysb[s % NY]
                for sub in range(2):
                    T.op(plq, [yB_, idxB], [Buf(f"ysc{s}_{sub}")], lambda sub=sub: G.indirect_dma_start(
                        out=y_d[:, :], out_offset=IOA(ap=idx_y[:, sub, s:s + 1], axis=0),
                        in_=y_[:, sub, :], in_offset=None, bounds_check=bc_x, oob_is_err=False))

            loads(0)
            loads(1)
            prep(0)
            for s in range(NSLOT):
                if s + 2 < NSLOT:
                    loads(s + 2)
                hstage(s)
                if s + 1 < NSLOT:
                    prep(s + 1)
                ystage(s)
                if s >= 1:
                    scatter(s - 1)
            scatter(NSLOT - 1)

            barrier()
            s2b.__exit__(None, None, None)
            l2g, l2gB = sbt(s2, "l2g", [128, D]); l2b, l2bB = sbt(s2, "l2b", [128, D])
            T.op(spq, [], [l2gB], lambda: nc.sync.dma_start(out=l2g[:], in_=dram_bc(ln2_g, 0, 128, D)))
            T.op(spq, [], [l2bB], lambda: nc.sync.dma_start(out=l2b[:], in_=dram_bc(ln2_b, 0, 128, D)))
            N3 = 3
            g2t, g2B = sbt(s2, "g2t3", [128, 2, D])
            T.op(spq, [g2dB], [g2B], lambda: nc.sync.dma_start(out=g2t[:], in_=g2_d))
            x1r = [sbt(s2, f"x1r{i}", [128, D]) for i in range(N3)]
            yg = [[sbt(s2, f"yg{i}_{k}", [128, D]) for k in range(2)] for i in range(N3)]
            accs = [sbt(s2, f"acc{i}", [128, D]) for i in range(N3)]
            yo = [sbt(s2, f"yo{i}", [128, D]) for i in range(N3)]
            st6s = [sbt(s2, f"st6b{i}", [128, 2, 6]) for i in range(N3)]
            mvs = [sbt(s2, f"mvb{i}", [128, 4]) for i in range(N3)]
            outB2 = Buf("outs2")

            def p3_body(ti, D_):
                samp = (ti == NPT)
                xr_, xrB_ = x1r[ti % N3]
                y_, yB_ = yo[ti % N3]
                acc, accB = accs[ti % N3]
                st6, st6B = st6s[ti % N3]
                mv, mvB = mvs[ti % N3]
                (y1, y1B), (y2, y2B) = yg[ti % N3]
                for k, (yt, ytB) in enumerate(((y1, y1B), (y2, y2B))):
                    D_(plq, [ydB, posB], [ytB], lambda k=k, yt=yt: G.indirect_dma_start(
                        out=yt[:, :], out_offset=None, in_=y_d[:, :],
                        in_offset=IOA(ap=pos_i[:, k, ti:ti + 1], axis=0), bounds_check=bc_x, oob_is_err=False))
                D_(spq, [x1dBs[ti]], [xrB_], lambda: nc.sync.dma_start(out=xr_[:], in_=x1_d[ti]))
                D_(act, [y1B, gwB], [accB], lambda: nc.scalar.activation(
                    out=acc[:], in_=y1[:], func=AF.Copy, scale=gw[:, 0, ti:ti + 1]))
                D_(dve, [y2B, gwB, accB], [accB], lambda: nc.vector.scalar_tensor_tensor(
                    out=acc[:], in0=y2[:], scalar=gw[:, 1, ti:ti + 1], in1=acc[:], op0=ALU.mult, op1=ALU.add))
                D_(pool, [accB, g2B], [accB], lambda: nc.gpsimd.tensor_tensor(
                    acc[:], acc[:], g2t[:, 1 if samp else 0, :], op=ALU.mult))
                D_(dve, [accB, xrB_], [accB], lambda: nc.vector.scalar_tensor_tensor(
                    out=acc[:], in0=xr_[:], scalar=ALPHA, in1=acc[:], op0=ALU.mult, op1=ALU.add))
                for i in range(2):
                    D_(dve, [accB], [st6B], lambda i=i: nc.vector.bn_stats(st6[:, i, :], acc[:, i * 512:(i + 1) * 512]))
                D_(dve, [st6B], [mvB], lambda: nc.vector.bn_aggr(mv[:, 0:2], st6[:].rearrange("p a b -> p (a b)")))
                D_(act, [mvB], [mvB], lambda: nc.scalar.activation(out=mv[:, 3:4], in_=mv[:, 1:2], func=AF.Sqrt, bias=EPS, scale=1.0))
                D_(dve, [mvB], [mvB], lambda: nc.vector.reciprocal(mv[:, 2:3], mv[:, 3:4]))
                D_(dve, [mvB], [mvB], lambda: nc.vector.tensor_scalar(
                    mv[:, 3:4], mv[:, 0:1], mv[:, 2:3], -1.0, op0=ALU.mult, op1=ALU.mult))
                D_(act, [accB, mvB], [yB_], lambda: nc.scalar.activation(
                    out=y_[:], in_=acc[:], func=AF.Identity, bias=mv[:, 3:4], scale=mv[:, 2:3]))
                D_(dve, [yB_, l2gB], [yB_], lambda: nc.vector.tensor_tensor(y_[:], y_[:], l2g[:], op=ALU.mult))
                D_(pool, [yB_, l2bB], [yB_], lambda: nc.gpsimd.tensor_tensor(y_[:], y_[:], l2b[:], op=ALU.add))
                dstd = ys if samp else yp[ti * 128:(ti + 1) * 128, :]
                D_(spq, [yB_], [Buf("o")], lambda: nc.sync.dma_start(out=dstd, in_=y_[:]))

            emit_skewed([lambda D_, ti=ti: p3_body(ti, D_) for ti in range(NTILE)], N3)
            if debug:
                dbgB = Buf("dbg")
                T.op(spq, [posB], [dbgB], lambda: nc.sync.dma_start(out=dbg_pos, in_=pos_i[:].rearrange("p a t -> p (a t)")))
                T.op(spq, [idxB], [dbgB], lambda: nc.sync.dma_start(out=dbg_iw, in_=idx_w[:]))
                T.op(spq, [idxB], [dbgB], lambda: nc.sync.dma_start(out=dbg_ix, in_=idx_x[:]))
                T.op(spq, [gwB], [dbgB], lambda: nc.sync.dma_start(out=dbg_gw, in_=gw[:].rearrange("p a t -> p (a t)")))
            barrier()
    return nc


def _consts():
    c = np.zeros((128, 160), np.float32)
    p = np.arange(128, dtype=np.float32)
    c[:, 0] = p
    c[:, 1] = 2 * p
    c[:, 2:18] = 256.0 * np.arange(1, 17, dtype=np.float32)[None, :]
    c[:, 18:50] = np.arange(32, dtype=np.float32)[None, :]
    c[:, 50:98] = np.arange(48, dtype=np.float32)[None, :]
    c[:, 98:114] = np.arange(16, dtype=np.float32)[None, :]
    c[:, 114:146] = 1.0
    m = np.zeros((128, 256), np.float32)
    m[:, 0:128] = np.triu(np.ones((128, 128), np.float32), k=1)
    m[:, 128:256] = 1.0
    return c, m


def _in_maps(inp):
    f = lambda a: np.ascontiguousarray(np.asarray(a, dtype=np.float32))
    shared = {
        "w_ada": f(inp["w_ada"][0]), "b_ada": f(inp["b_ada"]), "w_in": f(inp["w_in"][0]),
        "w_s": f(inp["w_s"][0]), "b_s": f(inp["b_s"][0]), "lnv_g": f(inp["lnv_g"]), "lnv_b": f(inp["lnv_b"]),
        "conv_w": f(inp["conv_w"][0]).reshape(16, 128), "conv_b": f(inp["conv_b"]).reshape(4, 128),
        "lru_wa": f(inp["lru_wa"][0]), "lru_ba": f(inp["lru_ba"][0]).reshape(4, 128),
        "lru_wx": f(inp["lru_wx"][0]), "lru_bx": f(inp["lru_bx"][0]).reshape(4, 128),
        "lru_lam": f(inp["lru_lam"]).reshape(4, 128),
        "w_out": f(inp["w_out"][0]), "ln1_g": f(inp["ln1_g"]), "ln1_b": f(inp["ln1_b"]),
        "w_rg": f(inp["w_rg"][0]), "b_rg": f(inp["b_rg"]), "w_re": f(inp["w_re"][0]), "b_re": f(inp["b_re"]),
        "w1": f(inp["w1"][0]).reshape(NE * 128, 4096), "w3": f(inp["w3"][0]).reshape(NE * 128, 4096),
        "w2": f(inp["w2"][0]).reshape(NE * 128, 4096), "cst": _consts()[0], "cmat": _consts()[1],
        "ln2_g": f(inp["ln2_g"]), "ln2_b": f(inp["ln2_b"]),
    }
    x_prompt = f(inp["x_prompt"]); x_sample = f(inp["x_sample"])
    sh = f(inp["state_rglru_h"])[0]; sc = f(inp["state_conv"])[0]
    c_p = f(inp["c_prompt"]); c_s = f(inp["c_sample"])
    in_maps = []
    for c in range(NCORE):
        m = dict(shared)
        m["xp"] = x_prompt[c]
        m["xs"] = np.ascontiguousarray(x_sample[NSS * c:NSS * (c + 1)].reshape(128, D))
        m["sth"] = np.ascontiguousarray(sh[NSS * c:NSS * (c + 1)])
        m["stc"] = np.ascontiguousarray(sc[NSS * c:NSS * (c + 1)].reshape(NSS * 3, 512))
        m["cp"] = np.ascontiguousarray(c_p[c:c + 1])
        m["cs"] = np.ascontiguousarray(c_s[NSS * c:NSS * (c + 1)])
        in_maps.append(m)
    return in_maps


def kernel(**inp):
    in_maps = _in_maps(inp)
    nc = build_nc()
    res = run_bass_kernel_spmd(nc, in_maps, core_ids=list(range(NCORE)))
    R = res.results
    y_prompt = np.stack([R[c]["yp"] for c in range(NCORE)], 0).astype(np.float32)
    y_sample = np.concatenate([R[c]["ys"].reshape(NSS, LS, D) for c in range(NCORE)], 0).astype(np.float32)
    new_h_prompt = np.concatenate([R[c]["hp"] for c in range(NCORE)], 0)[None].astype(np.float32)
    new_conv_prompt = np.stack([R[c]["cpo"] for c in range(NCORE)], 0)[None].astype(np.float32)
    new_h_sample = np.concatenate([R[c]["hs"] for c in range(NCORE)], 0)[None].astype(np.float32)
    new_conv_sample = np.concatenate([R[c]["cso"].reshape(NSS, 3, 512) for c in range(NCORE)], 0)[None].astype(np.float32)
    new_chunk_v = np.concatenate([R[c]["vs"].reshape(NSS, LS, 512) for c in range(NCORE)], 0)[None].astype(np.float32)
    return (y_prompt, y_sample, new_h_prompt, new_conv_prompt, new_h_sample, new_conv_sample, new_chunk_v)
```

```python
from contextlib import ExitStack
import numpy as np
import concourse.bass as bass
import concourse.mybir as mybir
from concourse.bass_utils import run_bass_kernel_spmd

F32 = mybir.dt.float32
BF16 = mybir.dt.bfloat16
I32 = mybir.dt.int32
AF = mybir.ActivationFunctionType
ALU = mybir.AluOpType
AX = mybir.AxisListType

D = 1024
NCORE = 8
SEQ = 2048
NPT = SEQ // 128
NTILE = NPT + 1
NSS = 16
LS = 8
NE = 32
ALPHA = 2.0 ** 0.25
EPS = 1e-5
KRING = 8
NSLOT = 48
NROWS = 2 * NTILE * 128 + 256


class Buf:
    def __init__(self, name):
        self.name = name
        self.w = None
        self.r = {}


class Q:
    def __init__(self, name, h, stream, sems, is_dma, is_pe=False):
        self.name, self.h, self.stream, self.sems = name, h, stream, sems
        self.is_dma, self.is_pe = is_dma, is_pe
        self.n = 0


class Trk:
    def __init__(self):
        self.seen = {}

    def _wait(self, q, tok):
        sem, val, src = tok
        key = (q.stream, sem.num)
        if self.seen.get(key, 0) >= val:
            return
        q.h.wait_ge(sem, val)
        self.seen[key] = val

    def op(self, q, reads, writes, emit, fin=True):
        toks = []
        for b in reads:
            if b.w is not None:
                toks.append(b.w)
        for b in writes:
            if b.w is not None:
                toks.append(b.w)
            toks.extend(b.r.values())
        for tok in toks:
            if tok[2] is q and q.is_pe:
                continue
            self._wait(q, tok)
        if q.is_dma:
            KR = len(q.sems)
            i = q.n
            sem = q.sems[i % KR]
            val = 16 * (i // KR + 1)
            if i >= KR:
                self._wait(q, (sem, val - 16, q))
            ins = emit()
            ins.then_inc(sem, 16)
            q.n += 1
            tok = (sem, val, q)
        else:
            ins = emit()
            if fin:
                q.n += 1
                ins.then_inc(q.sems[0], 1)
                tok = (q.sems[0], q.n, q)
            else:
                tok = (q.sems[0], q.n + 1, q)
        for b in reads:
            k = (tok[0].num)
            if b.r.get(k, (None, 0, None))[1] < tok[1]:
                b.r[k] = tok
        for b in writes:
            b.w = tok
            b.r = {}
        return ins


def dram_bc(ap, off, nrep, n):
    return bass.AP(ap.tensor, ap.offset + off, [[0, nrep], [1, n]])


def with_last(ap, step, n):
    return bass.AP(ap.tensor, ap.offset, [list(x) for x in ap.ap[:-1]] + [[step, n]])


def build_nc(debug=False):
    nc = bass.Bass("TRN2", target_bir_lowering=False)

    def din(n, s, dt=F32):
        return nc.dram_tensor(n, s, dt, kind="ExternalInput").ap()

    def dout(n, s, dt=F32):
        return nc.dram_tensor(n, s, dt, kind="ExternalOutput").ap()

    def dscr(n, s, dt=F32):
        return nc.dram_tensor(n, s, dt, kind="Internal").ap()

    xp = din("xp", [SEQ, D]); xs = din("xs", [128, D])
    sth = din("sth", [NSS, 512]); stc = din("stc", [NSS * 3, 512])
    cpd = din("cp", [1, D]); csd = din("cs", [NSS, D])
    w_ada = din("w_ada", [D, 6 * D]); b_ada = din("b_ada", [1, 6 * D])
    w_in = din("w_in", [D, 2048]); w_s = din("w_s", [4, 128, 128]); b_s = din("b_s", [4, 128])
    lnv_g = din("lnv_g", [1, 512]); lnv_b = din("lnv_b", [1, 512])
    conv_w = din("conv_w", [16, 128]); conv_b = din("conv_b", [4, 128])
    lru_wa = din("lru_wa", [8, 64, 64]); lru_ba = din("lru_ba", [4, 128])
    lru_wx = din("lru_wx", [8, 64, 64]); lru_bx = din("lru_bx", [4, 128])
    lru_lam = din("lru_lam", [4, 128])
    w_out = din("w_out", [D, D]); ln1_g = din("ln1_g", [1, D]); ln1_b = din("ln1_b", [1, D])
    w_rg = din("w_rg", [D, 4]); b_rg = din("b_rg", [1, 4]); w_re = din("w_re", [D, 32]); b_re = din("b_re", [1, 32])
    w1 = din("w1", [NE * 128, 4096]); w3 = din("w3", [NE * 128, 4096]); w2 = din("w2", [NE * 128, 4096])
    cst = din("cst", [128, 160]); cmat = din("cmat", [128, 256])
    ln2_g = din("ln2_g", [1, D]); ln2_b = din("ln2_b", [1, D])

    yp = dout("yp", [SEQ, D]); ys = dout("ys", [128, D])
    hp_o = dout("hp", [1, 512]); cp_o = dout("cpo", [3, 512])
    hs_o = dout("hs", [NSS, 512]); cs_o = dout("cso", [NSS * 3, 512]); vs_o = dout("vs", [128, 512])

    mods_d = dscr("mods_d", [128, 5 * D])
    x1_d = dscr("x1_d", [NTILE, 128, D])
    h2tok_d = dscr("h2tok_d", [NTILE, 128, D], BF16)
    g2_d = dscr("g2_d", [128, 2, D])
    xs_d = (dout if debug else dscr)("xs_d", [NROWS, D], BF16)
    y_d = (dout if debug else dscr)("y_d", [NROWS, D])
    if debug:
        dbg_pos = dout("dbg_pos", [128, 2 * NTILE], I32); dbg_iw = dout("dbg_iw", [128, NSLOT], I32)
        dbg_ix = dout("dbg_ix", [128, NSLOT], I32); dbg_gw = dout("dbg_gw", [128, 2 * NTILE])

    T = Trk()
    es_all = ExitStack()
    with es_all:
        nsem = [0]

        def S():
            nsem[0] += 1
            return es_all.enter_context(nc.semaphore(f"s{nsem[0]}"))

        pe = Q("pe", nc.tensor, "pe", [S()], False, True)
        act = Q("act", nc.scalar, "act", [S()], False)
        dve = Q("dve", nc.vector, "dve", [S()], False)
        pool = Q("pool", nc.gpsimd, "pool", [S()], False)
        spq = Q("spq", nc.sync, "sp", [S() for _ in range(KRING)], True)
        plq = Q("plq", nc.gpsimd, "pool", [S() for _ in range(32)], True)

        def barrier():
            qs = [pe, act, dve, pool, spq, plq]
            streams = {"pe": pe, "act": act, "dve": dve, "pool": pool, "sp": spq}
            for sname, qq in streams.items():
                for o in qs:
                    if o.is_dma:
                        KR = len(o.sems)
                        for j in range(min(o.n, KR)):
                            last = o.n - 1 - j
                            T._wait(qq, (o.sems[last % KR], 16 * (last // KR + 1), o))
                    elif o.n > 0 and o.stream != sname:
                        T._wait(qq, (o.sems[0], o.n, o))

        def emit_skewed(bodies, nset, serial_tail=0, n_umax=None):
            all_units = []
            for body in bodies:
                cur = []

                def D_(q, reads, writes, emit, fin=True, cur=cur):
                    cur.append((q, reads, writes, emit, fin))

                body(D_)
                units, g = [], []
                for opx in cur:
                    g.append(opx)
                    if opx[4]:
                        units.append(g)
                        g = []
                assert not g
                all_units.append(units)
            nb_ = len(bodies) - serial_tail
            umax = max(len(u) for u in all_units[:(n_umax or nb_)])
            stag = umax / nset
            sched = []
            for t, units in enumerate(all_units):
                base = t * stag if t < nb_ else (nb_ - 1) * stag + umax + (t - nb_) * umax
                for u, unit in enumerate(units):
                    sched.append((base + u, t, u, unit))
            sched.sort(key=lambda z: (z[0], z[1], z[2]))
            for _, _, _, unit in sched:
                for (q, r_, w_, e_, f_) in unit:
                    T.op(q, r_, w_, e_, fin=f_)

        pstk = ExitStack()
        es_all.enter_context(pstk)

        def sbt(stk, name, shape, dt=F32):
            t = stk.enter_context(nc.sbuf_tensor(name, shape, dt))
            return t, Buf(name)

        banks = []
        for i in range(8):
            t = pstk.enter_context(nc.psum_tensor(f"bank{i}", [128, 512], F32))
            banks.append((t, Buf(f"bank{i}")))
        bank_i = [0]

        def nbank():
            b = banks[bank_i[0] % 8]
            bank_i[0] += 1
            return b

        ident, identB = sbt(pstk, "ident", [128, 128])
        dmy, dmyB = sbt(pstk, "dmy", [128, 4])
        identb, identbB = sbt(pstk, "identb", [128, 128], BF16)
        gw, gwB = sbt(pstk, "gw", [128, 2, NTILE])
        pos_i, posB = sbt(pstk, "pos_i", [128, 2, NTILE], I32)
        idx_w, idxB = sbt(pstk, "idx_w", [128, NSLOT], I32)
        idx_x, _ = sbt(pstk, "idx_x", [128, NSLOT], I32)
        idx_y, _ = sbt(pstk, "idx_y", [128, 2, NSLOT], I32)

        T.op(pool, [], [identB], lambda: nc.gpsimd.memset(ident[:], 0.0))
        T.op(pool, [], [identB], lambda: nc.gpsimd.affine_select(
            out=ident[:], in_=ident[:], compare_op=ALU.not_equal, fill=1.0, base=0,
            pattern=[[-1, 128]], channel_multiplier=1))
        T.op(dve, [identB], [identbB], lambda: nc.vector.tensor_copy(identb[:], ident[:]))

        s1 = ExitStack()
        with s1:
            mod, modB = sbt(s1, "mod", [128, 5 * D])
            modBs = [Buf(f"mod{i}") for i in range(5)]
            winb, winB = sbt(s1, "winb", [128, 8, 2048], BF16)
            woutb, woutB = sbt(s1, "woutb", [128, 8, D], BF16)
            wrb, wrB = sbt(s1, "wrb", [128, 8, 36], BF16)
            rbias, rbiasB = sbt(s1, "rbias", [128, 36])
            wsT, wsTB = sbt(s1, "wsT", [128, 4, 128], BF16)
            wsTs, wsTsB = sbt(s1, "wsTs", [128, 4, 128], BF16)
            bsb, bsbB = sbt(s1, "bsb", [128, 4, 128])
            bsbs, bsbsB = sbt(s1, "bsbs", [128, 4, 128])
            wab, waB = sbt(s1, "wab", [128, 4, 128], BF16)
            wxb, wxB = sbt(s1, "wxb", [128, 4, 128], BF16)
            lnvg, lnvgB = sbt(s1, "lnvg", [128, 512]); lnvb, lnvbB = sbt(s1, "lnvb", [128, 512])
            l1g, l1gB = sbt(s1, "l1g", [128, D]); l1b, l1bB = sbt(s1, "l1b", [128, D])
            vt, vtB = sbt(s1, "vt", [128, 36])
            ca, caB = sbt(s1, "ca", [128, 8])
            h0T, h0TB = sbt(s1, "h0T", [128, 4, NSS])
            hcT, hcTB = sbt(s1, "hcT", [128, 4, NSS * 3])

            T.op(plq, [], [winB], lambda: nc.gpsimd.dma_start(
                out=winb[:], in_=w_in.rearrange("(k p) n -> p k n", p=128)))
            def sp_load(dst_ap, dstB, src_ap):
                T.op(spq, [], [dstB], lambda: nc.sync.dma_start(out=dst_ap, in_=src_ap))

            sp_load(lnvg[:], lnvgB, dram_bc(lnv_g, 0, 128, 512))
            sp_load(lnvb[:], lnvbB, dram_bc(lnv_b, 0, 128, 512))
            sp_load(l1g[:], l1gB, dram_bc(ln1_g, 0, 128, D))
            sp_load(l1b[:], l1bB, dram_bc(ln1_b, 0, 128, D))
            sp_load(bsb[:].rearrange("p a b -> p (a b)"), bsbB, dram_bc(b_s, 0, 128, 512))
            sp_load(rbias[:, 0:4], rbiasB, dram_bc(b_rg, 0, 128, 4))
            sp_load(rbias[:, 4:36], rbiasB, dram_bc(b_re, 0, 128, 32))

            s1a = ExitStack()
            s1a.__enter__()
            tmpA, tmpAB = sbt(s1a, "tmpA", [128, 4, 128])

            T.op(spq, [], [tmpAB], lambda: nc.sync.dma_start(
                out=tmpA[:, :, 0:8], in_=bass.AP(b_s.tensor, b_s.offset, [[0, 128], [128, 4], [1, 8]])))
            src = tmpA[:, :, 0:8]
            src4 = bass.AP(src.tensor, src.offset, [list(src.ap[0]), list(src.ap[1]), [0, NSS], [1, 8]])
            T.op(dve, [tmpAB], [bsbsB], lambda: nc.vector.tensor_copy(
                bsbs[:].rearrange("p a (s j) -> p a s j", j=8), src4))

            wsl, wslB = sbt(s1a, "wsl", [128, 4, 128])
            wss, wssB = sbt(s1a, "wss", [128, 4, 128])
            T.op(spq, [], [wslB], lambda: nc.sync.dma_start(out=wsl[:], in_=w_s.rearrange("h i j -> i h j")))
            T.op(pool, [], [wssB], lambda: nc.gpsimd.memset(wss[:], 0.0))
            for hd in range(4):
                for s in range(NSS):
                    T.op(spq, [wssB], [], lambda hd=hd, s=s: nc.sync.dma_start(
                        out=wss[8 * s:8 * s + 8, hd, 8 * s:8 * s + 8], in_=w_s[hd, 0:8, 0:8]))
            for (srcT, srcB, dstT, dstB) in ((wsl, wslB, wsT, wsTB), (wss, wssB, wsTs, wsTsB)):
                for hd in range(4):
                    T.op(pool, [srcB], [srcB], lambda srcT=srcT, hd=hd: nc.gpsimd.affine_select(
                        out=srcT[:, hd, :], in_=srcT[:, hd, :], compare_op=ALU.is_ge, fill=0.0, base=0,
                        pattern=[[-1, 128]], channel_multiplier=1))
                bk, bkB = nbank()
                for hd in range(4):
                    T.op(pe, [srcB, identB], [bkB], lambda srcT=srcT, hd=hd, bk=bk: nc.tensor.transpose(
                        bk[:, hd * 128:(hd + 1) * 128], srcT[:, hd, :], ident[:]), fin=(hd == 3))
                T.op(act, [bkB], [dstB], lambda dstT=dstT, bk=bk: nc.scalar.copy(
                    dstT[:].rearrange("p a b -> p (a b)"), bk[:]))

            vl, vlB = sbt(s1a, "vl", [36, 128])
            sp_load(vl[0:16, :], vlB, conv_w)
            sp_load(vl[16:20, :], vlB, conv_b)
            sp_load(vl[20:24, :], vlB, lru_ba)
            sp_load(vl[24:28, :], vlB, lru_bx)
            sp_load(vl[28:32, :], vlB, lru_lam)
            T.op(pool, [], [vlB], lambda: nc.gpsimd.memset(vl[32:36, :], 0.0))
            bk, bkB = nbank()
            T.op(pe, [vlB, identB], [bkB], lambda: nc.tensor.transpose(bk[:, 0:36], vl[:, :], ident[0:36, 0:36]))
            T.op(act, [bkB], [vtB], lambda: nc.scalar.copy(vt[:], bk[:, 0:36]))
            T.op(act, [vtB], [caB], lambda: nc.scalar.activation(out=ca[:, 0:4], in_=vt[:, 28:32], func=AF.Exp, scale=-1.0))
            T.op(act, [caB], [caB], lambda: nc.scalar.activation(out=ca[:, 0:4], in_=ca[:, 0:4], func=AF.Ln, bias=1.0, scale=1.0))
            T.op(dve, [caB], [caB], lambda: nc.vector.tensor_scalar(ca[:, 4:8], ca[:, 0:4], -16.0, None, op0=ALU.mult))
            T.op(dve, [caB], [caB], lambda: nc.vector.tensor_scalar(ca[:, 0:4], ca[:, 0:4], -8.0, None, op0=ALU.mult))

            stl, stlB = sbt(s1a, "stl", [64, 512])
            sp_load(stl[0:16, :], stlB, sth)
            sp_load(stl[16:64, :], stlB, stc)
            bk, bkB = nbank()
            for c in range(4):
                T.op(pe, [stlB, identB], [bkB], lambda c=c, bk=bk: nc.tensor.transpose(
                    bk[:, c * 64:(c + 1) * 64], stl[:, c * 128:(c + 1) * 128], ident[0:64, 0:64]), fin=(c == 3))
            bkv = bk[:, 0:256].rearrange("p (c r) -> p c r", r=64)
            T.op(act, [bkB], [h0TB], lambda: nc.scalar.copy(h0T[:], bkv[:, :, 0:16]))
            T.op(act, [bkB], [hcTB], lambda: nc.scalar.copy(hcT[:], bkv[:, :, 16:64]))

            for (dstT, dstB, srcw) in ((wab, waB, lru_wa), (wxb, wxB, lru_wx)):
                T.op(pool, [], [dstB], lambda dstT=dstT: nc.gpsimd.memset(dstT[:], 0.0))
                for c in range(4):
                    for hh in range(2):
                        T.op(plq, [dstB], [], lambda dstT=dstT, srcw=srcw, c=c, hh=hh: nc.gpsimd.dma_start(
                            out=dstT[64 * hh:64 * hh + 64, c, 64 * hh:64 * hh + 64], in_=srcw[2 * c + hh]))
                T.op(pool, [], [dstB, dmyB], lambda: nc.gpsimd.memset(dmy[:, 0:1], 0.0))
            with nc.allow_non_contiguous_dma(reason="tiny router weight load"):
                T.op(plq, [], [wrB], lambda: nc.gpsimd.dma_start(
                    out=wrb[:, :, 0:4], in_=w_rg.rearrange("(k p) n -> p k n", p=128)))
                T.op(plq, [], [wrB], lambda: nc.gpsimd.dma_start(
                    out=wrb[:, :, 4:36], in_=w_re.rearrange("(k p) n -> p k n", p=128)))

            ctl, ctlB = sbt(s1a, "ctl", [128, 2, D])
            sp_load(ctl[:, 0, :], ctlB, dram_bc(cpd, 0, 128, D))
            for s in range(NSS):
                T.op(spq, [ctlB], [], lambda s=s: nc.sync.dma_start(out=ctl[8 * s:8 * s + 8, 1, :], in_=dram_bc(csd, s * D, 8, D)))
            T.op(act, [identB], [ctlB, dmyB], lambda: nc.scalar.copy(dmy[:, 1:2], ident[:, 0:1]))
            ctb, ctbB = sbt(s1a, "ctb", [128, 2, D], BF16)
            T.op(act, [ctlB], [ctbB], lambda: nc.scalar.activation(out=ctb[:], in_=ctl[:], func=AF.Silu))
            cT, cTB = sbt(s1a, "cT", [128, 2, 8, 128], BF16)
            for g in range(2):
                bk, bkB = nbank()
                bkb = bk[:].bitcast(BF16)
                for k in range(8):
                    T.op(pe, [ctbB, identbB], [bkB], lambda g=g, k=k, bkb=bkb: nc.tensor.transpose(
                        bkb[:, k * 128:(k + 1) * 128], ctb[:, g, k * 128:(k + 1) * 128], identb[:]), fin=(k == 7))
                T.op(act, [bkB], [cTB], lambda g=g, bkb=bkb: nc.scalar.copy(
                    cT[:, g, :, :].rearrange("p k t -> p (k t)"), bkb[:, 0:1024]))

            g2t, g2B = sbt(s1a, "g2t", [128, 2, D])
            g2dB = Buf("g2_d")
            wad = [sbt(s1a, f"wad{i}", [128, 8, 512], BF16) for i in range(3)]
            bab = [sbt(s1a, f"bab{i}", [128, 512]) for i in range(2)]
            mst = [sbt(s1a, f"mst{i}", [128, 512]) for i in range(2)]
            modsB = Buf("mods_d")
            for n in range(12):
                wt, wB = wad[n % 3]
                T.op(plq, [], [wB], lambda wt=wt, n=n: nc.gpsimd.dma_start(
                    out=wt[:], in_=w_ada[:, n * 512:(n + 1) * 512].rearrange("(k p) n -> p k n", p=128)))
                bt, bB = bab[n % 2]
                sp_load(bt[:], bB, dram_bc(b_ada, n * 512, 128, 512))
                plus1 = 1.0 if (n // 2) in (1, 2, 4, 5) else 0.0
                for g in range(2):
                    bk, bkB = nbank()
                    for k in range(8):
                        T.op(pe, [cTB, wB], [bkB], lambda g=g, k=k, bk=bk, wt=wt: nc.tensor.matmul(
                            bk[:], cT[:, g, k, :], wt[:, k, :], start=(k == 0), stop=(k == 7)), fin=(k == 7))
                    if n >= 10:
                        T.op(dve, [bkB, bB], [g2B], lambda bk=bk, bt=bt, n=n, g=g, plus1=plus1: nc.vector.scalar_tensor_tensor(
                            out=g2t[:, g, (n - 10) * 512:(n - 9) * 512], in0=bk[:], scalar=plus1, in1=bt[:], op0=ALU.add, op1=ALU.add))
                    elif g == 0:
                        T.op(dve, [bkB, bB], [modBs[n // 2]], lambda bk=bk, bt=bt, n=n, plus1=plus1: nc.vector.scalar_tensor_tensor(
                            out=mod[:, n * 512:(n + 1) * 512], in0=bk[:], scalar=plus1, in1=bt[:], op0=ALU.add, op1=ALU.add))
                    else:
                        mt, mB = mst[n % 2]
                        T.op(dve, [bkB, bB], [mB], lambda bk=bk, bt=bt, mt=mt, plus1=plus1: nc.vector.scalar_tensor_tensor(
                            out=mt[:], in0=bk[:], scalar=plus1, in1=bt[:], op0=ALU.add, op1=ALU.add))
                        T.op(spq, [mB], [modsB], lambda mt=mt, n=n: nc.sync.dma_start(
                            out=mods_d[:, n * 512:(n + 1) * 512], in_=mt[:]))
            T.op(plq, [], [woutB], lambda: nc.gpsimd.dma_start(
                out=woutb[:], in_=w_out.rearrange("(k p) n -> p k n", p=128)))
            T.op(spq, [g2B], [g2dB], lambda: nc.sync.dma_start(out=g2_d, in_=g2t[:]))

            barrier()
            s1a.__exit__(None, None, None)
            NSET = 3
            RING = 4

            def mkset(i):
                d = {}
                for (nm, shp, dt) in (("xt", [128, D], F32), ("hb", [128, D], BF16), ("hT", [128, 8, 128], BF16),
                                      ("uT", [128, 4, 128], F32), ("gyT", [128, 4, 128], F32), ("xc", [128, 4, 128], F32),
                                      ("xcb", [128, 4, 128], BF16), ("At", [128, 4, 128], F32), ("Gx", [128, 4, 128], F32),
                                      ("Mt", [128, 4, 128], F32), ("mixT", [128, 8, 128], BF16),
                                      ("vnb", [128, 512], BF16), ("st6", [128, 2, 6], F32), ("mv", [128, 4], F32),
                                      ("zt", [128, D], F32),
                                      ):
                    d[nm] = sbt(s1b, f"{nm}_{i}", shp, dt)
                return d

            rla, rlaB = sbt(s1, "rla", [128, NTILE, 36])
            s1b = ExitStack()
            s1b.__enter__()
            sets = [mkset(i) for i in range(NSET)]
            XR = [sbt(s1b, f"xr{i}", [128, 4, NSS * (3 + LS)]) for i in range(RING)]
            HS = [sbt(s1b, f"hs{i}", [128, 4, 128]) for i in range(RING)]
            xrtok, xrtokB = sbt(s1b, "xrtok", [128, 512])
            hl, hlB = sbt(s1b, "hl", [128, 4, NSS])
            hrow, hrowB = xrtok, xrtokB
            tC, tCB = sbt(s1b, "tC", [128, 4, NSS])
            x1dBs = [Buf(f"x1_d{i}") for i in range(NTILE)]; h2tokBs = [Buf(f"h2tok_d{i}") for i in range(NTILE)]
            outB = Buf("outs")

            mod_reads = [0] * 5

            def tile_body(it, ti, D_):
                samp = (ti == NPT)
                sx = it % NSET
                S_ = sets[sx]
                xtile, xB = S_["xt"]; hb, hbB = S_["hb"]; hT, hTB = S_["hT"]; uT, uTB = S_["uT"]; gyT, gyTB = S_["gyT"]
                xc, xcB = S_["xc"]; xcb, xcbB = S_["xcb"]; At, AB = S_["At"]; Gx, GxB = S_["Gx"]; Mt, MB = S_["Mt"]
                mixT, mixTB = S_["mixT"]; vnb, vnbB = S_["vnb"]; st6, st6B = S_["st6"]; mv, mvB = S_["mv"]
                zt, ztB = S_["zt"]; h2b, h2bB = hb, hbB; h2T, h2TB = hT, hTB
                vn = Mt[:].rearrange("p c t -> p (c t)"); vnB = MB
                x1t, x1B = zt, ztB
                bi = [0]

                def nb():
                    nbk = (3, 3, 2)[sx]
                    b = banks[(0, 3, 6)[sx] + bi[0] % nbk]
                    bi[0] += 1
                    return b

                def layer_norm(src_ap, srcB, Dn, gT, gB, bT, bB, dst_ap, dstB):
                    nch = Dn // 512
                    src_full = src_ap[:, 0:Dn]
                    for i in range(nch):
                        D_(dve, [srcB], [st6B], lambda i=i: nc.vector.bn_stats(st6[:, i, :], src_ap[:, i * 512:(i + 1) * 512]))
                    D_(dve, [st6B], [mvB], lambda: nc.vector.bn_aggr(mv[:, 0:2], st6[:, 0:nch, :].rearrange("p a b -> p (a b)")))
                    D_(act, [mvB], [mvB], lambda: nc.scalar.activation(out=mv[:, 3:4], in_=mv[:, 1:2], func=AF.Sqrt, bias=EPS, scale=1.0))
                    D_(dve, [mvB], [mvB], lambda: nc.vector.reciprocal(mv[:, 2:3], mv[:, 3:4]))
                    D_(dve, [srcB, mvB], [dstB], lambda: nc.vector.tensor_scalar(
                        dst_ap, src_full, mv[:, 0:1], mv[:, 2:3], op0=ALU.subtract, op1=ALU.mult))
                    if Dn == D:
                        D_(pool, [dstB, gB], [dstB], lambda: nc.gpsimd.tensor_tensor(dst_ap, dst_ap, gT[:, 0:Dn], op=ALU.mult))
                        D_(pool, [dstB, bB], [dstB], lambda: nc.gpsimd.tensor_tensor(dst_ap, dst_ap, bT[:, 0:Dn], op=ALU.add))
                    else:
                        D_(dve, [dstB, gB], [dstB], lambda: nc.vector.tensor_tensor(dst_ap, dst_ap, gT[:, 0:Dn], op=ALU.mult))
                        D_(dve, [dstB, bB], [dstB], lambda: nc.vector.tensor_tensor(dst_ap, dst_ap, bT[:, 0:Dn], op=ALU.add))

                def cnt_read(i, ins):
                    if not samp:
                        mod_reads[i] += 1
                    return ins

                def mod_chunk(i):
                    if samp:
                        def reload():
                            assert mod_reads[i] == NPT * (2 if i == 2 else 1), (i, mod_reads[i])
                            return nc.sync.dma_start(out=mod[:, i * D:(i + 1) * D], in_=mods_d[:, i * D:(i + 1) * D])
                        D_(spq, [modsB], [modBs[i]], reload)
                    return modBs[i]

                nseq, L = (NSS, LS) if samp else (1, 128)
                W = 3 + L
                xsrc = xs if samp else xp[ti * 128:(ti + 1) * 128, :]
                D_(spq, [], [xB], lambda: nc.sync.dma_start(out=xtile[:], in_=xsrc))
                m1B = mod_chunk(1)
                m0B = mod_chunk(0)
                D_(dve, [xB, m1B], [ztB], lambda: cnt_read(1, nc.vector.tensor_tensor(zt[:], xtile[:], mod[:, D:2 * D], op=ALU.mult)))
                D_(pool, [ztB, m0B], [hbB], lambda: cnt_read(0, nc.gpsimd.tensor_tensor(hb[:], zt[:], mod[:, 0:D], op=ALU.add)))
                bk, bkB = nb()
                bkb = bk[:].bitcast(BF16)
                for k in range(8):
                    D_(pe, [hbB, identbB], [bkB], lambda k=k, bkb=bkb: nc.tensor.transpose(
                        bkb[:, k * 128:(k + 1) * 128], hb[:, k * 128:(k + 1) * 128], identb[:]), fin=(k == 7))
                D_(act, [bkB], [hTB], lambda bkb=bkb: nc.scalar.copy(hT[:].rearrange("p k t -> p (k t)"), bkb[:, 0:1024]))

                xr, xrB = XR[it % RING]
                xrp, xrpB = XR[(it - 1) % RING]
                xr4 = xr[:, :, 0:nseq * W].rearrange("p c (s w) -> p c s w", w=W)
                pbanks = []
                for gi, col0 in enumerate((0, 1024, 1536)):
                    bk, bkB = nb()
                    for c in range(4):
                        for k in range(8):
                            D_(pe, [winB, hTB], [bkB], lambda c=c, k=k, bk=bk, col0=col0: nc.tensor.matmul(
                                bk[:, c * 128:(c + 1) * 128], winb[:, k, col0 + c * 128:col0 + (c + 1) * 128], hT[:, k, :],
                                start=(k == 0), stop=(k == 7)), fin=(c == 3 and k == 7))
                    if gi == 0:
                        D_(act, [bkB], [uTB], lambda b=bk: nc.scalar.copy(uT[:].rearrange("p c t -> p (c t)"), b[:]))
                    elif gi == 1:
                        D_(act, [bkB], [xrB], lambda b=bk: nc.scalar.copy(
                            xr4[:, :, :, 3:W], b[:].rearrange("p (c s l) -> p c s l", c=4, s=nseq)))
                    else:
                        D_(act, [bkB], [gyTB], lambda b=bk: nc.scalar.activation(
                            out=gyT[:].rearrange("p c t -> p (c t)"), in_=b[:], func=AF.Gelu_apprx_tanh))
                if samp:
                    D_(dve, [hcTB], [xrB], lambda: nc.vector.tensor_copy(
                        xr4[:, :, :, 0:3], hcT[:].rearrange("p c (s r) -> p c s r", r=3)))
                elif ti == 0:
                    D_(dve, [], [xrB], lambda: nc.vector.memset(xr[:, :, 0:3], 0.0))
                else:
                    D_(dve, [xrpB], [xrB], lambda: nc.vector.tensor_copy(xr[:, :, 0:3], xrp[:, :, 128:131]))

                bkv_, bkvB = nb()
                for k in range(8):
                    D_(pe, [winB, hTB], [bkvB], lambda k=k, bkv_=bkv_: nc.tensor.matmul(
                        bkv_[:], hT[:, k, :], winb[:, k, 512:1024], start=(k == 0), stop=(k == 7)), fin=(k == 7))
                layer_norm(bkv_, bkvB, 512, lnvg, lnvgB, lnvb, lnvbB, vn, vnB)
                D_(act, [vnB], [vnbB], lambda: nc.scalar.copy(vnb[:], vn))
                if samp:
                    D_(spq, [vnB], [Buf("o")], lambda: nc.sync.dma_start(out=vs_o, in_=vn))
                if samp or ti == NPT - 1:
                    bkx, bkxB = nb()
                    for k in range(8):
                        D_(pe, [winB, hTB], [bkxB], lambda k=k, bkx=bkx: nc.tensor.matmul(
                            bkx[:], hT[:, k, :], winb[:, k, 1024:1536], start=(k == 0), stop=(k == 7)), fin=(k == 7))
                    D_(act, [bkxB], [xrtokB], lambda bkx=bkx: nc.scalar.copy(xrtok[:], bkx[:]))
                    if samp:
                        for s in range(NSS):
                            D_(spq, [xrtokB], [Buf("o")], lambda s=s: nc.sync.dma_start(
                                out=cs_o[3 * s:3 * s + 3, :], in_=xrtok[8 * s + 5:8 * s + 8, :]))
                    else:
                        D_(spq, [xrtokB], [Buf("o")], lambda: nc.sync.dma_start(out=cp_o, in_=xrtok[125:128, :]))

                wsX, wsXB = (wsTs, wsTsB) if samp else (wsT, wsTB)
                bsX, bsXB = (bsbs, bsbsB) if samp else (bsb, bsbB)
                bkg, bkgB = nb()
                for hd in range(4):
                    D_(pe, [vnbB, wsXB], [bkgB], lambda hd=hd, bkg=bkg, wsX=wsX: nc.tensor.matmul(
                        bkg[:, hd * 128:(hd + 1) * 128], vnb[:, hd * 128:(hd + 1) * 128], wsX[:, hd, :],
                        start=True, stop=True), fin=(hd == 3))
                D_(dve, [bkgB, bsXB], [GxB], lambda bkg=bkg, bsX=bsX: nc.vector.tensor_tensor(
                    Gx[:].rearrange("p a b -> p (a b)"), bkg[:], bsX[:].rearrange("p a b -> p (a b)"), op=ALU.add))
                D_(dve, [GxB, uTB], [mixTB], lambda: nc.vector.tensor_tensor(
                    mixT[:, 0:4, :], Gx[:], uT[:], op=ALU.mult))

                xc4 = xc[:].rearrange("p c (s l) -> p c s l", l=L)
                for c in range(4):
                    D_(dve, [xrB, vtB], [xcB], lambda c=c: nc.vector.tensor_scalar(
                        xc4[:, c, :, :], xr4[:, c, :, 0:L], vt[:, c:c + 1], vt[:, 16 + c:17 + c], op0=ALU.mult, op1=ALU.add))
                    for k in range(1, 4):
                        D_(dve, [xrB, vtB, xcB], [xcB], lambda c=c, k=k: nc.vector.scalar_tensor_tensor(
                            out=xc4[:, c, :, :], in0=xr4[:, c, :, k:k + L], scalar=vt[:, 4 * k + c:4 * k + c + 1],
                            in1=xc4[:, c, :, :], op0=ALU.mult, op1=ALU.add))
                D_(act, [xcB], [xcbB], lambda: nc.scalar.copy(xcb[:], xc[:]))
                bka, bkaB = nb()
                bkx2, bkx2B = nb()
                for c in range(4):
                    D_(pe, [xcbB, waB], [bkaB], lambda c=c, bka=bka: nc.tensor.matmul(
                        bka[:, c * 128:(c + 1) * 128], wab[:, c, :], xcb[:, c, :], start=True, stop=True), fin=(c == 3))
                for c in range(4):
                    D_(pe, [xcbB, wxB], [bkx2B], lambda c=c, bkx2=bkx2: nc.tensor.matmul(
                        bkx2[:, c * 128:(c + 1) * 128], wxb[:, c, :], xcb[:, c, :], start=True, stop=True), fin=(c == 3))
                for c in range(4):
                    D_(act, [bkaB, vtB], [AB], lambda c=c, bka=bka: nc.scalar.activation(
                        out=At[:, c, :], in_=bka[:, c * 128:(c + 1) * 128], func=AF.Sigmoid, bias=vt[:, 20 + c:21 + c], scale=1.0))
                    D_(act, [bkx2B, vtB], [GxB], lambda c=c, bkx2=bkx2: nc.scalar.activation(
                        out=Gx[:, c, :], in_=bkx2[:, c * 128:(c + 1) * 128], func=AF.Sigmoid, bias=vt[:, 24 + c:25 + c], scale=1.0))
                for c in range(4):
                    D_(act, [AB, caB], [MB], lambda c=c: nc.scalar.activation(
                        out=Mt[:, c, :], in_=At[:, c, :], func=AF.Exp, scale=ca[:, 4 + c:5 + c]))
                    D_(act, [AB, caB], [AB], lambda c=c: nc.scalar.activation(
                        out=At[:, c, :], in_=At[:, c, :], func=AF.Exp, scale=ca[:, c:c + 1]))
                D_(act, [MB], [MB], lambda: nc.scalar.activation(out=Mt[:], in_=Mt[:], func=AF.Sqrt, bias=1.0, scale=-1.0))
                D_(dve, [MB, GxB], [MB], lambda: nc.vector.tensor_tensor(Mt[:], Mt[:], Gx[:], op=ALU.mult))
                D_(dve, [MB, xcB], [MB], lambda: nc.vector.tensor_tensor(Mt[:], Mt[:], xc[:], op=ALU.mult))
                hs_, hsB = HS[it % RING]
                hsp, hspB = HS[(it - 1) % RING]
                if samp:
                    A4 = At[:].rearrange("p c (s l) -> p c s l", l=L)
                    M4 = Mt[:].rearrange("p c (s l) -> p c s l", l=L)
                    h04 = h0T[:].rearrange("p c (s o) -> p c s o", o=1)
                    t4 = tC[:].rearrange("p c (s o) -> p c s o", o=1)
                    D_(dve, [AB, h0TB], [tCB], lambda: nc.vector.tensor_tensor(t4, A4[:, :, :, 0:1], h04, op=ALU.mult))
                    D_(dve, [tCB, MB], [MB], lambda: nc.vector.tensor_tensor(M4[:, :, :, 0:1], M4[:, :, :, 0:1], t4, op=ALU.add))
                    D_(dve, [AB], [AB], lambda: nc.vector.memset(A4[:, :, :, 0:1], 0.0))
                for c in range(4):
                    init = 0.0 if (samp or ti == 0) else hsp[:, c, 127:128]
                    rds = [AB, MB] + ([] if (samp or ti == 0) else [hspB])
                    D_(dve, rds, [hsB], lambda c=c, init=init, hs_=hs_: nc.vector.tensor_tensor_scan(
                        out=hs_[:, c, :], data0=At[:, c, :], data1=Mt[:, c, :], initial=init, op0=ALU.mult, op1=ALU.add))
                D_(dve, [hsB, gyTB], [mixTB], lambda hs_=hs_: nc.vector.tensor_tensor(mixT[:, 4:8, :], hs_[:], gyT[:], op=ALU.mult))
                if samp or ti == NPT - 1:
                    ncol = NSS if samp else 1
                    if samp:
                        hs4 = hs_[:].rearrange("p c (s l) -> p c s l", l=L)
                        D_(dve, [hsB], [hlB], lambda hs4=hs4: nc.vector.tensor_copy(
                            hl[:].rearrange("p c (s o) -> p c s o", o=1), hs4[:, :, :, L - 1:L]))
                    else:
                        D_(dve, [hsB], [hlB], lambda hs_=hs_: nc.vector.tensor_copy(hl[:, :, 0:1], hs_[:, :, 127:128]))
                    bkh, bkhB = nb()
                    for c in range(4):
                        D_(pe, [hlB, identB], [bkhB], lambda c=c, bkh=bkh, ncol=ncol: nc.tensor.transpose(
                            bkh[0:ncol, c * 128:(c + 1) * 128], hl[:, c, 0:ncol], ident[:]), fin=(c == 3))
                    D_(act, [bkhB], [hrowB], lambda bkh=bkh, ncol=ncol: nc.scalar.copy(hrow[0:ncol, :], bkh[0:ncol, :]))
                    D_(spq, [hrowB], [Buf("o")], lambda ncol=ncol, samp=samp: nc.sync.dma_start(
                        out=(hs_o if samp else hp_o), in_=hrow[0:ncol, :]))

                m2B = mod_chunk(2)
                for cb in range(2):
                    bk, bkB = nb()
                    for k in range(8):
                        D_(pe, [mixTB, woutB], [bkB], lambda k=k, bk=bk, cb=cb: nc.tensor.matmul(
                            bk[:], mixT[:, k, :], woutb[:, k, cb * 512:(cb + 1) * 512], start=(k == 0), stop=(k == 7)), fin=(k == 7))
                    D_(dve, [bkB, m2B], [ztB], lambda bk=bk, cb=cb: cnt_read(2, nc.vector.tensor_tensor(
                        zt[:, cb * 512:(cb + 1) * 512], bk[:], mod[:, 2 * D + cb * 512:2 * D + (cb + 1) * 512], op=ALU.mult)))
                D_(dve, [xB, ztB], [ztB], lambda: nc.vector.scalar_tensor_tensor(
                    out=zt[:], in0=xtile[:], scalar=ALPHA, in1=zt[:], op0=ALU.mult, op1=ALU.add))
                layer_norm(zt, ztB, D, l1g, l1gB, l1b, l1bB, x1t[:], x1B)
                D_(spq, [x1B], [x1dBs[ti]], lambda: nc.sync.dma_start(out=x1_d[ti], in_=x1t[:]))
                m4B = mod_chunk(4)
                m3B = mod_chunk(3)
                D_(pool, [x1B, m4B], [xB], lambda: cnt_read(4, nc.gpsimd.tensor_tensor(xtile[:], x1t[:], mod[:, 4 * D:5 * D], op=ALU.mult)))
                D_(pool, [xB, m3B], [h2bB], lambda: cnt_read(3, nc.gpsimd.tensor_tensor(h2b[:], xtile[:], mod[:, 3 * D:4 * D], op=ALU.add)))
                D_(spq, [h2bB], [h2tokBs[ti]], lambda: nc.sync.dma_start(out=h2tok_d[ti], in_=h2b[:]))
                bk, bkB = nb()
                bkb = bk[:].bitcast(BF16)
                for k in range(8):
                    D_(pe, [h2bB, identbB], [bkB], lambda k=k, bkb=bkb: nc.tensor.transpose(
                        bkb[:, k * 128:(k + 1) * 128], h2b[:, k * 128:(k + 1) * 128], identb[:]), fin=(k == 7))
                D_(act, [bkB], [h2TB], lambda bkb=bkb: nc.scalar.copy(h2T[:].rearrange("p k t -> p (k t)"), bkb[:, 0:1024]))
                bkr, bkrB = nb()
                for k in range(8):
                    D_(pe, [h2TB, wrB], [bkrB], lambda k=k, bkr=bkr: nc.tensor.matmul(
                        bkr[:, 0:36], h2T[:, k, :], wrb[:, k, :], start=(k == 0), stop=(k == 7)), fin=(k == 7))
                D_(dve, [bkrB, rbiasB], [rlaB], lambda bkr=bkr: nc.vector.tensor_tensor(rla[:, ti, :], bkr[:, 0:36], rbias[:], op=ALU.add))

            order = list(range(NPT)) + [NPT]
            emit_skewed([lambda D_, it=it, ti=ti: tile_body(it, ti, D_) for it, ti in enumerate(order)], NSET, n_umax=NPT - 1)

            barrier()
            s1b.__exit__(None, None, None)
            G = nc.gpsimd
            bc_x = G.to_reg(NROWS - 1)
            bc_w = G.to_reg(NE * 128 - 1)
            bc_x2 = G.to_reg(NROWS - 2)
            IOA = bass.IndirectOffsetOnAxis
            h2all, h2allB = sbt(s1, "h2all", [128, NTILE, D], BF16)
            T.op(spq, h2tokBs, [h2allB], lambda: nc.sync.dma_start(out=h2all[:], in_=h2tok_d.rearrange("t p n -> p t n")))
            zpad, zpadB = sbt(s1, "zpad", [128, 2, D])
            T.op(pool, [], [zpadB], lambda: nc.gpsimd.memset(zpad[:], 0.0))
            NPAIR = 2 * NTILE * 128
            T.op(plq, [zpadB], [Buf("xs_pad")], lambda: nc.gpsimd.dma_start(
                out=xs_d[NPAIR:NROWS, :].rearrange("(p a) n -> p a n", a=2), in_=zpad[:]))
            T.op(spq, [zpadB], [Buf("y_pad")], lambda: nc.sync.dma_start(
                out=y_d[NPAIR:NROWS, :].rearrange("(p a) n -> p a n", a=2), in_=zpad[:]))
            V = nc.vector
            NT = NTILE
            gmax, rtB = sbt(s1, "gmax", [128, NT])
            ohg, _ = sbt(s1, "ohg", [128, NT, 4]); ex, _ = sbt(s1, "ex", [128, NT, 4])
            sumex, _ = sbt(s1, "sumex", [128, NT]); pg, _ = sbt(s1, "pg", [128, NT])
            lsel, _ = sbt(s1, "lsel", [128, NT, 8]); ltmp, _ = sbt(s1, "ltmp", [128, NT, 8])
            v1, _ = sbt(s1, "v1", [128, NT]); v2, _ = sbt(s1, "v2", [128, NT])
            oh1, _ = sbt(s1, "oh1", [128, NT, 8]); oh2, _ = sbt(s1, "oh2", [128, NT, 8]); msk, _ = sbt(s1, "msk", [128, NT, 8])
            wA, _ = sbt(s1, "wA", [128, NT]); wB_, _ = sbt(s1, "wB", [128, NT]); within, _ = sbt(s1, "within", [128, NT, 8])

            def bcl(ap2, n):
                a = [list(x) for x in ap2.ap]
                if len(a) == 3:
                    assert a[2][1] == 1
                    a = a[:2]
                assert len(a) == 2
                return bass.AP(ap2.tensor, ap2.offset, a + [[0, n]])

            def R(emit, q=dve, extra_r=(), extra_w=()):
                T.op(q, [rtB, rlaB] + list(extra_r), [rtB] + list(extra_w), emit)

            lg = rla[:, :, 0:4]
            R(lambda: V.tensor_reduce(gmax[:], lg, axis=AX.X, op=ALU.max))
            R(lambda: V.tensor_tensor(ohg[:], lg, bcl(gmax[:], 4), op=ALU.is_equal))
            R(lambda: V.tensor_tensor(ex[:], lg, bcl(gmax[:], 4), op=ALU.subtract))
            R(lambda: nc.scalar.activation(out=ex[:], in_=ex[:], func=AF.Exp), q=act)
            R(lambda: V.tensor_reduce(sumex[:], ex[:], axis=AX.X, op=ALU.add))
            R(lambda: V.reciprocal(pg[:], sumex[:]))
            for g in range(4):
                srcl = rla[:, :, 4 + 8 * g:12 + 8 * g]
                selb = bcl(ohg[:, :, g], 8)
                if g == 0:
                    R(lambda srcl=srcl, selb=selb: V.tensor_tensor(lsel[:], srcl, selb, op=ALU.mult))
                else:
                    R(lambda srcl=srcl, selb=selb: V.tensor_tensor(ltmp[:], srcl, selb, op=ALU.mult))
                    R(lambda: V.tensor_tensor(lsel[:], lsel[:], ltmp[:], op=ALU.add))
            R(lambda: V.tensor_reduce(v1[:], lsel[:], axis=AX.X, op=ALU.max))
            R(lambda: V.tensor_tensor(oh1[:], lsel[:], bcl(v1[:], 8), op=ALU.is_equal))
            R(lambda: V.scalar_tensor_tensor(out=msk[:], in0=oh1[:], scalar=-1e30, in1=lsel[:], op0=ALU.mult, op1=ALU.add))
            R(lambda: V.tensor_reduce(v2[:], msk[:], axis=AX.X, op=ALU.max))
            R(lambda: V.tensor_tensor(oh2[:], msk[:], bcl(v2[:], 8), op=ALU.is_equal))
            R(lambda: V.tensor_tensor(wA[:], v1[:], v2[:], op=ALU.subtract))
            R(lambda: nc.scalar.activation(out=wA[:], in_=wA[:], func=AF.Sigmoid), q=act)
            R(lambda: V.tensor_scalar(wB_[:], wA[:], -1.0, 1.0, op0=ALU.mult, op1=ALU.add))
            R(lambda: V.tensor_tensor(wA[:], wA[:], pg[:], op=ALU.mult))
            R(lambda: V.tensor_tensor(wB_[:], wB_[:], pg[:], op=ALU.mult))
            R(lambda: V.tensor_copy(gw[:, 0, :], wA[:]), extra_w=[gwB])
            R(lambda: V.tensor_copy(gw[:, 1, :], wB_[:]), extra_w=[gwB])

            def bcm(ap2, m):
                a = [list(x) for x in ap2.ap]
                assert len(a) == 2
                return bass.AP(ap2.tensor, ap2.offset, [a[0], [0, m], a[1]])

            M1, _ = sbt(s1, "M1", [128, NT, 32]); M2, _ = sbt(s1, "M2", [128, NT, 32])
            Mb, MbB = sbt(s1, "Mb", [128, NT, 32], BF16)
            cst_sb, cstB = sbt(s1, "cst_sb", [128, 160]); cm_f, cmfB = sbt(s1, "cm_f", [128, 256])
            cm_b, cmbB = sbt(s1, "cm_b", [128, 256], BF16)
            rank_sb, _ = sbt(s1, "rank_sb", [128, NT, 32]); tot_sb, _ = sbt(s1, "tot_sb", [128, 32])
            incl, _ = sbt(s1, "incl", [128, 32]); off, _ = sbt(s1, "off", [128, 32])
            posf, _ = sbt(s1, "posf", [128, 2, NT])
            cmpA, _ = sbt(s1, "cmpA", [128, 32, 16]); cmpB, _ = sbt(s1, "cmpB", [128, 16, 32])
            rko, _ = sbt(s1, "rko", [128, 32]); rka, _ = sbt(s1, "rka", [128, 16])
            big, _ = sbt(s1, "big", [128, NSLOT, 32]); big2, _ = sbt(s1, "big2", [128, NSLOT, 16])
            psf, _ = sbt(s1, "psf", [128, NSLOT]); psf2, _ = sbt(s1, "psf2", [128, NSLOT]); esf, _ = sbt(s1, "esf", [128, NSLOT])
            T.op(spq, [], [cstB], lambda: nc.sync.dma_start(out=cst_sb[:], in_=cst))
            T.op(spq, [], [cmfB], lambda: nc.sync.dma_start(out=cm_f[:], in_=cmat))
            T.op(dve, [cmfB], [cmbB], lambda: nc.vector.tensor_copy(cm_b[:], cm_f[:]))
            pidx = cst_sb[:, 0:1]; p2idx = cst_sb[:, 1:2]; a_j = cst_sb[:, 2:18]; eidx = cst_sb[:, 18:50]
            sidx = cst_sb[:, 50:50 + NSLOT]; jm1 = cst_sb[:, 98:114]; ones32 = cst_sb[:, 114:146]
            for g in range(4):
                R(lambda g=g: V.tensor_tensor(M1[:, :, 8 * g:8 * g + 8], oh1[:], bcl(ohg[:, :, g], 8), op=ALU.mult))
                R(lambda g=g: V.tensor_tensor(M2[:, :, 8 * g:8 * g + 8], oh2[:], bcl(ohg[:, :, g], 8), op=ALU.mult))
            R(lambda: V.tensor_tensor(Mb[:], M1[:], M2[:], op=ALU.add), extra_w=[MbB])
            rkA, rkAB = nbank(); rkB, rkBB = nbank()
            for ti in range(NT):
                bk, bkB_ = (rkA, rkAB) if ti < 16 else (rkB, rkBB)
                c0 = (ti % 16) * 32
                T.op(pe, [MbB, cmbB], [bkB_], lambda bk=bk, c0=c0, ti=ti: nc.tensor.matmul(
                    bk[:, c0:c0 + 32], cm_b[:, 0:128], Mb[:, ti, :], start=True, stop=(ti == 0)), fin=(ti == 0))
                for tj in range(ti):
                    T.op(pe, [MbB, cmbB], [bkB_], lambda bk=bk, c0=c0, tj=tj, ti=ti: nc.tensor.matmul(
                        bk[:, c0:c0 + 32], cm_b[:, 128:256], Mb[:, tj, :], start=False, stop=(tj == ti - 1)), fin=(tj == ti - 1))
            for tj in range(NT):
                T.op(pe, [MbB, cmbB], [rkBB], lambda tj=tj: nc.tensor.matmul(
                    rkB[:, 32:64], cm_b[:, 128:256], Mb[:, tj, :], start=(tj == 0), stop=(tj == NT - 1)), fin=(tj == NT - 1))
            R(lambda: nc.scalar.copy(rank_sb[:, 0:16, :].rearrange("p t e -> p (t e)"), rkA[:, 0:512]), q=act, extra_r=[rkAB])
            R(lambda: nc.scalar.copy(rank_sb[:, 16, :], rkB[:, 0:32]), q=act, extra_r=[rkBB])
            R(lambda: nc.scalar.copy(tot_sb[:], rkB[:, 32:64]), q=act, extra_r=[rkBB])
            R(lambda: V.tensor_tensor_scan(out=incl[:], data0=ones32, data1=tot_sb[:], initial=0.0, op0=ALU.mult, op1=ALU.add),
              extra_r=[cstB])
            R(lambda: V.tensor_tensor(off[:], incl[:], tot_sb[:], op=ALU.subtract))
            R(lambda: V.tensor_tensor(rank_sb[:], rank_sb[:], bcm(off[:], NT), op=ALU.add))
            R(lambda: V.tensor_tensor(M1[:], M1[:], rank_sb[:], op=ALU.mult))
            R(lambda: V.tensor_reduce(posf[:, 0, :], M1[:], axis=AX.X, op=ALU.add))
            R(lambda: V.tensor_tensor(M2[:], M2[:], rank_sb[:], op=ALU.mult))
            R(lambda: V.tensor_reduce(posf[:, 1, :], M2[:], axis=AX.X, op=ALU.add))
            R(lambda: V.tensor_copy(pos_i[:], posf[:]), extra_w=[posB])
            for ti in range(NTILE):
                for k in range(2):
                    T.op(plq, [h2allB, posB], [Buf(f"xs_{ti}_{k}")], lambda ti=ti, k=k: G.indirect_dma_start(
                        out=xs_d[:, :], out_offset=IOA(ap=pos_i[:, k, ti:ti + 1], axis=0),
                        in_=h2all[:, ti, :], in_offset=None, bounds_check=bc_x, oob_is_err=False))
            R(lambda: V.tensor_tensor(cmpA[:], bcl(off[:], 16), bcm(a_j, 32), op=ALU.is_ge))
            R(lambda: V.tensor_reduce(rko[:], cmpA[:], axis=AX.X, op=ALU.add))
            R(lambda: V.tensor_tensor(rko[:], rko[:], eidx, op=ALU.add))
            R(lambda: V.tensor_tensor(cmpB[:], bcl(a_j, 32), bcm(off[:], 16), op=ALU.is_gt))
            R(lambda: V.tensor_reduce(rka[:], cmpB[:], axis=AX.X, op=ALU.add))
            R(lambda: V.tensor_tensor(rka[:], rka[:], jm1, op=ALU.add))
            R(lambda: V.tensor_tensor(big[:], bcm(rko[:], NSLOT), bcl(sidx, 32), op=ALU.is_equal))
            R(lambda: V.tensor_tensor(big[:], big[:], bcm(off[:], NSLOT), op=ALU.mult))
            R(lambda: V.tensor_reduce(psf[:], big[:], axis=AX.X, op=ALU.add))
            R(lambda: V.tensor_tensor(big2[:], bcm(rka[:], NSLOT), bcl(sidx, 16), op=ALU.is_equal))
            R(lambda: V.tensor_tensor(big2[:], big2[:], bcm(a_j, NSLOT), op=ALU.mult))
            R(lambda: V.tensor_reduce(psf2[:], big2[:], axis=AX.X, op=ALU.add))
            R(lambda: V.tensor_tensor(psf[:], psf[:], psf2[:], op=ALU.add))
            R(lambda: V.tensor_tensor(big[:], bcm(rko[:], NSLOT), bcl(sidx, 32), op=ALU.is_le))
            R(lambda: V.tensor_reduce(esf[:], big[:], axis=AX.X, op=ALU.add))
            R(lambda: V.tensor_scalar(esf[:], esf[:], 128.0, -128.0, op0=ALU.mult, op1=ALU.add))
            R(lambda: V.tensor_scalar(esf[:], esf[:], pidx, None, op0=ALU.add))
            R(lambda: V.tensor_copy(idx_w[:], esf[:]), extra_w=[idxB])
            pend, _ = sbt(s1, "pend", [128, NSLOT]); rowf, _ = sbt(s1, "rowf", [128, NSLOT]); inval, _ = sbt(s1, "inval", [128, NSLOT])
            R(lambda: V.tensor_copy(pend[:, 0:NSLOT - 1], psf[:, 1:NSLOT]))
            R(lambda: V.memset(pend[:, NSLOT - 1:NSLOT], float(2 * NTILE * 128)))
            R(lambda: V.tensor_scalar(psf[:], psf[:], p2idx, None, op0=ALU.add))
            R(lambda: V.tensor_copy(idx_x[:], psf[:]), extra_w=[idxB])
            for sub in range(2):
                R(lambda sub=sub: V.tensor_scalar(rowf[:], psf[:], float(sub), None, op0=ALU.add))
                R(lambda: V.tensor_tensor(inval[:], rowf[:], pend[:], op=ALU.is_ge))
                R(lambda: V.scalar_tensor_tensor(out=rowf[:], in0=inval[:], scalar=1048576.0, in1=rowf[:], op0=ALU.mult, op1=ALU.add))
                R(lambda sub=sub: V.tensor_copy(idx_y[:, sub, :], rowf[:]), extra_w=[idxB])

            barrier()
        s2 = ExitStack()
        with s2:
            s2b = ExitStack()
            s2b.__enter__()
            NW = 3
            WB = [[sbt(s2b, f"w{n}b{i}", [128, 4096], BF16) for n in (1, 3, 2)] for i in range(NW)]
            XRb = [sbt(s2b, f"xrb{i}", [128, 2, D], BF16) for i in range(NW)]
            XRb2 = [Buf(f"xrb{i}_1") for i in range(NW)]
            for (xr0_, xr0B_), xr0B2_ in zip(XRb, XRb2):
                T.op(pool, [], [xr0B_, xr0B2_], lambda xr0_=xr0_: nc.gpsimd.memset(xr0_[:], 0.0))
            XT = [sbt(s2b, f"xT{i}", [128, 8, 256], BF16) for i in range(2)]
            hdn = [sbt(s2b, f"hdn{i}", [128, 4, 256], BF16) for i in range(2)]
            sil = [sbt(s2b, f"sil{i}", [128, 256]) for i in range(2)]
            NY = 4
            ysb = [sbt(s2b, f"ysb{i}", [128, 2, D]) for i in range(NY)]
            ydB = Buf("y_d")
            cT = [0]; cH = [0]; cO = [0]

            def bankT():
                b = banks[cT[0] % 2]; cT[0] += 1; return b

            def bankH():
                b = banks[2 + cH[0] % 4]; cH[0] += 1; return b

            def bankO():
                b = banks[6 + cO[0] % 2]; cO[0] += 1; return b

            def loads(s):
                xr_, xrB_ = XRb[s % NW]
                for sub in range(2):
                    T.op(plq, [idxB], [xrB_ if sub == 0 else XRb2[s % NW]], lambda sub=sub: G.indirect_dma_start(
                        out=xr_[:, sub, :], out_offset=None, in_=xs_d[:, :],
                        in_offset=IOA(ap=idx_y[:, sub, s:s + 1], axis=0), bounds_check=bc_x, oob_is_err=False))
                for (wt_, wB_), src in zip(WB[s % NW], (w1, w3, w2)):
                    T.op(plq, [idxB], [wB_], lambda wt_=wt_, src=src: G.indirect_dma_start(
                        out=wt_[:, :], out_offset=None, in_=src[:, :],
                        in_offset=IOA(ap=idx_w[:, s:s + 1], axis=0), bounds_check=bc_w, oob_is_err=False))

            def prep(s):
                xr_, xrB_ = XRb[s % NW]
                xT_, xTB_ = XT[s % 2]
                for sub in range(2):
                    bk, bkB = bankT()
                    bkb = bk[:].bitcast(BF16)
                    for k in range(8):
                        T.op(pe, [xrB_ if sub == 0 else XRb2[s % NW], identbB], [bkB], lambda k=k, bkb=bkb, sub=sub: nc.tensor.transpose(
                            bkb[:, k * 128:(k + 1) * 128], xr_[:, sub, bass.ds(k, 128, step=8)], identb[:]), fin=(k == 7))
                    dstv = xT_[:, :, sub * 128:(sub + 1) * 128]
                    srcv = bkb[:, 0:1024].rearrange("p (k t) -> p k t", k=8)
                    if sub == 0:
                        T.op(act, [bkB], [xTB_], lambda dstv=dstv, srcv=srcv: nc.scalar.copy(dstv, srcv))
                    else:
                        T.op(dve, [bkB], [xTB_], lambda dstv=dstv, srcv=srcv: nc.vector.tensor_copy(dstv, srcv))

            def hstage(s):
                (w1t, w1B), (w3t, w3B), (w2t, w2B) = WB[s % NW]
                xT_, xTB_ = XT[s % 2]
                hd_, hdB = hdn[s % 2]
                for j in range(4):
                    b1, b1B = bankH()
                    b3, b3B = bankH()
                    for k in range(8):
                        T.op(pe, [w1B, xTB_], [b1B], lambda k=k, j=j, b1=b1: nc.tensor.matmul(
                            b1[:, 0:256], w1t[:, bass.ds(k * 512 + j, 128, step=4)], xT_[:, k, :], start=(k == 0), stop=(k == 7)), fin=(k == 7))
                    for k in range(8):
                        T.op(pe, [w3B, xTB_], [b3B], lambda k=k, j=j, b3=b3: nc.tensor.matmul(
                            b3[:, 0:256], w3t[:, bass.ds(k * 512 + j, 128, step=4)], xT_[:, k, :], start=(k == 0), stop=(k == 7)), fin=(k == 7))
                    st_, sB = sil[j % 2]
                    T.op(act, [b1B], [sB], lambda b1=b1, st_=st_: nc.scalar.activation(out=st_[:], in_=b1[:, 0:256], func=AF.Silu))
                    T.op(dve, [sB, b3B], [hdB], lambda b3=b3, st_=st_, j=j: nc.vector.tensor_tensor(
                        hd_[:, j, :], st_[:], b3[:, 0:256], op=ALU.mult))

            def ystage(s):
                (w1t, w1B), (w3t, w3B), (w2t, w2B) = WB[s % NW]
                hd_, hdB = hdn[s % 2]
                y_, yB_ = ysb[s % NY]
                for sub in range(2):
                    for cb in range(2):
                        bo, boB = bankO()
                        for j in range(4):
                            T.op(pe, [hdB, w2B], [boB], lambda j=j, bo=bo, sub=sub, cb=cb: nc.tensor.matmul(
                                bo[:], hd_[:, j, sub * 128:(sub + 1) * 128], w2t[:, j * 1024 + cb * 512:j * 1024 + (cb + 1) * 512],
                                start=(j == 0), stop=(j == 3)), fin=(j == 3))
                        dst = y_[:, sub, cb * 512:(cb + 1) * 512]
                        if cb == 0:
                            T.op(act, [boB], [yB_], lambda bo=bo, dst=dst: nc.scalar.copy(dst, bo[:]))
                        else:
                            T.op(dve, [boB], [yB_], lambda bo=bo, dst=dst: nc.vector.tensor_copy(dst, bo[:]))

            def scatter(s):
                y_, yB_ = ysb[s % NY]
                for sub in range(2):
                    T.op(plq, [yB_, idxB], [Buf(f"ysc{s}_{sub}")], lambda sub=sub: G.indirect_dma_start(
                        out=y_d[:, :], out_offset=IOA(ap=idx_y[:, sub, s:s + 1], axis=0),
                        in_=y_[:, sub, :], in_offset=None, bounds_check=bc_x, oob_is_err=False))

            loads(0)
            loads(1)
            prep(0)
            for s in range(NSLOT):
                if s + 2 < NSLOT:
                    loads(s + 2)
                hstage(s)
                if s + 1 < NSLOT:
                    prep(s + 1)
                ystage(s)
                if s >= 1:
                    scatter(s - 1)
            scatter(NSLOT - 1)

            barrier()
            s2b.__exit__(None, None, None)
            l2g, l2gB = sbt(s2, "l2g", [128, D]); l2b, l2bB = sbt(s2, "l2b", [128, D])
            T.op(spq, [], [l2gB], lambda: nc.sync.dma_start(out=l2g[:], in_=dram_bc(ln2_g, 0, 128, D)))
            T.op(spq, [], [l2bB], lambda: nc.sync.dma_start(out=l2b[:], in_=dram_bc(ln2_b, 0, 128, D)))
            N3 = 3
            g2t, g2B = sbt(s2, "g2t3", [128, 2, D])
            T.op(spq, [g2dB], [g2B], lambda: nc.sync.dma_start(out=g2t[:], in_=g2_d))
            x1r = [sbt(s2, f"x1r{i}", [128, D]) for i in range(N3)]
            yg = [[sbt(s2, f"yg{i}_{k}", [128, D]) for k in range(2)] for i in range(N3)]
            accs = [sbt(s2, f"acc{i}", [128, D]) for i in range(N3)]
            yo = [sbt(s2, f"yo{i}", [128, D]) for i in range(N3)]
            st6s = [sbt(s2, f"st6b{i}", [128, 2, 6]) for i in range(N3)]
            mvs = [sbt(s2, f"mvb{i}", [128, 4]) for i in range(N3)]
            outB2 = Buf("outs2")

            def p3_body(ti, D_):
                samp = (ti == NPT)
                xr_, xrB_ = x1r[ti % N3]
                y_, yB_ = yo[ti % N3]
                acc, accB = accs[ti % N3]
                st6, st6B = st6s[ti % N3]
                mv, mvB = mvs[ti % N3]
                (y1, y1B), (y2, y2B) = yg[ti % N3]
                for k, (yt, ytB) in enumerate(((y1, y1B), (y2, y2B))):
                    D_(plq, [ydB, posB], [ytB], lambda k=k, yt=yt: G.indirect_dma_start(
                        out=yt[:, :], out_offset=None, in_=y_d[:, :],
                        in_offset=IOA(ap=pos_i[:, k, ti:ti + 1], axis=0), bounds_check=bc_x, oob_is_err=False))
                D_(spq, [x1dBs[ti]], [xrB_], lambda: nc.sync.dma_start(out=xr_[:], in_=x1_d[ti]))
                D_(act, [y1B, gwB], [accB], lambda: nc.scalar.activation(
                    out=acc[:], in_=y1[:], func=AF.Copy, scale=gw[:, 0, ti:ti + 1]))
                D_(dve, [y2B, gwB, accB], [accB], lambda: nc.vector.scalar_tensor_tensor(
                    out=acc[:], in0=y2[:], scalar=gw[:, 1, ti:ti + 1], in1=acc[:], op0=ALU.mult, op1=ALU.add))
                D_(pool, [accB, g2B], [accB], lambda: nc.gpsimd.tensor_tensor(
                    acc[:], acc[:], g2t[:, 1 if samp else 0, :], op=ALU.mult))
                D_(dve, [accB, xrB_], [accB], lambda: nc.vector.scalar_tensor_tensor(
                    out=acc[:], in0=xr_[:], scalar=ALPHA, in1=acc[:], op0=ALU.mult, op1=ALU.add))
                for i in range(2):
                    D_(dve, [accB], [st6B], lambda i=i: nc.vector.bn_stats(st6[:, i, :], acc[:, i * 512:(i + 1) * 512]))
                D_(dve, [st6B], [mvB], lambda: nc.vector.bn_aggr(mv[:, 0:2], st6[:].rearrange("p a b -> p (a b)")))
                D_(act, [mvB], [mvB], lambda: nc.scalar.activation(out=mv[:, 3:4], in_=mv[:, 1:2], func=AF.Sqrt, bias=EPS, scale=1.0))
                D_(dve, [mvB], [mvB], lambda: nc.vector.reciprocal(mv[:, 2:3], mv[:, 3:4]))
                D_(dve, [mvB], [mvB], lambda: nc.vector.tensor_scalar(
                    mv[:, 3:4], mv[:, 0:1], mv[:, 2:3], -1.0, op0=ALU.mult, op1=ALU.mult))
                D_(act, [accB, mvB], [yB_], lambda: nc.scalar.activation(
                    out=y_[:], in_=acc[:], func=AF.Identity, bias=mv[:, 3:4], scale=mv[:, 2:3]))
                D_(dve, [yB_, l2gB], [yB_], lambda: nc.vector.tensor_tensor(y_[:], y_[:], l2g[:], op=ALU.mult))
                D_(pool, [yB_, l2bB], [yB_], lambda: nc.gpsimd.tensor_tensor(y_[:], y_[:], l2b[:], op=ALU.add))
                dstd = ys if samp else yp[ti * 128:(ti + 1) * 128, :]
                D_(spq, [yB_], [Buf("o")], lambda: nc.sync.dma_start(out=dstd, in_=y_[:]))

            emit_skewed([lambda D_, ti=ti: p3_body(ti, D_) for ti in range(NTILE)], N3)
            if debug:
                dbgB = Buf("dbg")
                T.op(spq, [posB], [dbgB], lambda: nc.sync.dma_start(out=dbg_pos, in_=pos_i[:].rearrange("p a t -> p (a t)")))
                T.op(spq, [idxB], [dbgB], lambda: nc.sync.dma_start(out=dbg_iw, in_=idx_w[:]))
                T.op(spq, [idxB], [dbgB], lambda: nc.sync.dma_start(out=dbg_ix, in_=idx_x[:]))
                T.op(spq, [gwB], [dbgB], lambda: nc.sync.dma_start(out=dbg_gw, in_=gw[:].rearrange("p a t -> p (a t)")))
            barrier()
    return nc


def _consts():
    c = np.zeros((128, 160), np.float32)
    p = np.arange(128, dtype=np.float32)
    c[:, 0] = p
    c[:, 1] = 2 * p
    c[:, 2:18] = 256.0 * np.arange(1, 17, dtype=np.float32)[None, :]
    c[:, 18:50] = np.arange(32, dtype=np.float32)[None, :]
    c[:, 50:98] = np.arange(48, dtype=np.float32)[None, :]
    c[:, 98:114] = np.arange(16, dtype=np.float32)[None, :]
    c[:, 114:146] = 1.0
    m = np.zeros((128, 256), np.float32)
    m[:, 0:128] = np.triu(np.ones((128, 128), np.float32), k=1)
    m[:, 128:256] = 1.0
    return c, m


def _in_maps(inp):
    f = lambda a: np.ascontiguousarray(np.asarray(a, dtype=np.float32))
    shared = {
        "w_ada": f(inp["w_ada"][0]), "b_ada": f(inp["b_ada"]), "w_in": f(inp["w_in"][0]),
        "w_s": f(inp["w_s"][0]), "b_s": f(inp["b_s"][0]), "lnv_g": f(inp["lnv_g"]), "lnv_b": f(inp["lnv_b"]),
        "conv_w": f(inp["conv_w"][0]).reshape(16, 128), "conv_b": f(inp["conv_b"]).reshape(4, 128),
        "lru_wa": f(inp["lru_wa"][0]), "lru_ba": f(inp["lru_ba"][0]).reshape(4, 128),
        "lru_wx": f(inp["lru_wx"][0]), "lru_bx": f(inp["lru_bx"][0]).reshape(4, 128),
        "lru_lam": f(inp["lru_lam"]).reshape(4, 128),
        "w_out": f(inp["w_out"][0]), "ln1_g": f(inp["ln1_g"]), "ln1_b": f(inp["ln1_b"]),
        "w_rg": f(inp["w_rg"][0]), "b_rg": f(inp["b_rg"]), "w_re": f(inp["w_re"][0]), "b_re": f(inp["b_re"]),
        "w1": f(inp["w1"][0]).reshape(NE * 128, 4096), "w3": f(inp["w3"][0]).reshape(NE * 128, 4096),
        "w2": f(inp["w2"][0]).reshape(NE * 128, 4096), "cst": _consts()[0], "cmat": _consts()[1],
        "ln2_g": f(inp["ln2_g"]), "ln2_b": f(inp["ln2_b"]),
    }
    x_prompt = f(inp["x_prompt"]); x_sample = f(inp["x_sample"])
    sh = f(inp["state_rglru_h"])[0]; sc = f(inp["state_conv"])[0]
    c_p = f(inp["c_prompt"]); c_s = f(inp["c_sample"])
    in_maps = []
    for c in range(NCORE):
        m = dict(shared)
        m["xp"] = x_prompt[c]
        m["xs"] = np.ascontiguousarray(x_sample[NSS * c:NSS * (c + 1)].reshape(128, D))
        m["sth"] = np.ascontiguousarray(sh[NSS * c:NSS * (c + 1)])
        m["stc"] = np.ascontiguousarray(sc[NSS * c:NSS * (c + 1)].reshape(NSS * 3, 512))
        m["cp"] = np.ascontiguousarray(c_p[c:c + 1])
        m["cs"] = np.ascontiguousarray(c_s[NSS * c:NSS * (c + 1)])
        in_maps.append(m)
    return in_maps


def kernel(**inp):
    in_maps = _in_maps(inp)
    nc = build_nc()
    res = run_bass_kernel_spmd(nc, in_maps, core_ids=list(range(NCORE)))
    R = res.results
    y_prompt = np.stack([R[c]["yp"] for c in range(NCORE)], 0).astype(np.float32)
    y_sample = np.concatenate([R[c]["ys"].reshape(NSS, LS, D) for c in range(NCORE)], 0).astype(np.float32)
    new_h_prompt = np.concatenate([R[c]["hp"] for c in range(NCORE)], 0)[None].astype(np.float32)
    new_conv_prompt = np.stack([R[c]["cpo"] for c in range(NCORE)], 0)[None].astype(np.float32)
    new_h_sample = np.concatenate([R[c]["hs"] for c in range(NCORE)], 0)[None].astype(np.float32)
    new_conv_sample = np.concatenate([R[c]["cso"].reshape(NSS, 3, 512) for c in range(NCORE)], 0)[None].astype(np.float32)
    new_chunk_v = np.concatenate([R[c]["vs"].reshape(NSS, LS, 512) for c in range(NCORE)], 0)[None].astype(np.float32)
    return (y_prompt, y_sample, new_h_prompt, new_conv_prompt, new_h_sample, new_conv_sample, new_chunk_v)
```

```python
from contextlib import ExitStack
import numpy as np
import concourse.bass as bass
import concourse.mybir as mybir
from concourse.bass_utils import run_bass_kernel_spmd

F32 = mybir.dt.float32
BF16 = mybir.dt.bfloat16
I32 = mybir.dt.int32
AF = mybir.ActivationFunctionType
ALU = mybir.AluOpType
AX = mybir.AxisListType

D = 1024
NCORE = 8
SEQ = 2048
NPT = SEQ // 128
NTILE = NPT + 1
NSS = 16
LS = 8
NE = 32
ALPHA = 2.0 ** 0.25
EPS = 1e-5
KRING = 8
NSLOT = 48
NROWS = 2 * NTILE * 128 + 256


class Buf:
    def __init__(self, name):
        self.name = name
        self.w = None
        self.r = {}


class Q:
    def __init__(self, name, h, stream, sems, is_dma, is_pe=False):
        self.name, self.h, self.stream, self.sems = name, h, stream, sems
        self.is_dma, self.is_pe = is_dma, is_pe
        self.n = 0


class Trk:
    def __init__(self):
        self.seen = {}

    def _wait(self, q, tok):
        sem, val, src = tok
        key = (q.stream, sem.num)
        if self.seen.get(key, 0) >= val:
            return
        q.h.wait_ge(sem, val)
        self.seen[key] = val

    def op(self, q, reads, writes, emit, fin=True):
        toks = []
        for b in reads:
            if b.w is not None:
                toks.append(b.w)
        for b in writes:
            if b.w is not None:
                toks.append(b.w)
            toks.extend(b.r.values())
        for tok in toks:
            if tok[2] is q and q.is_pe:
                continue
            self._wait(q, tok)
        if q.is_dma:
            KR = len(q.sems)
            i = q.n
            sem = q.sems[i % KR]
            val = 16 * (i // KR + 1)
            if i >= KR:
                self._wait(q, (sem, val - 16, q))
            ins = emit()
            ins.then_inc(sem, 16)
            q.n += 1
            tok = (sem, val, q)
        else:
            ins = emit()
            if fin:
                q.n += 1
                ins.then_inc(q.sems[0], 1)
                tok = (q.sems[0], q.n, q)
            else:
                tok = (q.sems[0], q.n + 1, q)
        for b in reads:
            k = (tok[0].num)
            if b.r.get(k, (None, 0, None))[1] < tok[1]:
                b.r[k] = tok
        for b in writes:
            b.w = tok
            b.r = {}
        return ins


def dram_bc(ap, off, nrep, n):
    return bass.AP(ap.tensor, ap.offset + off, [[0, nrep], [1, n]])


def with_last(ap, step, n):
    return bass.AP(ap.tensor, ap.offset, [list(x) for x in ap.ap[:-1]] + [[step, n]])


def build_nc(debug=False):
    nc = bass.Bass("TRN2", target_bir_lowering=False)

    def din(n, s, dt=F32):
        return nc.dram_tensor(n, s, dt, kind="ExternalInput").ap()

    def dout(n, s, dt=F32):
        return nc.dram_tensor(n, s, dt, kind="ExternalOutput").ap()

    def dscr(n, s, dt=F32):
        return nc.dram_tensor(n, s, dt, kind="Internal").ap()

    xp = din("xp", [SEQ, D]); xs = din("xs", [128, D])
    sth = din("sth", [NSS, 512]); stc = din("stc", [NSS * 3, 512])
    cpd = din("cp", [1, D]); csd = din("cs", [NSS, D])
    w_ada = din("w_ada", [D, 6 * D]); b_ada = din("b_ada", [1, 6 * D])
    w_in = din("w_in", [D, 2048]); w_s = din("w_s", [4, 128, 128]); b_s = din("b_s", [4, 128])
    lnv_g = din("lnv_g", [1, 512]); lnv_b = din("lnv_b", [1, 512])
    conv_w = din("conv_w", [16, 128]); conv_b = din("conv_b", [4, 128])
    lru_wa = din("lru_wa", [8, 64, 64]); lru_ba = din("lru_ba", [4, 128])
    lru_wx = din("lru_wx", [8, 64, 64]); lru_bx = din("lru_bx", [4, 128])
    lru_lam = din("lru_lam", [4, 128])
    w_out = din("w_out", [D, D]); ln1_g = din("ln1_g", [1, D]); ln1_b = din("ln1_b", [1, D])
    w_rg = din("w_rg", [D, 4]); b_rg = din("b_rg", [1, 4]); w_re = din("w_re", [D, 32]); b_re = din("b_re", [1, 32])
    w1 = din("w1", [NE * 128, 4096]); w3 = din("w3", [NE * 128, 4096]); w2 = din("w2", [NE * 128, 4096])
    cst = din("cst", [128, 160]); cmat = din("cmat", [128, 256])
    ln2_g = din("ln2_g", [1, D]); ln2_b = din("ln2_b", [1, D])

    yp = dout("yp", [SEQ, D]); ys = dout("ys", [128, D])
    hp_o = dout("hp", [1, 512]); cp_o = dout("cpo", [3, 512])
    hs_o = dout("hs", [NSS, 512]); cs_o = dout("cso", [NSS * 3, 512]); vs_o = dout("vs", [128, 512])

    mods_d = dscr("mods_d", [128, 5 * D])
    x1_d = dscr("x1_d", [NTILE, 128, D])
    h2tok_d = dscr("h2tok_d", [NTILE, 128, D], BF16)
    g2_d = dscr("g2_d", [128, 2, D])
    xs_d = (dout if debug else dscr)("xs_d", [NROWS, D], BF16)
    y_d = (dout if debug else dscr)("y_d", [NROWS, D])
    if debug:
        dbg_pos = dout("dbg_pos", [128, 2 * NTILE], I32); dbg_iw = dout("dbg_iw", [128, NSLOT], I32)
        dbg_ix = dout("dbg_ix", [128, NSLOT], I32); dbg_gw = dout("dbg_gw", [128, 2 * NTILE])

    T = Trk()
    es_all = ExitStack()
    with es_all:
        nsem = [0]

        def S():
            nsem[0] += 1
            return es_all.enter_context(nc.semaphore(f"s{nsem[0]}"))

        pe = Q("pe", nc.tensor, "pe", [S()], False, True)
        act = Q("act", nc.scalar, "act", [S()], False)
        dve = Q("dve", nc.vector, "dve", [S()], False)
        pool = Q("pool", nc.gpsimd, "pool", [S()], False)
        spq = Q("spq", nc.sync, "sp", [S() for _ in range(KRING)], True)
        plq = Q("plq", nc.gpsimd, "pool", [S() for _ in range(32)], True)

        def barrier():
            qs = [pe, act, dve, pool, spq, plq]
            streams = {"pe": pe, "act": act, "dve": dve, "pool": pool, "sp": spq}
            for sname, qq in streams.items():
                for o in qs:
                    if o.is_dma:
                        KR = len(o.sems)
                        for j in range(min(o.n, KR)):
                            last = o.n - 1 - j
                            T._wait(qq, (o.sems[last % KR], 16 * (last // KR + 1), o))
                    elif o.n > 0 and o.stream != sname:
                        T._wait(qq, (o.sems[0], o.n, o))

        def emit_skewed(bodies, nset, serial_tail=0, n_umax=None):
            all_units = []
            for body in bodies:
                cur = []

                def D_(q, reads, writes, emit, fin=True, cur=cur):
                    cur.append((q, reads, writes, emit, fin))

                body(D_)
                units, g = [], []
                for opx in cur:
                    g.append(opx)
                    if opx[4]:
                        units.append(g)
                        g = []
                assert not g
                all_units.append(units)
            nb_ = len(bodies) - serial_tail
            umax = max(len(u) for u in all_units[:(n_umax or nb_)])
            stag = umax / nset
            sched = []
            for t, units in enumerate(all_units):
                base = t * stag if t < nb_ else (nb_ - 1) * stag + umax + (t - nb_) * umax
                for u, unit in enumerate(units):
                    sched.append((base + u, t, u, unit))
            sched.sort(key=lambda z: (z[0], z[1], z[2]))
            for _, _, _, unit in sched:
                for (q, r_, w_, e_, f_) in unit:
                    T.op(q, r_, w_, e_, fin=f_)

        pstk = ExitStack()
        es_all.enter_context(pstk)

        def sbt(stk, name, shape, dt=F32):
            t = stk.enter_context(nc.sbuf_tensor(name, shape, dt))
            return t, Buf(name)

        banks = []
        for i in range(8):
            t = pstk.enter_context(nc.psum_tensor(f"bank{i}", [128, 512], F32))
            banks.append((t, Buf(f"bank{i}")))
        bank_i = [0]

        def nbank():
            b = banks[bank_i[0] % 8]
            bank_i[0] += 1
            return b

        ident, identB = sbt(pstk, "ident", [128, 128])
        dmy, dmyB = sbt(pstk, "dmy", [128, 4])
        identb, identbB = sbt(pstk, "identb", [128, 128], BF16)
        gw, gwB = sbt(pstk, "gw", [128, 2, NTILE])
        pos_i, posB = sbt(pstk, "pos_i", [128, 2, NTILE], I32)
        idx_w, idxB = sbt(pstk, "idx_w", [128, NSLOT], I32)
        idx_x, _ = sbt(pstk, "idx_x", [128, NSLOT], I32)
        idx_y, _ = sbt(pstk, "idx_y", [128, 2, NSLOT], I32)

        T.op(pool, [], [identB], lambda: nc.gpsimd.memset(ident[:], 0.0))
        T.op(pool, [], [identB], lambda: nc.gpsimd.affine_select(
            out=ident[:], in_=ident[:], compare_op=ALU.not_equal, fill=1.0, base=0,
            pattern=[[-1, 128]], channel_multiplier=1))
        T.op(dve, [identB], [identbB], lambda: nc.vector.tensor_copy(identb[:], ident[:]))

        s1 = ExitStack()
        with s1:
            mod, modB = sbt(s1, "mod", [128, 5 * D])
            modBs = [Buf(f"mod{i}") for i in range(5)]
            winb, winB = sbt(s1, "winb", [128, 8, 2048], BF16)
            woutb, woutB = sbt(s1, "woutb", [128, 8, D], BF16)
            wrb, wrB = sbt(s1, "wrb", [128, 8, 36], BF16)
            rbias, rbiasB = sbt(s1, "rbias", [128, 36])
            wsT, wsTB = sbt(s1, "wsT", [128, 4, 128], BF16)
            wsTs, wsTsB = sbt(s1, "wsTs", [128, 4, 128], BF16)
            bsb, bsbB = sbt(s1, "bsb", [128, 4, 128])
            bsbs, bsbsB = sbt(s1, "bsbs", [128, 4, 128])
            wab, waB = sbt(s1, "wab", [128, 4, 128], BF16)
            wxb, wxB = sbt(s1, "wxb", [128, 4, 128], BF16)
            lnvg, lnvgB = sbt(s1, "lnvg", [128, 512]); lnvb, lnvbB = sbt(s1, "lnvb", [128, 512])
            l1g, l1gB = sbt(s1, "l1g", [128, D]); l1b, l1bB = sbt(s1, "l1b", [128, D])
            vt, vtB = sbt(s1, "vt", [128, 36])
            ca, caB = sbt(s1, "ca", [128, 8])
            h0T, h0TB = sbt(s1, "h0T", [128, 4, NSS])
            hcT, hcTB = sbt(s1, "hcT", [128, 4, NSS * 3])

            T.op(plq, [], [winB], lambda: nc.gpsimd.dma_start(
                out=winb[:], in_=w_in.rearrange("(k p) n -> p k n", p=128)))
            def sp_load(dst_ap, dstB, src_ap):
                T.op(spq, [], [dstB], lambda: nc.sync.dma_start(out=dst_ap, in_=src_ap))

            sp_load(lnvg[:], lnvgB, dram_bc(lnv_g, 0, 128, 512))
            sp_load(lnvb[:], lnvbB, dram_bc(lnv_b, 0, 128, 512))
            sp_load(l1g[:], l1gB, dram_bc(ln1_g, 0, 128, D))
            sp_load(l1b[:], l1bB, dram_bc(ln1_b, 0, 128, D))
            sp_load(bsb[:].rearrange("p a b -> p (a b)"), bsbB, dram_bc(b_s, 0, 128, 512))
            sp_load(rbias[:, 0:4], rbiasB, dram_bc(b_rg, 0, 128, 4))
            sp_load(rbias[:, 4:36], rbiasB, dram_bc(b_re, 0, 128, 32))

            s1a = ExitStack()
            s1a.__enter__()
            tmpA, tmpAB = sbt(s1a, "tmpA", [128, 4, 128])

            T.op(spq, [], [tmpAB], lambda: nc.sync.dma_start(
                out=tmpA[:, :, 0:8], in_=bass.AP(b_s.tensor, b_s.offset, [[0, 128], [128, 4], [1, 8]])))
            src = tmpA[:, :, 0:8]
            src4 = bass.AP(src.tensor, src.offset, [list(src.ap[0]), list(src.ap[1]), [0, NSS], [1, 8]])
            T.op(dve, [tmpAB], [bsbsB], lambda: nc.vector.tensor_copy(
                bsbs[:].rearrange("p a (s j) -> p a s j", j=8), src4))

            wsl, wslB = sbt(s1a, "wsl", [128, 4, 128])
            wss, wssB = sbt(s1a, "wss", [128, 4, 128])
            T.op(spq, [], [wslB], lambda: nc.sync.dma_start(out=wsl[:], in_=w_s.rearrange("h i j -> i h j")))
            T.op(pool, [], [wssB], lambda: nc.gpsimd.memset(wss[:], 0.0))
            for hd in range(4):
                for s in range(NSS):
                    T.op(spq, [wssB], [], lambda hd=hd, s=s: nc.sync.dma_start(
                        out=wss[8 * s:8 * s + 8, hd, 8 * s:8 * s + 8], in_=w_s[hd, 0:8, 0:8]))
            for (srcT, srcB, dstT, dstB) in ((wsl, wslB, wsT, wsTB), (wss, wssB, wsTs, wsTsB)):
                for hd in range(4):
                    T.op(pool, [srcB], [srcB], lambda srcT=srcT, hd=hd: nc.gpsimd.affine_select(
                        out=srcT[:, hd, :], in_=srcT[:, hd, :], compare_op=ALU.is_ge, fill=0.0, base=0,
                        pattern=[[-1, 128]], channel_multiplier=1))
                bk, bkB = nbank()
                for hd in range(4):
                    T.op(pe, [srcB, identB], [bkB], lambda srcT=srcT, hd=hd, bk=bk: nc.tensor.transpose(
                        bk[:, hd * 128:(hd + 1) * 128], srcT[:, hd, :], ident[:]), fin=(hd == 3))
                T.op(act, [bkB], [dstB], lambda dstT=dstT, bk=bk: nc.scalar.copy(
                    dstT[:].rearrange("p a b -> p (a b)"), bk[:]))

            vl, vlB = sbt(s1a, "vl", [36, 128])
            sp_load(vl[0:16, :], vlB, conv_w)
            sp_load(vl[16:20, :], vlB, conv_b)
            sp_load(vl[20:24, :], vlB, lru_ba)
            sp_load(vl[24:28, :], vlB, lru_bx)
            sp_load(vl[28:32, :], vlB, lru_lam)
            T.op(pool, [], [vlB], lambda: nc.gpsimd.memset(vl[32:36, :], 0.0))
            bk, bkB = nbank()
            T.op(pe, [vlB, identB], [bkB], lambda: nc.tensor.transpose(bk[:, 0:36], vl[:, :], ident[0:36, 0:36]))
            T.op(act, [bkB], [vtB], lambda: nc.scalar.copy(vt[:], bk[:, 0:36]))
            T.op(act, [vtB], [caB], lambda: nc.scalar.activation(out=ca[:, 0:4], in_=vt[:, 28:32], func=AF.Exp, scale=-1.0))
            T.op(act, [caB], [caB], lambda: nc.scalar.activation(out=ca[:, 0:4], in_=ca[:, 0:4], func=AF.Ln, bias=1.0, scale=1.0))
            T.op(dve, [caB], [caB], lambda: nc.vector.tensor_scalar(ca[:, 4:8], ca[:, 0:4], -16.0, None, op0=ALU.mult))
            T.op(dve, [caB], [caB], lambda: nc.vector.tensor_scalar(ca[:, 0:4], ca[:, 0:4], -8.0, None, op0=ALU.mult))

            stl, stlB = sbt(s1a, "stl", [64, 512])
            sp_load(stl[0:16, :], stlB, sth)
            sp_load(stl[16:64, :], stlB, stc)
            bk, bkB = nbank()
            for c in range(4):
                T.op(pe, [stlB, identB], [bkB], lambda c=c, bk=bk: nc.tensor.transpose(
                    bk[:, c * 64:(c + 1) * 64], stl[:, c * 128:(c + 1) * 128], ident[0:64, 0:64]), fin=(c == 3))
            bkv = bk[:, 0:256].rearrange("p (c r) -> p c r", r=64)
            T.op(act, [bkB], [h0TB], lambda: nc.scalar.copy(h0T[:], bkv[:, :, 0:16]))
            T.op(act, [bkB], [hcTB], lambda: nc.scalar.copy(hcT[:], bkv[:, :, 16:64]))

            for (dstT, dstB, srcw) in ((wab, waB, lru_wa), (wxb, wxB, lru_wx)):
                T.op(pool, [], [dstB], lambda dstT=dstT: nc.gpsimd.memset(dstT[:], 0.0))
                for c in range(4):
                    for hh in range(2):
                        T.op(plq, [dstB], [], lambda dstT=dstT, srcw=srcw, c=c, hh=hh: nc.gpsimd.dma_start(
                            out=dstT[64 * hh:64 * hh + 64, c, 64 * hh:64 * hh + 64], in_=srcw[2 * c + hh]))
                T.op(pool, [], [dstB, dmyB], lambda: nc.gpsimd.memset(dmy[:, 0:1], 0.0))
            with nc.allow_non_contiguous_dma(reason="tiny router weight load"):
                T.op(plq, [], [wrB], lambda: nc.gpsimd.dma_start(
                    out=wrb[:, :, 0:4], in_=w_rg.rearrange("(k p) n -> p k n", p=128)))
                T.op(plq, [], [wrB], lambda: nc.gpsimd.dma_start(
                    out=wrb[:, :, 4:36], in_=w_re.rearrange("(k p) n -> p k n", p=128)))

            ctl, ctlB = sbt(s1a, "ctl", [128, 2, D])
            sp_load(ctl[:, 0, :], ctlB, dram_bc(cpd, 0, 128, D))
            for s in range(NSS):
                T.op(spq, [ctlB], [], lambda s=s: nc.sync.dma_start(out=ctl[8 * s:8 * s + 8, 1, :], in_=dram_bc(csd, s * D, 8, D)))
            T.op(act, [identB], [ctlB, dmyB], lambda: nc.scalar.copy(dmy[:, 1:2], ident[:, 0:1]))
            ctb, ctbB = sbt(s1a, "ctb", [128, 2, D], BF16)
            T.op(act, [ctlB], [ctbB], lambda: nc.scalar.activation(out=ctb[:], in_=ctl[:], func=AF.Silu))
            cT, cTB = sbt(s1a, "cT", [128, 2, 8, 128], BF16)
            for g in range(2):
                bk, bkB = nbank()
                bkb = bk[:].bitcast(BF16)
                for k in range(8):
                    T.op(pe, [ctbB, identbB], [bkB], lambda g=g, k=k, bkb=bkb: nc.tensor.transpose(
                        bkb[:, k * 128:(k + 1) * 128], ctb[:, g, k * 128:(k + 1) * 128], identb[:]), fin=(k == 7))
                T.op(act, [bkB], [cTB], lambda g=g, bkb=bkb: nc.scalar.copy(
                    cT[:, g, :, :].rearrange("p k t -> p (k t)"), bkb[:, 0:1024]))

            g2t, g2B = sbt(s1a, "g2t", [128, 2, D])
            g2dB = Buf("g2_d")
            wad = [sbt(s1a, f"wad{i}", [128, 8, 512], BF16) for i in range(3)]
            bab = [sbt(s1a, f"bab{i}", [128, 512]) for i in range(2)]
            mst = [sbt(s1a, f"mst{i}", [128, 512]) for i in range(2)]
            modsB = Buf("mods_d")
            for n in range(12):
                wt, wB = wad[n % 3]
                T.op(plq, [], [wB], lambda wt=wt, n=n: nc.gpsimd.dma_start(
                    out=wt[:], in_=w_ada[:, n * 512:(n + 1) * 512].rearrange("(k p) n -> p k n", p=128)))
                bt, bB = bab[n % 2]
                sp_load(bt[:], bB, dram_bc(b_ada, n * 512, 128, 512))
                plus1 = 1.0 if (n // 2) in (1, 2, 4, 5) else 0.0
                for g in range(2):
                    bk, bkB = nbank()
                    for k in range(8):
                        T.op(pe, [cTB, wB], [bkB], lambda g=g, k=k, bk=bk, wt=wt: nc.tensor.matmul(
                            bk[:], cT[:, g, k, :], wt[:, k, :], start=(k == 0), stop=(k == 7)), fin=(k == 7))
                    if n >= 10:
                        T.op(dve, [bkB, bB], [g2B], lambda bk=bk, bt=bt, n=n, g=g, plus1=plus1: nc.vector.scalar_tensor_tensor(
                            out=g2t[:, g, (n - 10) * 512:(n - 9) * 512], in0=bk[:], scalar=plus1, in1=bt[:], op0=ALU.add, op1=ALU.add))
                    elif g == 0:
                        T.op(dve, [bkB, bB], [modBs[n // 2]], lambda bk=bk, bt=bt, n=n, plus1=plus1: nc.vector.scalar_tensor_tensor(
                            out=mod[:, n * 512:(n + 1) * 512], in0=bk[:], scalar=plus1, in1=bt[:], op0=ALU.add, op1=ALU.add))
                    else:
                        mt, mB = mst[n % 2]
                        T.op(dve, [bkB, bB], [mB], lambda bk=bk, bt=bt, mt=mt, plus1=plus1: nc.vector.scalar_tensor_tensor(
                            out=mt[:], in0=bk[:], scalar=plus1, in1=bt[:], op0=ALU.add, op1=ALU.add))
                        T.op(spq, [mB], [modsB], lambda mt=mt, n=n: nc.sync.dma_start(
                            out=mods_d[:, n * 512:(n + 1) * 512], in_=mt[:]))
            T.op(plq, [], [woutB], lambda: nc.gpsimd.dma_start(
                out=woutb[:], in_=w_out.rearrange("(k p) n -> p k n", p=128)))
            T.op(spq, [g2B], [g2dB], lambda: nc.sync.dma_start(out=g2_d, in_=g2t[:]))

            barrier()
            s1a.__exit__(None, None, None)
            NSET = 3
            RING = 4

            def mkset(i):
                d = {}
                for (nm, shp, dt) in (("xt", [128, D], F32), ("hb", [128, D], BF16), ("hT", [128, 8, 128], BF16),
                                      ("uT", [128, 4, 128], F32), ("gyT", [128, 4, 128], F32), ("xc", [128, 4, 128], F32),
                                      ("xcb", [128, 4, 128], BF16), ("At", [128, 4, 128], F32), ("Gx", [128, 4, 128], F32),
                                      ("Mt", [128, 4, 128], F32), ("mixT", [128, 8, 128], BF16),
                                      ("vnb", [128, 512], BF16), ("st6", [128, 2, 6], F32), ("mv", [128, 4], F32),
                                      ("zt", [128, D], F32),
                                      ):
                    d[nm] = sbt(s1b, f"{nm}_{i}", shp, dt)
                return d

            rla, rlaB = sbt(s1, "rla", [128, NTILE, 36])
            s1b = ExitStack()
            s1b.__enter__()
            sets = [mkset(i) for i in range(NSET)]
            nh, nhB = sbt(s1b, "nhalf", [128, 1])
            T.op(pool, [], [nhB], lambda: nc.gpsimd.memset(nh[:], -0.5))
            XR = [sbt(s1b, f"xr{i}", [128, 4, NSS * (3 + LS)]) for i in range(RING)]
            HS = [sbt(s1b, f"hs{i}", [128, 4, 128]) for i in range(RING)]
            xrtok, xrtokB = sbt(s1b, "xrtok", [128, 512])
            hl, hlB = sbt(s1b, "hl", [128, 4, NSS])
            hrow, hrowB = xrtok, xrtokB
            tC, tCB = sbt(s1b, "tC", [128, 4, NSS])
            x1dBs = [Buf(f"x1_d{i}") for i in range(NTILE)]; h2tokBs = [Buf(f"h2tok_d{i}") for i in range(NTILE)]
            outB = Buf("outs")

            mod_reads = [0] * 5

            def tile_body(it, ti, D_):
                samp = (ti == NPT)
                sx = it % NSET
                S_ = sets[sx]
                xtile, xB = S_["xt"]; hb, hbB = S_["hb"]; hT, hTB = S_["hT"]; uT, uTB = S_["uT"]; gyT, gyTB = S_["gyT"]
                xc, xcB = S_["xc"]; xcb, xcbB = S_["xcb"]; At, AB = S_["At"]; Gx, GxB = S_["Gx"]; Mt, MB = S_["Mt"]
                mixT, mixTB = S_["mixT"]; vnb, vnbB = S_["vnb"]; st6, st6B = S_["st6"]; mv, mvB = S_["mv"]
                zt, ztB = S_["zt"]; h2b, h2bB = hb, hbB; h2T, h2TB = hT, hTB
                vn = Mt[:].rearrange("p c t -> p (c t)"); vnB = MB
                x1t, x1B = zt, ztB
                bi = [0]

                def nb():
                    nbk = (3, 3, 2)[sx]
                    b = banks[(0, 3, 6)[sx] + bi[0] % nbk]
                    bi[0] += 1
                    return b

                def layer_norm(src_ap, srcB, Dn, gT, gB, bT, bB, dst_ap, dstB):
                    nch = Dn // 512
                    src_full = src_ap[:, 0:Dn]
                    for i in range(nch):
                        D_(dve, [srcB], [st6B], lambda i=i: nc.vector.bn_stats(st6[:, i, :], src_ap[:, i * 512:(i + 1) * 512]))
                    D_(dve, [st6B], [mvB], lambda: nc.vector.bn_aggr(mv[:, 0:2], st6[:, 0:nch, :].rearrange("p a b -> p (a b)")))
                    D_(dve, [mvB], [mvB], lambda: nc.vector.tensor_scalar(mv[:, 3:4], mv[:, 1:2], EPS, None, op0=ALU.add))
                    D_(pool, [mvB, nhB], [mvB], lambda: nc.gpsimd.tensor_tensor(mv[:, 2:3], mv[:, 3:4], nh[:, 0:1], op=ALU.pow))
                    D_(dve, [srcB, mvB], [dstB], lambda: nc.vector.tensor_scalar(
                        dst_ap, src_full, mv[:, 0:1], mv[:, 2:3], op0=ALU.subtract, op1=ALU.mult))
                    if Dn == D:
                        D_(pool, [dstB, gB], [dstB], lambda: nc.gpsimd.tensor_tensor(dst_ap, dst_ap, gT[:, 0:Dn], op=ALU.mult))
                        D_(pool, [dstB, bB], [dstB], lambda: nc.gpsimd.tensor_tensor(dst_ap, dst_ap, bT[:, 0:Dn], op=ALU.add))
                    else:
                        D_(dve, [dstB, gB], [dstB], lambda: nc.vector.tensor_tensor(dst_ap, dst_ap, gT[:, 0:Dn], op=ALU.mult))
                        D_(dve, [dstB, bB], [dstB], lambda: nc.vector.tensor_tensor(dst_ap, dst_ap, bT[:, 0:Dn], op=ALU.add))

                def cnt_read(i, ins):
                    if not samp:
                        mod_reads[i] += 1
                    return ins

                def mod_chunk(i):
                    if samp:
                        def reload():
                            assert mod_reads[i] == NPT * (2 if i == 2 else 1), (i, mod_reads[i])
                            return nc.sync.dma_start(out=mod[:, i * D:(i + 1) * D], in_=mods_d[:, i * D:(i + 1) * D])
                        D_(spq, [modsB], [modBs[i]], reload)
                    return modBs[i]

                nseq, L = (NSS, LS) if samp else (1, 128)
                W = 3 + L
                xsrc = xs if samp else xp[ti * 128:(ti + 1) * 128, :]
                D_(spq, [], [xB], lambda: nc.sync.dma_start(out=xtile[:], in_=xsrc))
                m1B = mod_chunk(1)
                m0B = mod_chunk(0)
                D_(dve, [xB, m1B], [ztB], lambda: cnt_read(1, nc.vector.tensor_tensor(zt[:], xtile[:], mod[:, D:2 * D], op=ALU.mult)))
                D_(pool, [ztB, m0B], [hbB], lambda: cnt_read(0, nc.gpsimd.tensor_tensor(hb[:], zt[:], mod[:, 0:D], op=ALU.add)))
                bk, bkB = nb()
                bkb = bk[:].bitcast(BF16)
                for k in range(8):
                    D_(pe, [hbB, identbB], [bkB], lambda k=k, bkb=bkb: nc.tensor.transpose(
                        bkb[:, k * 128:(k + 1) * 128], hb[:, k * 128:(k + 1) * 128], identb[:]), fin=(k == 7))
                D_(act, [bkB], [hTB], lambda bkb=bkb: nc.scalar.copy(hT[:].rearrange("p k t -> p (k t)"), bkb[:, 0:1024]))

                xr, xrB = XR[it % RING]
                xrp, xrpB = XR[(it - 1) % RING]
                xr4 = xr[:, :, 0:nseq * W].rearrange("p c (s w) -> p c s w", w=W)
                pbanks = []
                for gi, col0 in enumerate((0, 1024, 1536)):
                    bk, bkB = nb()
                    for c in range(4):
                        for k in range(8):
                            D_(pe, [winB, hTB], [bkB], lambda c=c, k=k, bk=bk, col0=col0: nc.tensor.matmul(
                                bk[:, c * 128:(c + 1) * 128], winb[:, k, col0 + c * 128:col0 + (c + 1) * 128], hT[:, k, :],
                                start=(k == 0), stop=(k == 7)), fin=(c == 3 and k == 7))
                    if gi == 0:
                        D_(act, [bkB], [uTB], lambda b=bk: nc.scalar.copy(uT[:].rearrange("p c t -> p (c t)"), b[:]))
                    elif gi == 1:
                        D_(act, [bkB], [xrB], lambda b=bk: nc.scalar.copy(
                            xr4[:, :, :, 3:W], b[:].rearrange("p (c s l) -> p c s l", c=4, s=nseq)))
                    else:
                        D_(act, [bkB], [gyTB], lambda b=bk: nc.scalar.activation(
                            out=gyT[:].rearrange("p c t -> p (c t)"), in_=b[:], func=AF.Gelu_apprx_tanh))
                if samp:
                    D_(dve, [hcTB], [xrB], lambda: nc.vector.tensor_copy(
                        xr4[:, :, :, 0:3], hcT[:].rearrange("p c (s r) -> p c s r", r=3)))
                elif ti == 0:
                    D_(dve, [], [xrB], lambda: nc.vector.memset(xr[:, :, 0:3], 0.0))
                else:
                    D_(dve, [xrpB], [xrB], lambda: nc.vector.tensor_copy(xr[:, :, 0:3], xrp[:, :, 128:131]))

                bkv_, bkvB = nb()
                for k in range(8):
                    D_(pe, [winB, hTB], [bkvB], lambda k=k, bkv_=bkv_: nc.tensor.matmul(
                        bkv_[:], hT[:, k, :], winb[:, k, 512:1024], start=(k == 0), stop=(k == 7)), fin=(k == 7))
                layer_norm(bkv_, bkvB, 512, lnvg, lnvgB, lnvb, lnvbB, vn, vnB)
                D_(act, [vnB], [vnbB], lambda: nc.scalar.copy(vnb[:], vn))
                if samp:
                    D_(spq, [vnB], [Buf("o")], lambda: nc.sync.dma_start(out=vs_o, in_=vn))
                if samp or ti == NPT - 1:
                    bkx, bkxB = nb()
                    for k in range(8):
                        D_(pe, [winB, hTB], [bkxB], lambda k=k, bkx=bkx: nc.tensor.matmul(
                            bkx[:], hT[:, k, :], winb[:, k, 1024:1536], start=(k == 0), stop=(k == 7)), fin=(k == 7))
                    D_(act, [bkxB], [xrtokB], lambda bkx=bkx: nc.scalar.copy(xrtok[:], bkx[:]))
                    if samp:
                        for s in range(NSS):
                            D_(spq, [xrtokB], [Buf("o")], lambda s=s: nc.sync.dma_start(
                                out=cs_o[3 * s:3 * s + 3, :], in_=xrtok[8 * s + 5:8 * s + 8, :]))
                    else:
                        D_(spq, [xrtokB], [Buf("o")], lambda: nc.sync.dma_start(out=cp_o, in_=xrtok[125:128, :]))

                wsX, wsXB = (wsTs, wsTsB) if samp else (wsT, wsTB)
                bsX, bsXB = (bsbs, bsbsB) if samp else (bsb, bsbB)
                bkg, bkgB = nb()
                for hd in range(4):
                    D_(pe, [vnbB, wsXB], [bkgB], lambda hd=hd, bkg=bkg, wsX=wsX: nc.tensor.matmul(
                        bkg[:, hd * 128:(hd + 1) * 128], vnb[:, hd * 128:(hd + 1) * 128], wsX[:, hd, :],
                        start=True, stop=True), fin=(hd == 3))
                D_(dve, [bkgB, bsXB], [GxB], lambda bkg=bkg, bsX=bsX: nc.vector.tensor_tensor(
                    Gx[:].rearrange("p a b -> p (a b)"), bkg[:], bsX[:].rearrange("p a b -> p (a b)"), op=ALU.add))
                D_(dve, [GxB, uTB], [mixTB], lambda: nc.vector.tensor_tensor(
                    mixT[:, 0:4, :], Gx[:], uT[:], op=ALU.mult))

                xc4 = xc[:].rearrange("p c (s l) -> p c s l", l=L)
                for c in range(4):
                    D_(dve, [xrB, vtB], [xcB], lambda c=c: nc.vector.tensor_scalar(
                        xc4[:, c, :, :], xr4[:, c, :, 0:L], vt[:, c:c + 1], vt[:, 16 + c:17 + c], op0=ALU.mult, op1=ALU.add))
                    for k in range(1, 4):
                        D_(dve, [xrB, vtB, xcB], [xcB], lambda c=c, k=k: nc.vector.scalar_tensor_tensor(
                            out=xc4[:, c, :, :], in0=xr4[:, c, :, k:k + L], scalar=vt[:, 4 * k + c:4 * k + c + 1],
                            in1=xc4[:, c, :, :], op0=ALU.mult, op1=ALU.add))
                D_(act, [xcB], [xcbB], lambda: nc.scalar.copy(xcb[:], xc[:]))
                bka, bkaB = nb()
                bkx2, bkx2B = nb()
                for c in range(4):
                    D_(pe, [xcbB, waB], [bkaB], lambda c=c, bka=bka: nc.tensor.matmul(
                        bka[:, c * 128:(c + 1) * 128], wab[:, c, :], xcb[:, c, :], start=True, stop=True), fin=(c == 3))
                for c in range(4):
                    D_(pe, [xcbB, wxB], [bkx2B], lambda c=c, bkx2=bkx2: nc.tensor.matmul(
                        bkx2[:, c * 128:(c + 1) * 128], wxb[:, c, :], xcb[:, c, :], start=True, stop=True), fin=(c == 3))
                for c in range(4):
                    D_(act, [bkaB, vtB], [AB], lambda c=c, bka=bka: nc.scalar.activation(
                        out=At[:, c, :], in_=bka[:, c * 128:(c + 1) * 128], func=AF.Sigmoid, bias=vt[:, 20 + c:21 + c], scale=1.0))
                    D_(act, [bkx2B, vtB], [GxB], lambda c=c, bkx2=bkx2: nc.scalar.activation(
                        out=Gx[:, c, :], in_=bkx2[:, c * 128:(c + 1) * 128], func=AF.Sigmoid, bias=vt[:, 24 + c:25 + c], scale=1.0))
                for c in range(4):
                    D_(act, [AB, caB], [MB], lambda c=c: nc.scalar.activation(
                        out=Mt[:, c, :], in_=At[:, c, :], func=AF.Exp, scale=ca[:, 4 + c:5 + c]))
                    D_(act, [AB, caB], [AB], lambda c=c: nc.scalar.activation(
                        out=At[:, c, :], in_=At[:, c, :], func=AF.Exp, scale=ca[:, c:c + 1]))
                D_(act, [MB], [MB], lambda: nc.scalar.activation(out=Mt[:], in_=Mt[:], func=AF.Sqrt, bias=1.0, scale=-1.0))
                D_(dve, [MB, GxB], [MB], lambda: nc.vector.tensor_tensor(Mt[:], Mt[:], Gx[:], op=ALU.mult))
                D_(dve, [MB, xcB], [MB], lambda: nc.vector.tensor_tensor(Mt[:], Mt[:], xc[:], op=ALU.mult))
                hs_, hsB = HS[it % RING]
                hsp, hspB = HS[(it - 1) % RING]
                if samp:
                    A4 = At[:].rearrange("p c (s l) -> p c s l", l=L)
                    M4 = Mt[:].rearrange("p c (s l) -> p c s l", l=L)
                    h04 = h0T[:].rearrange("p c (s o) -> p c s o", o=1)
                    t4 = tC[:].rearrange("p c (s o) -> p c s o", o=1)
                    D_(dve, [AB, h0TB], [tCB], lambda: nc.vector.tensor_tensor(t4, A4[:, :, :, 0:1], h04, op=ALU.mult))
                    D_(dve, [tCB, MB], [MB], lambda: nc.vector.tensor_tensor(M4[:, :, :, 0:1], M4[:, :, :, 0:1], t4, op=ALU.add))
                    D_(dve, [AB], [AB], lambda: nc.vector.memset(A4[:, :, :, 0:1], 0.0))
                for c in range(4):
                    init = 0.0 if (samp or ti == 0) else hsp[:, c, 127:128]
                    rds = [AB, MB] + ([] if (samp or ti == 0) else [hspB])
                    D_(dve, rds, [hsB], lambda c=c, init=init, hs_=hs_: nc.vector.tensor_tensor_scan(
                        out=hs_[:, c, :], data0=At[:, c, :], data1=Mt[:, c, :], initial=init, op0=ALU.mult, op1=ALU.add))
                D_(dve, [hsB, gyTB], [mixTB], lambda hs_=hs_: nc.vector.tensor_tensor(mixT[:, 4:8, :], hs_[:], gyT[:], op=ALU.mult))
                if samp or ti == NPT - 1:
                    ncol = NSS if samp else 1
                    if samp:
                        hs4 = hs_[:].rearrange("p c (s l) -> p c s l", l=L)
                        D_(dve, [hsB], [hlB], lambda hs4=hs4: nc.vector.tensor_copy(
                            hl[:].rearrange("p c (s o) -> p c s o", o=1), hs4[:, :, :, L - 1:L]))
                    else:
                        D_(dve, [hsB], [hlB], lambda hs_=hs_: nc.vector.tensor_copy(hl[:, :, 0:1], hs_[:, :, 127:128]))
                    bkh, bkhB = nb()
                    for c in range(4):
                        D_(pe, [hlB, identB], [bkhB], lambda c=c, bkh=bkh, ncol=ncol: nc.tensor.transpose(
                            bkh[0:ncol, c * 128:(c + 1) * 128], hl[:, c, 0:ncol], ident[:]), fin=(c == 3))
                    D_(act, [bkhB], [hrowB], lambda bkh=bkh, ncol=ncol: nc.scalar.copy(hrow[0:ncol, :], bkh[0:ncol, :]))
                    D_(spq, [hrowB], [Buf("o")], lambda ncol=ncol, samp=samp: nc.sync.dma_start(
                        out=(hs_o if samp else hp_o), in_=hrow[0:ncol, :]))

                m2B = mod_chunk(2)
                for cb in range(2):
                    bk, bkB = nb()
                    for k in range(8):
                        D_(pe, [mixTB, woutB], [bkB], lambda k=k, bk=bk, cb=cb: nc.tensor.matmul(
                            bk[:], mixT[:, k, :], woutb[:, k, cb * 512:(cb + 1) * 512], start=(k == 0), stop=(k == 7)), fin=(k == 7))
                    D_(dve, [bkB, m2B], [ztB], lambda bk=bk, cb=cb: cnt_read(2, nc.vector.tensor_tensor(
                        zt[:, cb * 512:(cb + 1) * 512], bk[:], mod[:, 2 * D + cb * 512:2 * D + (cb + 1) * 512], op=ALU.mult)))
                D_(dve, [xB, ztB], [ztB], lambda: nc.vector.scalar_tensor_tensor(
                    out=zt[:], in0=xtile[:], scalar=ALPHA, in1=zt[:], op0=ALU.mult, op1=ALU.add))
                layer_norm(zt, ztB, D, l1g, l1gB, l1b, l1bB, x1t[:], x1B)
                D_(spq, [x1B], [x1dBs[ti]], lambda: nc.sync.dma_start(out=x1_d[ti], in_=x1t[:]))
                m4B = mod_chunk(4)
                m3B = mod_chunk(3)
                D_(pool, [x1B, m4B], [xB], lambda: cnt_read(4, nc.gpsimd.tensor_tensor(xtile[:], x1t[:], mod[:, 4 * D:5 * D], op=ALU.mult)))
                D_(pool, [xB, m3B], [h2bB], lambda: cnt_read(3, nc.gpsimd.tensor_tensor(h2b[:], xtile[:], mod[:, 3 * D:4 * D], op=ALU.add)))
                D_(spq, [h2bB], [h2tokBs[ti]], lambda: nc.sync.dma_start(out=h2tok_d[ti], in_=h2b[:]))
                bk, bkB = nb()
                bkb = bk[:].bitcast(BF16)
                for k in range(8):
                    D_(pe, [h2bB, identbB], [bkB], lambda k=k, bkb=bkb: nc.tensor.transpose(
                        bkb[:, k * 128:(k + 1) * 128], h2b[:, k * 128:(k + 1) * 128], identb[:]), fin=(k == 7))
                D_(act, [bkB], [h2TB], lambda bkb=bkb: nc.scalar.copy(h2T[:].rearrange("p k t -> p (k t)"), bkb[:, 0:1024]))
                bkr, bkrB = nb()
                for k in range(8):
                    D_(pe, [h2TB, wrB], [bkrB], lambda k=k, bkr=bkr: nc.tensor.matmul(
                        bkr[:, 0:36], h2T[:, k, :], wrb[:, k, :], start=(k == 0), stop=(k == 7)), fin=(k == 7))
                D_(dve, [bkrB, rbiasB], [rlaB], lambda bkr=bkr: nc.vector.tensor_tensor(rla[:, ti, :], bkr[:, 0:36], rbias[:], op=ALU.add))

            order = list(range(NPT)) + [NPT]
            emit_skewed([lambda D_, it=it, ti=ti: tile_body(it, ti, D_) for it, ti in enumerate(order)], NSET, n_umax=NPT - 1)

            barrier()
            s1b.__exit__(None, None, None)
            G = nc.gpsimd
            bc_x = G.to_reg(NROWS - 1)
            bc_w = G.to_reg(NE * 128 - 1)
            bc_x2 = G.to_reg(NROWS - 2)
            IOA = bass.IndirectOffsetOnAxis
            h2all, h2allB = sbt(s1, "h2all", [128, NTILE, D], BF16)
            T.op(spq, h2tokBs, [h2allB], lambda: nc.sync.dma_start(out=h2all[:], in_=h2tok_d.rearrange("t p n -> p t n")))
            zpad, zpadB = sbt(s1, "zpad", [128, 2, D])
            T.op(pool, [], [zpadB], lambda: nc.gpsimd.memset(zpad[:], 0.0))
            NPAIR = 2 * NTILE * 128
            T.op(plq, [zpadB], [Buf("xs_pad")], lambda: nc.gpsimd.dma_start(
                out=xs_d[NPAIR:NROWS, :].rearrange("(p a) n -> p a n", a=2), in_=zpad[:]))
            T.op(spq, [zpadB], [Buf("y_pad")], lambda: nc.sync.dma_start(
                out=y_d[NPAIR:NROWS, :].rearrange("(p a) n -> p a n", a=2), in_=zpad[:]))
            V = nc.vector
            NT = NTILE
            gmax, rtB = sbt(s1, "gmax", [128, NT])
            ohg, _ = sbt(s1, "ohg", [128, NT, 4]); ex, _ = sbt(s1, "ex", [128, NT, 4])
            sumex, _ = sbt(s1, "sumex", [128, NT]); pg, _ = sbt(s1, "pg", [128, NT])
            lsel, _ = sbt(s1, "lsel", [128, NT, 8]); ltmp, _ = sbt(s1, "ltmp", [128, NT, 8])
            v1, _ = sbt(s1, "v1", [128, NT]); v2, _ = sbt(s1, "v2", [128, NT])
            oh1, _ = sbt(s1, "oh1", [128, NT, 8]); oh2, _ = sbt(s1, "oh2", [128, NT, 8]); msk, _ = sbt(s1, "msk", [128, NT, 8])
            wA, _ = sbt(s1, "wA", [128, NT]); wB_, _ = sbt(s1, "wB", [128, NT]); within, _ = sbt(s1, "within", [128, NT, 8])

            def bcl(ap2, n):
                a = [list(x) for x in ap2.ap]
                if len(a) == 3:
                    assert a[2][1] == 1
                    a = a[:2]
                assert len(a) == 2
                return bass.AP(ap2.tensor, ap2.offset, a + [[0, n]])

            def R(emit, q=dve, extra_r=(), extra_w=()):
                T.op(q, [rtB, rlaB] + list(extra_r), [rtB] + list(extra_w), emit)

            lg = rla[:, :, 0:4]
            R(lambda: V.tensor_reduce(gmax[:], lg, axis=AX.X, op=ALU.max))
            R(lambda: V.tensor_tensor(ohg[:], lg, bcl(gmax[:], 4), op=ALU.is_equal))
            R(lambda: V.tensor_tensor(ex[:], lg, bcl(gmax[:], 4), op=ALU.subtract))
            R(lambda: nc.scalar.activation(out=ex[:], in_=ex[:], func=AF.Exp), q=act)
            R(lambda: V.tensor_reduce(sumex[:], ex[:], axis=AX.X, op=ALU.add))
            R(lambda: V.reciprocal(pg[:], sumex[:]))
            for g in range(4):
                srcl = rla[:, :, 4 + 8 * g:12 + 8 * g]
                selb = bcl(ohg[:, :, g], 8)
                if g == 0:
                    R(lambda srcl=srcl, selb=selb: V.tensor_tensor(lsel[:], srcl, selb, op=ALU.mult))
                else:
                    R(lambda srcl=srcl, selb=selb: V.tensor_tensor(ltmp[:], srcl, selb, op=ALU.mult))
                    R(lambda: V.tensor_tensor(lsel[:], lsel[:], ltmp[:], op=ALU.add))
            R(lambda: V.tensor_reduce(v1[:], lsel[:], axis=AX.X, op=ALU.max))
            R(lambda: V.tensor_tensor(oh1[:], lsel[:], bcl(v1[:], 8), op=ALU.is_equal))
            R(lambda: V.scalar_tensor_tensor(out=msk[:], in0=oh1[:], scalar=-1e30, in1=lsel[:], op0=ALU.mult, op1=ALU.add))
            R(lambda: V.tensor_reduce(v2[:], msk[:], axis=AX.X, op=ALU.max))
            R(lambda: V.tensor_tensor(oh2[:], msk[:], bcl(v2[:], 8), op=ALU.is_equal))
            R(lambda: V.tensor_tensor(wA[:], v1[:], v2[:], op=ALU.subtract))
            R(lambda: nc.scalar.activation(out=wA[:], in_=wA[:], func=AF.Sigmoid), q=act)
            R(lambda: V.tensor_scalar(wB_[:], wA[:], -1.0, 1.0, op0=ALU.mult, op1=ALU.add))
            R(lambda: V.tensor_tensor(wA[:], wA[:], pg[:], op=ALU.mult))
            R(lambda: V.tensor_tensor(wB_[:], wB_[:], pg[:], op=ALU.mult))
            R(lambda: V.tensor_copy(gw[:, 0, :], wA[:]), extra_w=[gwB])
            R(lambda: V.tensor_copy(gw[:, 1, :], wB_[:]), extra_w=[gwB])

            def bcm(ap2, m):
                a = [list(x) for x in ap2.ap]
                assert len(a) == 2
                return bass.AP(ap2.tensor, ap2.offset, [a[0], [0, m], a[1]])

            M1, _ = sbt(s1, "M1", [128, NT, 32]); M2, _ = sbt(s1, "M2", [128, NT, 32])
            Mb, MbB = sbt(s1, "Mb", [128, NT, 32], BF16)
            cst_sb, cstB = sbt(s1, "cst_sb", [128, 160]); cm_f, cmfB = sbt(s1, "cm_f", [128, 256])
            cm_b, cmbB = sbt(s1, "cm_b", [128, 256], BF16)
            rank_sb, _ = sbt(s1, "rank_sb", [128, NT, 32]); tot_sb, _ = sbt(s1, "tot_sb", [128, 32])
            incl, _ = sbt(s1, "incl", [128, 32]); off, _ = sbt(s1, "off", [128, 32])
            posf, _ = sbt(s1, "posf", [128, 2, NT])
            cmpA, _ = sbt(s1, "cmpA", [128, 32, 16]); cmpB, _ = sbt(s1, "cmpB", [128, 16, 32])
            rko, _ = sbt(s1, "rko", [128, 32]); rka, _ = sbt(s1, "rka", [128, 16])
            big, _ = sbt(s1, "big", [128, NSLOT, 32]); big2, _ = sbt(s1, "big2", [128, NSLOT, 16])
            psf, _ = sbt(s1, "psf", [128, NSLOT]); psf2, _ = sbt(s1, "psf2", [128, NSLOT]); esf, _ = sbt(s1, "esf", [128, NSLOT])
            T.op(spq, [], [cstB], lambda: nc.sync.dma_start(out=cst_sb[:], in_=cst))
            T.op(spq, [], [cmfB], lambda: nc.sync.dma_start(out=cm_f[:], in_=cmat))
            T.op(dve, [cmfB], [cmbB], lambda: nc.vector.tensor_copy(cm_b[:], cm_f[:]))
            pidx = cst_sb[:, 0:1]; p2idx = cst_sb[:, 1:2]; a_j = cst_sb[:, 2:18]; eidx = cst_sb[:, 18:50]
            sidx = cst_sb[:, 50:50 + NSLOT]; jm1 = cst_sb[:, 98:114]; ones32 = cst_sb[:, 114:146]
            for g in range(4):
                R(lambda g=g: V.tensor_tensor(M1[:, :, 8 * g:8 * g + 8], oh1[:], bcl(ohg[:, :, g], 8), op=ALU.mult))
                R(lambda g=g: V.tensor_tensor(M2[:, :, 8 * g:8 * g + 8], oh2[:], bcl(ohg[:, :, g], 8), op=ALU.mult))
            R(lambda: V.tensor_tensor(Mb[:], M1[:], M2[:], op=ALU.add), extra_w=[MbB])
            rkA, rkAB = nbank(); rkB, rkBB = nbank()
            for ti in range(NT):
                bk, bkB_ = (rkA, rkAB) if ti < 16 else (rkB, rkBB)
                c0 = (ti % 16) * 32
                T.op(pe, [MbB, cmbB], [bkB_], lambda bk=bk, c0=c0, ti=ti: nc.tensor.matmul(
                    bk[:, c0:c0 + 32], cm_b[:, 0:128], Mb[:, ti, :], start=True, stop=(ti == 0)), fin=(ti == 0))
                for tj in range(ti):
                    T.op(pe, [MbB, cmbB], [bkB_], lambda bk=bk, c0=c0, tj=tj, ti=ti: nc.tensor.matmul(
                        bk[:, c0:c0 + 32], cm_b[:, 128:256], Mb[:, tj, :], start=False, stop=(tj == ti - 1)), fin=(tj == ti - 1))
            for tj in range(NT):
                T.op(pe, [MbB, cmbB], [rkBB], lambda tj=tj: nc.tensor.matmul(
                    rkB[:, 32:64], cm_b[:, 128:256], Mb[:, tj, :], start=(tj == 0), stop=(tj == NT - 1)), fin=(tj == NT - 1))
            R(lambda: nc.scalar.copy(rank_sb[:, 0:16, :].rearrange("p t e -> p (t e)"), rkA[:, 0:512]), q=act, extra_r=[rkAB])
            R(lambda: nc.scalar.copy(rank_sb[:, 16, :], rkB[:, 0:32]), q=act, extra_r=[rkBB])
            R(lambda: nc.scalar.copy(tot_sb[:], rkB[:, 32:64]), q=act, extra_r=[rkBB])
            R(lambda: V.tensor_tensor_scan(out=incl[:], data0=ones32, data1=tot_sb[:], initial=0.0, op0=ALU.mult, op1=ALU.add),
              extra_r=[cstB])
            R(lambda: V.tensor_tensor(off[:], incl[:], tot_sb[:], op=ALU.subtract))
            R(lambda: V.tensor_tensor(rank_sb[:], rank_sb[:], bcm(off[:], NT), op=ALU.add))
            R(lambda: V.tensor_tensor(M1[:], M1[:], rank_sb[:], op=ALU.mult))
            R(lambda: V.tensor_reduce(posf[:, 0, :], M1[:], axis=AX.X, op=ALU.add))
            R(lambda: V.tensor_tensor(M2[:], M2[:], rank_sb[:], op=ALU.mult))
            R(lambda: V.tensor_reduce(posf[:, 1, :], M2[:], axis=AX.X, op=ALU.add))
            R(lambda: V.tensor_copy(pos_i[:], posf[:]), extra_w=[posB])
            for ti in range(NTILE):
                for k in range(2):
                    T.op(plq, [h2allB, posB], [Buf(f"xs_{ti}_{k}")], lambda ti=ti, k=k: G.indirect_dma_start(
                        out=xs_d[:, :], out_offset=IOA(ap=pos_i[:, k, ti:ti + 1], axis=0),
                        in_=h2all[:, ti, :], in_offset=None, bounds_check=bc_x, oob_is_err=False))
            R(lambda: V.tensor_tensor(cmpA[:], bcl(off[:], 16), bcm(a_j, 32), op=ALU.is_ge))
            R(lambda: V.tensor_reduce(rko[:], cmpA[:], axis=AX.X, op=ALU.add))
            R(lambda: V.tensor_tensor(rko[:], rko[:], eidx, op=ALU.add))
            R(lambda: V.tensor_tensor(cmpB[:], bcl(a_j, 32), bcm(off[:], 16), op=ALU.is_gt))
            R(lambda: V.tensor_reduce(rka[:], cmpB[:], axis=AX.X, op=ALU.add))
            R(lambda: V.tensor_tensor(rka[:], rka[:], jm1, op=ALU.add))
            R(lambda: V.tensor_tensor(big[:], bcm(rko[:], NSLOT), bcl(sidx, 32), op=ALU.is_equal))
            R(lambda: V.tensor_tensor(big[:], big[:], bcm(off[:], NSLOT), op=ALU.mult))
            R(lambda: V.tensor_reduce(psf[:], big[:], axis=AX.X, op=ALU.add))
            R(lambda: V.tensor_tensor(big2[:], bcm(rka[:], NSLOT), bcl(sidx, 16), op=ALU.is_equal))
            R(lambda: V.tensor_tensor(big2[:], big2[:], bcm(a_j, NSLOT), op=ALU.mult))
            R(lambda: V.tensor_reduce(psf2[:], big2[:], axis=AX.X, op=ALU.add))
            R(lambda: V.tensor_tensor(psf[:], psf[:], psf2[:], op=ALU.add))
            R(lambda: V.tensor_tensor(big[:], bcm(rko[:], NSLOT), bcl(sidx, 32), op=ALU.is_le))
            R(lambda: V.tensor_reduce(esf[:], big[:], axis=AX.X, op=ALU.add))
            R(lambda: V.tensor_scalar(esf[:], esf[:], 128.0, -128.0, op0=ALU.mult, op1=ALU.add))
            R(lambda: V.tensor_scalar(esf[:], esf[:], pidx, None, op0=ALU.add))
            R(lambda: V.tensor_copy(idx_w[:], esf[:]), extra_w=[idxB])
            pend, _ = sbt(s1, "pend", [128, NSLOT]); rowf, _ = sbt(s1, "rowf", [128, NSLOT]); inval, _ = sbt(s1, "inval", [128, NSLOT])
            R(lambda: V.tensor_copy(pend[:, 0:NSLOT - 1], psf[:, 1:NSLOT]))
            R(lambda: V.memset(pend[:, NSLOT - 1:NSLOT], float(2 * NTILE * 128)))
            R(lambda: V.tensor_scalar(psf[:], psf[:], p2idx, None, op0=ALU.add))
            R(lambda: V.tensor_copy(idx_x[:], psf[:]), extra_w=[idxB])
            for sub in range(2):
                R(lambda sub=sub: V.tensor_scalar(rowf[:], psf[:], float(sub), None, op0=ALU.add))
                R(lambda: V.tensor_tensor(inval[:], rowf[:], pend[:], op=ALU.is_ge))
                R(lambda: V.scalar_tensor_tensor(out=rowf[:], in0=inval[:], scalar=1048576.0, in1=rowf[:], op0=ALU.mult, op1=ALU.add))
                R(lambda sub=sub: V.tensor_copy(idx_y[:, sub, :], rowf[:]), extra_w=[idxB])

            barrier()
        s2 = ExitStack()
        with s2:
            s2b = ExitStack()
            s2b.__enter__()
            NW = 3
            WB = [[sbt(s2b, f"w{n}b{i}", [128, 4096], BF16) for n in (1, 3, 2)] for i in range(NW)]
            XRb = [sbt(s2b, f"xrb{i}", [128, 2, D], BF16) for i in range(NW)]
            XRb2 = [Buf(f"xrb{i}_1") for i in range(NW)]
            for (xr0_, xr0B_), xr0B2_ in zip(XRb, XRb2):
                T.op(pool, [], [xr0B_, xr0B2_], lambda xr0_=xr0_: nc.gpsimd.memset(xr0_[:], 0.0))
            XT = [sbt(s2b, f"xT{i}", [128, 8, 256], BF16) for i in range(2)]
            hdn = [sbt(s2b, f"hdn{i}", [128, 4, 256], BF16) for i in range(2)]
            sil = [sbt(s2b, f"sil{i}", [128, 256]) for i in range(2)]
            NY = 4
            ysb = [sbt(s2b, f"ysb{i}", [128, 2, D]) for i in range(NY)]
            ydB = Buf("y_d")
            cT = [0]; cH = [0]; cO = [0]

            def bankT():
                b = banks[cT[0] % 2]; cT[0] += 1; return b

            def bankH():
                b = banks[2 + cH[0] % 4]; cH[0] += 1; return b

            def bankO():
                b = banks[6 + cO[0] % 2]; cO[0] += 1; return b

            def loads(s):
                xr_, xrB_ = XRb[s % NW]
                for sub in range(2):
                    T.op(plq, [idxB], [xrB_ if sub == 0 else XRb2[s % NW]], lambda sub=sub: G.indirect_dma_start(
                        out=xr_[:, sub, :], out_offset=None, in_=xs_d[:, :],
                        in_offset=IOA(ap=idx_y[:, sub, s:s + 1], axis=0), bounds_check=bc_x, oob_is_err=False))
                for (wt_, wB_), src in zip(WB[s % NW], (w1, w3, w2)):
                    T.op(plq, [idxB], [wB_], lambda wt_=wt_, src=src: G.indirect_dma_start(
                        out=wt_[:, :], out_offset=None, in_=src[:, :],
                        in_offset=IOA(ap=idx_w[:, s:s + 1], axis=0), bounds_check=bc_w, oob_is_err=False))

            def prep(s):
                xr_, xrB_ = XRb[s % NW]
                xT_, xTB_ = XT[s % 2]
                for sub in range(2):
                    bk, bkB = bankT()
                    bkb = bk[:].bitcast(BF16)
                    for k in range(8):
                        T.op(pe, [xrB_ if sub == 0 else XRb2[s % NW], identbB], [bkB], lambda k=k, bkb=bkb, sub=sub: nc.tensor.transpose(
                            bkb[:, k * 128:(k + 1) * 128], xr_[:, sub, bass.ds(k, 128, step=8)], identb[:]), fin=(k == 7))
                    dstv = xT_[:, :, sub * 128:(sub + 1) * 128]
                    srcv = bkb[:, 0:1024].rearrange("p (k t) -> p k t", k=8)
                    if sub == 0:
                        T.op(act, [bkB], [xTB_], lambda dstv=dstv, srcv=srcv: nc.scalar.copy(dstv, srcv))
                    else:
                        T.op(dve, [bkB], [xTB_], lambda dstv=dstv, srcv=srcv: nc.vector.tensor_copy(dstv, srcv))

            def hstage(s):
                (w1t, w1B), (w3t, w3B), (w2t, w2B) = WB[s % NW]
                xT_, xTB_ = XT[s % 2]
                hd_, hdB = hdn[s % 2]
                for j in range(4):
                    b1, b1B = bankH()
                    b3, b3B = bankH()
                    for k in range(8):
                        T.op(pe, [w1B, xTB_], [b1B], lambda k=k, j=j, b1=b1: nc.tensor.matmul(
                            b1[:, 0:256], w1t[:, bass.ds(k * 512 + j, 128, step=4)], xT_[:, k, :], start=(k == 0), stop=(k == 7)), fin=(k == 7))
                    for k in range(8):
                        T.op(pe, [w3B, xTB_], [b3B], lambda k=k, j=j, b3=b3: nc.tensor.matmul(
                            b3[:, 0:256], w3t[:, bass.ds(k * 512 + j, 128, step=4)], xT_[:, k, :], start=(k == 0), stop=(k == 7)), fin=(k == 7))
                    st_, sB = sil[j % 2]
                    T.op(act, [b1B], [sB], lambda b1=b1, st_=st_: nc.scalar.activation(out=st_[:], in_=b1[:, 0:256], func=AF.Silu))
                    T.op(dve, [sB, b3B], [hdB], lambda b3=b3, st_=st_, j=j: nc.vector.tensor_tensor(
                        hd_[:, j, :], st_[:], b3[:, 0:256], op=ALU.mult))

            def ystage(s):
                (w1t, w1B), (w3t, w3B), (w2t, w2B) = WB[s % NW]
                hd_, hdB = hdn[s % 2]
                y_, yB_ = ysb[s % NY]
                for sub in range(2):
                    for cb in range(2):
                        bo, boB = bankO()
                        for j in range(4):
                            T.op(pe, [hdB, w2B], [boB], lambda j=j, bo=bo, sub=sub, cb=cb: nc.tensor.matmul(
                                bo[:], hd_[:, j, sub * 128:(sub + 1) * 128], w2t[:, j * 1024 + cb * 512:j * 1024 + (cb + 1) * 512],
                                start=(j == 0), stop=(j == 3)), fin=(j == 3))
                        dst = y_[:, sub, cb * 512:(cb + 1) * 512]
                        if cb == 0:
                            T.op(act, [boB], [yB_], lambda bo=bo, dst=dst: nc.scalar.copy(dst, bo[:]))
                        else:
                            T.op(dve, [boB], [yB_], lambda bo=bo, dst=dst: nc.vector.tensor_copy(dst, bo[:]))

            def scatter(s):
                y_, yB_ = ysb[s % NY]
                for sub in range(2):
                    T.op(plq, [yB_, idxB], [Buf(f"ysc{s}_{sub}")], lambda sub=sub: G.indirect_dma_start(
                        out=y_d[:, :], out_offset=IOA(ap=idx_y[:, sub, s:s + 1], axis=0),
                        in_=y_[:, sub, :], in_offset=None, bounds_check=bc_x, oob_is_err=False))

            loads(0)
            loads(1)
            prep(0)
            for s in range(NSLOT):
                if s + 2 < NSLOT:
                    loads(s + 2)
                hstage(s)
                if s + 1 < NSLOT:
                    prep(s + 1)
                ystage(s)
                if s >= 1:
                    scatter(s - 1)
            scatter(NSLOT - 1)

            barrier()
            s2b.__exit__(None, None, None)
            l2g, l2gB = sbt(s2, "l2g", [128, D]); l2b, l2bB = sbt(s2, "l2b", [128, D])
            T.op(spq, [], [l2gB], lambda: nc.sync.dma_start(out=l2g[:], in_=dram_bc(ln2_g, 0, 128, D)))
            T.op(spq, [], [l2bB], lambda: nc.sync.dma_start(out=l2b[:], in_=dram_bc(ln2_b, 0, 128, D)))
            N3 = 3
            g2t, g2B = sbt(s2, "g2t3", [128, 2, D])
            T.op(spq, [g2dB], [g2B], lambda: nc.sync.dma_start(out=g2t[:], in_=g2_d))
            x1r = [sbt(s2, f"x1r{i}", [128, D]) for i in range(N3)]
            yg = [[sbt(s2, f"yg{i}_{k}", [128, D]) for k in range(2)] for i in range(N3)]
            accs = [sbt(s2, f"acc{i}", [128, D]) for i in range(N3)]
            yo = [sbt(s2, f"yo{i}", [128, D]) for i in range(N3)]
            st6s = [sbt(s2, f"st6b{i}", [128, 2, 6]) for i in range(N3)]
            mvs = [sbt(s2, f"mvb{i}", [128, 4]) for i in range(N3)]
            outB2 = Buf("outs2")

            def p3_body(ti, D_):
                samp = (ti == NPT)
                xr_, xrB_ = x1r[ti % N3]
                y_, yB_ = yo[ti % N3]
                acc, accB = accs[ti % N3]
                st6, st6B = st6s[ti % N3]
                mv, mvB = mvs[ti % N3]
                (y1, y1B), (y2, y2B) = yg[ti % N3]
                for k, (yt, ytB) in enumerate(((y1, y1B), (y2, y2B))):
                    D_(plq, [ydB, posB], [ytB], lambda k=k, yt=yt: G.indirect_dma_start(
                        out=yt[:, :], out_offset=None, in_=y_d[:, :],
                        in_offset=IOA(ap=pos_i[:, k, ti:ti + 1], axis=0), bounds_check=bc_x, oob_is_err=False))
                D_(spq, [x1dBs[ti]], [xrB_], lambda: nc.sync.dma_start(out=xr_[:], in_=x1_d[ti]))
                D_(act, [y1B, gwB], [accB], lambda: nc.scalar.activation(
                    out=acc[:], in_=y1[:], func=AF.Copy, scale=gw[:, 0, ti:ti + 1]))
                D_(dve, [y2B, gwB, accB], [accB], lambda: nc.vector.scalar_tensor_tensor(
                    out=acc[:], in0=y2[:], scalar=gw[:, 1, ti:ti + 1], in1=acc[:], op0=ALU.mult, op1=ALU.add))
                D_(pool, [accB, g2B], [accB], lambda: nc.gpsimd.tensor_tensor(
                    acc[:], acc[:], g2t[:, 1 if samp else 0, :], op=ALU.mult))
                D_(dve, [accB, xrB_], [accB], lambda: nc.vector.scalar_tensor_tensor(
                    out=acc[:], in0=xr_[:], scalar=ALPHA, in1=acc[:], op0=ALU.mult, op1=ALU.add))
                for i in range(2):
                    D_(dve, [accB], [st6B], lambda i=i: nc.vector.bn_stats(st6[:, i, :], acc[:, i * 512:(i + 1) * 512]))
                D_(dve, [st6B], [mvB], lambda: nc.vector.bn_aggr(mv[:, 0:2], st6[:].rearrange("p a b -> p (a b)")))
                D_(act, [mvB], [mvB], lambda: nc.scalar.activation(out=mv[:, 3:4], in_=mv[:, 1:2], func=AF.Sqrt, bias=EPS, scale=1.0))
                D_(dve, [mvB], [mvB], lambda: nc.vector.reciprocal(mv[:, 2:3], mv[:, 3:4]))
                D_(dve, [mvB], [mvB], lambda: nc.vector.tensor_scalar(
                    mv[:, 3:4], mv[:, 0:1], mv[:, 2:3], -1.0, op0=ALU.mult, op1=ALU.mult))
                D_(act, [accB, mvB], [yB_], lambda: nc.scalar.activation(
                    out=y_[:], in_=acc[:], func=AF.Identity, bias=mv[:, 3:4], scale=mv[:, 2:3]))
                D_(dve, [yB_, l2gB], [yB_], lambda: nc.vector.tensor_tensor(y_[:], y_[:], l2g[:], op=ALU.mult))
                D_(pool, [yB_, l2bB], [yB_], lambda: nc.gpsimd.tensor_tensor(y_[:], y_[:], l2b[:], op=ALU.add))
                dstd = ys if samp else yp[ti * 128:(ti + 1) * 128, :]
                D_(spq, [yB_], [Buf("o")], lambda: nc.sync.dma_start(out=dstd, in_=y_[:]))

            emit_skewed([lambda D_, ti=ti: p3_body(ti, D_) for ti in range(NTILE)], N3)
            if debug:
                dbgB = Buf("dbg")
                T.op(spq, [posB], [dbgB], lambda: nc.sync.dma_start(out=dbg_pos, in_=pos_i[:].rearrange("p a t -> p (a t)")))
                T.op(spq, [idxB], [dbgB], lambda: nc.sync.dma_start(out=dbg_iw, in_=idx_w[:]))
                T.op(spq, [idxB], [dbgB], lambda: nc.sync.dma_start(out=dbg_ix, in_=idx_x[:]))
                T.op(spq, [gwB], [dbgB], lambda: nc.sync.dma_start(out=dbg_gw, in_=gw[:].rearrange("p a t -> p (a t)")))
            barrier()
    return nc


def _consts():
    c = np.zeros((128, 160), np.float32)
    p = np.arange(128, dtype=np.float32)
    c[:, 0] = p
    c[:, 1] = 2 * p
    c[:, 2:18] = 256.0 * np.arange(1, 17, dtype=np.float32)[None, :]
    c[:, 18:50] = np.arange(32, dtype=np.float32)[None, :]
    c[:, 50:98] = np.arange(48, dtype=np.float32)[None, :]
    c[:, 98:114] = np.arange(16, dtype=np.float32)[None, :]
    c[:, 114:146] = 1.0
    m = np.zeros((128, 256), np.float32)
    m[:, 0:128] = np.triu(np.ones((128, 128), np.float32), k=1)
    m[:, 128:256] = 1.0
    return c, m


def _in_maps(inp):
    f = lambda a: np.ascontiguousarray(np.asarray(a, dtype=np.float32))
    shared = {
        "w_ada": f(inp["w_ada"][0]), "b_ada": f(inp["b_ada"]), "w_in": f(inp["w_in"][0]),
        "w_s": f(inp["w_s"][0]), "b_s": f(inp["b_s"][0]), "lnv_g": f(inp["lnv_g"]), "lnv_b": f(inp["lnv_b"]),
        "conv_w": f(inp["conv_w"][0]).reshape(16, 128), "conv_b": f(inp["conv_b"]).reshape(4, 128),
        "lru_wa": f(inp["lru_wa"][0]), "lru_ba": f(inp["lru_ba"][0]).reshape(4, 128),
        "lru_wx": f(inp["lru_wx"][0]), "lru_bx": f(inp["lru_bx"][0]).reshape(4, 128),
        "lru_lam": f(inp["lru_lam"]).reshape(4, 128),
        "w_out": f(inp["w_out"][0]), "ln1_g": f(inp["ln1_g"]), "ln1_b": f(inp["ln1_b"]),
        "w_rg": f(inp["w_rg"][0]), "b_rg": f(inp["b_rg"]), "w_re": f(inp["w_re"][0]), "b_re": f(inp["b_re"]),
        "w1": f(inp["w1"][0]).reshape(NE * 128, 4096), "w3": f(inp["w3"][0]).reshape(NE * 128, 4096),
        "w2": f(inp["w2"][0]).reshape(NE * 128, 4096), "cst": _consts()[0], "cmat": _consts()[1],
        "ln2_g": f(inp["ln2_g"]), "ln2_b": f(inp["ln2_b"]),
    }
    x_prompt = f(inp["x_prompt"]); x_sample = f(inp["x_sample"])
    sh = f(inp["state_rglru_h"])[0]; sc = f(inp["state_conv"])[0]
    c_p = f(inp["c_prompt"]); c_s = f(inp["c_sample"])
    in_maps = []
    for c in range(NCORE):
        m = dict(shared)
        m["xp"] = x_prompt[c]
        m["xs"] = np.ascontiguousarray(x_sample[NSS * c:NSS * (c + 1)].reshape(128, D))
        m["sth"] = np.ascontiguousarray(sh[NSS * c:NSS * (c + 1)])
        m["stc"] = np.ascontiguousarray(sc[NSS * c:NSS * (c + 1)].reshape(NSS * 3, 512))
        m["cp"] = np.ascontiguousarray(c_p[c:c + 1])
        m["cs"] = np.ascontiguousarray(c_s[NSS * c:NSS * (c + 1)])
        in_maps.append(m)
    return in_maps


def kernel(**inp):
    in_maps = _in_maps(inp)
    nc = build_nc()
    res = run_bass_kernel_spmd(nc, in_maps, core_ids=list(range(NCORE)))
    R = res.results
    y_prompt = np.stack([R[c]["yp"] for c in range(NCORE)], 0).astype(np.float32)
    y_sample = np.concatenate([R[c]["ys"].reshape(NSS, LS, D) for c in range(NCORE)], 0).astype(np.float32)
    new_h_prompt = np.concatenate([R[c]["hp"] for c in range(NCORE)], 0)[None].astype(np.float32)
    new_conv_prompt = np.stack([R[c]["cpo"] for c in range(NCORE)], 0)[None].astype(np.float32)
    new_h_sample = np.concatenate([R[c]["hs"] for c in range(NCORE)], 0)[None].astype(np.float32)
    new_conv_sample = np.concatenate([R[c]["cso"].reshape(NSS, 3, 512) for c in range(NCORE)], 0)[None].astype(np.float32)
    new_chunk_v = np.concatenate([R[c]["vs"].reshape(NSS, LS, 512) for c in range(NCORE)], 0)[None].astype(np.float32)
    return (y_prompt, y_sample, new_h_prompt, new_conv_prompt, new_h_sample, new_conv_sample, new_chunk_v)
```
